# Optimizing a Trainium2 kernel written in Bass

```python
import math
import jax
import jax.numpy as jnp
from jax import lax
import numpy as np

D_MODEL = 1024
BATCH = 4
SEQ = 4096
DEPTH = 4

GRID_W = 64
CTX_LEN = 256
N_GROUPS = 4
GROUP_W = D_MODEL // N_GROUPS
GROUP_HEADS = 4
HEAD_DIM = GROUP_W // GROUP_HEADS
NA_WIN_H = 8
NA_WIN_W = 16
DN_CONV_K = 5
DN_CHUNK = 64
DIFF_DIM = HEAD_DIM // 2
Q_BLOCK = 128
FT_DIM = GROUP_W // GROUP_HEADS
N_EXPERTS = 16
EC_FACTOR = 2
D_EXPERT = 2 * D_MODEL
ROPE_BASE = 10000.0
RMS_EPS = 1e-6
IN_WIDTHS = (GROUP_W, GROUP_W, GROUP_W, 3 * GROUP_W, 2 * GROUP_HEADS, 2 * GROUP_HEADS, GROUP_W, GROUP_W, GROUP_W, GROUP_W, GROUP_W)
IN_W = 11 * GROUP_W + 4 * GROUP_HEADS
F32 = jnp.float32

kernel_name = 'hybrid_parallel_group_dit_block'


def rmsnorm(x, g):
    xf = x.astype(F32)
    y = xf * lax.rsqrt(jnp.mean(xf * xf, axis=-1, keepdims=True) + RMS_EPS)
    return (y * g.astype(F32)).astype(x.dtype)


def l2norm(x):
    return x * lax.rsqrt(jnp.sum(x * x, axis=-1, keepdims=True) + 1e-6)


def modulate(h, shift, scale):
    return h * (1 + scale) + shift


def split_heads(t, d=HEAD_DIM):
    return t.reshape(*t.shape[:-1], t.shape[-1] // d, d)


def split_proj(p):
    idx, acc = [], 0
    for w in IN_WIDTHS[:-1]:
        acc += w
        idx.append(acc)
    return jnp.split(p, idx, axis=-1)


def axial_rope(n_tok, dim):
    t = jnp.arange(n_tok)
    pos = jnp.stack([t // GRID_W, t % GRID_W], axis=-1).astype(F32)
    n_freq = dim // 4
    inv = ROPE_BASE ** (-jnp.arange(n_freq, dtype=F32) / n_freq)
    ang = (pos[:, :, None] * inv).reshape(n_tok, 2 * n_freq)
    return jnp.cos(ang), jnp.sin(ang)


def apply_rope(x, cos, sin):
    xf = x.astype(F32).reshape(*x.shape[:-1], -1, 2)
    x1, x2 = xf[..., 0], xf[..., 1]
    out = jnp.stack([x1 * cos - x2 * sin, x1 * sin + x2 * cos], axis=-1)
    return out.reshape(x.shape).astype(x.dtype)


def softmax_attention(q, k, v):
    s = jnp.einsum('bqhd,bkhd->bhqk', q, k).astype(F32) * q.shape[-1] ** -0.5
    p = jax.nn.softmax(s, axis=-1)
    return jnp.einsum('bhqk,bkhd->bqhd', p, v.astype(F32))


def neighbourhood_attention(q, k, v, k_ctx, v_ctx, rpb):
    B, S, H, dh = q.shape
    rows = S // GRID_W
    wh = min(NA_WIN_H, rows)
    r = jnp.arange(rows)
    row_idx = jnp.clip(r - wh // 2, 0, rows - wh)[:, None] + jnp.arange(wh)[None, :]
    cq = jnp.arange(GRID_W)
    c0 = jnp.clip(cq - NA_WIN_W // 2, 0, GRID_W - NA_WIN_W)
    in_win = (cq[None, :] >= c0[:, None]) & (cq[None, :] < c0[:, None] + NA_WIN_W)
    dy = row_idx - r[:, None] + NA_WIN_H - 1
    dx = jnp.clip(cq[None, :] - cq[:, None], 1 - NA_WIN_W, NA_WIN_W - 1) + NA_WIN_W - 1
    bias = rpb[:, dy[:, None, :, None], dx[None, :, None, :]].astype(F32)
    qg = q.reshape(B, rows, GRID_W, H, dh)
    k_blk = k.reshape(B, rows, GRID_W, H, dh)[:, row_idx]
    v_blk = v.reshape(B, rows, GRID_W, H, dh)[:, row_idx]
    scale = dh ** -0.5
    s_win = jnp.einsum('brqhd,brwkhd->bhrqwk', qg, k_blk).astype(F32) * scale + bias
    s_win = jnp.where(in_win[:, None, :], s_win, -jnp.inf)
    s_ctx = jnp.einsum('brqhd,blhd->bhrql', qg, k_ctx).astype(F32) * scale
    n_win = wh * GRID_W
    p = jax.nn.softmax(jnp.concatenate([s_win.reshape(B, H, rows, GRID_W, n_win), s_ctx], axis=-1), axis=-1)
    p_win = p[..., :n_win].reshape(B, H, rows, GRID_W, wh, GRID_W)
    p_ctx = p[..., n_win:]
    out = jnp.einsum('bhrqwk,brwkhd->brqhd', p_win, v_blk.astype(F32)) + jnp.einsum('bhrql,blhd->brqhd', p_ctx, v_ctx.astype(F32))
    return out.reshape(B, S, H, dh)


def short_conv(u, w):
    return lax.conv_general_dilated(u, w[:, None, :].astype(u.dtype), window_strides=(1,), padding=[(DN_CONV_K // 2, DN_CONV_K // 2)], dimension_numbers=('NWC', 'WIO', 'NWC'), feature_group_count=u.shape[-1])


def chunk_gated_delta(q, k, v, g, beta, s0):
    B, T, H, dk = q.shape
    n, C = T // DN_CHUNK, DN_CHUNK

    def chunks(t):
        return jnp.swapaxes(t.reshape(B, n, C, H, *t.shape[3:]), 2, 3)
    qc, kc, vc, bc = chunks(q), chunks(k), chunks(v), chunks(beta)
    gc = jnp.cumsum(chunks(g), axis=-1)
    incl = jnp.tril(jnp.ones((C, C), bool))
    strict = jnp.tril(jnp.ones((C, C), bool), -1)
    decay = jnp.exp(jnp.where(incl, gc[..., :, None] - gc[..., None, :], -jnp.inf))
    kb = kc * bc[..., None]
    lower = jnp.where(strict, jnp.einsum('bnhid,bnhjd->bnhij', kb, kc) * decay, 0.0)
    eye = jnp.eye(C, dtype=F32)
    t_inv = lax.linalg.triangular_solve(eye + lower, jnp.broadcast_to(eye, lower.shape), left_side=True, lower=True, unit_diagonal=True)
    u = t_inv @ (vc * bc[..., None])
    w = t_inv @ (kb * jnp.exp(gc)[..., None])
    qk = jnp.where(incl, jnp.einsum('bnhid,bnhjd->bnhij', qc, kc) * decay, 0.0)
    q_dec = qc * jnp.exp(gc)[..., None]
    k_dec = kc * jnp.exp(gc[..., -1:] - gc)[..., None]
    g_last = jnp.exp(gc[..., -1])

    def step(s, xs):
        u_i, w_i, qk_i, qd_i, kd_i, gl_i = xs
        v_new = u_i - jnp.einsum('bhcd,bhde->bhce', w_i, s)
        o_i = jnp.einsum('bhcd,bhde->bhce', qd_i, s) + jnp.einsum('bhcj,bhje->bhce', qk_i, v_new)
        s = s * gl_i[..., None, None] + jnp.einsum('bhcd,bhce->bhde', kd_i, v_new)
        return s, o_i
    xs = tuple(jnp.swapaxes(t, 0, 1) for t in (u, w, qk, q_dec, k_dec, g_last))
    s_fin, o = lax.scan(step, s0, xs)
    o = jnp.swapaxes(jnp.swapaxes(o, 0, 1), 2, 3).reshape(B, T, H, -1)
    return o, s_fin


def gated_deltanet(lat, ctx, conv_w, a_log, dt_bias, norm_g):
    def prep(qkv, a, b):
        B, T, _ = qkv.shape
        qkv = jax.nn.silu(short_conv(qkv, conv_w)).astype(F32)
        q, k, v = [split_heads(t) for t in jnp.split(qkv, 3, axis=-1)]
        q = l2norm(q) * HEAD_DIM ** -0.5
        k = l2norm(k)
        a = a.astype(F32).reshape(B, T, 2, GROUP_HEADS)
        b = b.astype(F32).reshape(B, T, 2, GROUP_HEADS)
        g = -jnp.exp(a_log.astype(F32)) * jax.nn.softplus(a + dt_bias.astype(F32))
        return q, k, v, g, jax.nn.sigmoid(b)
    ql, kl, vl, gl, bl = prep(lat[0], lat[1], lat[2])
    qc, kc, vc, gc, bc = prep(ctx[0], ctx[1], ctx[2])
    s0 = jnp.zeros((ql.shape[0], GROUP_HEADS, HEAD_DIM, HEAD_DIM), F32)
    flip = lambda t: jnp.flip(t, axis=1)
    oc_f, sc_f = chunk_gated_delta(qc, kc, vc, gc[:, :, 0], bc[:, :, 0], s0)
    ol_f, _ = chunk_gated_delta(ql, kl, vl, gl[:, :, 0], bl[:, :, 0], sc_f)
    oc_b, sc_b = chunk_gated_delta(flip(qc), flip(kc), flip(vc), flip(gc[:, :, 1]), flip(bc[:, :, 1]), s0)
    ol_b, _ = chunk_gated_delta(flip(ql), flip(kl), flip(vl), flip(gl[:, :, 1]), flip(bl[:, :, 1]), sc_b)

    def finish(o, gate):
        y = rmsnorm(o, norm_g) * jax.nn.silu(split_heads(gate.astype(F32)))
        return y.reshape(gate.shape).astype(gate.dtype)
    return finish(ol_f + flip(ol_b), lat[3]), finish(oc_f + flip(oc_b), ctx[3])


def diff_core(q, k, v, lam):
    s = jnp.einsum('bqhmd,bkhmd->bhmqk', q, k).astype(F32) * q.shape[-1] ** -0.5
    p = jax.nn.softmax(s, axis=-1)
    a = p[:, :, 0] - lam * p[:, :, 1]
    return jnp.einsum('bhqk,bkhd->bqhd', a, v.astype(F32))


def fourier_mix(u, w):
    B, T, _ = u.shape
    uf = u.astype(F32).reshape(B, T, GROUP_HEADS, FT_DIM)
    y = jnp.fft.fft2(uf, axes=(1, 3), norm='ortho').real.reshape(B, T, GROUP_W)
    return (y @ w.astype(F32)).astype(u.dtype)


def expert_choice_ffn(h, w_router, w_gate, w_up, w_down):
    B, N, D = h.shape
    cap = EC_FACTOR * N // N_EXPERTS
    aff = jax.nn.softmax((h @ w_router).astype(F32), axis=-1)
    gate, idx = lax.top_k(jnp.swapaxes(aff, 1, 2), cap)
    xg = jax.vmap(lambda hb, ib: hb[ib])(h, idx)
    hid = jax.nn.silu(jnp.einsum('becd,edf->becf', xg, w_gate)) * jnp.einsum('becd,edf->becf', xg, w_up)
    y = jnp.einsum('becf,efd->becd', hid, w_down) * gate[..., None].astype(h.dtype)
    return jax.vmap(lambda yb, ib: jnp.zeros((N, D), yb.dtype).at[ib.reshape(-1)].add(yb.reshape(-1, D)))(y, idx)


def token_mixers(h_lat, h_ctx, w_in, rpb, conv_w, a_log, dt_bias, dn_g, df_lam, lam_init, df_g, ft_w, cos, sin, ctx_out):
    B, S, _ = h_lat.shape
    L = h_ctx.shape[1]
    (na_q, na_k, na_v, dn_qkv, dn_a, dn_b, dn_gate, df_q, df_k, df_v, ft_u) = split_proj(h_lat @ w_in)
    (cna_q, cna_k, cna_v, cdn_qkv, cdn_a, cdn_b, cdn_gate, cdf_q, cdf_k, cdf_v, cft_u) = split_proj(h_ctx @ w_in)
    diff_heads = lambda t: t.reshape(*t.shape[:-1], GROUP_HEADS, 2, DIFF_DIM)
    dt = h_lat.dtype
    o_na = neighbourhood_attention(split_heads(na_q), split_heads(na_k), split_heads(na_v), split_heads(cna_k), split_heads(cna_v), rpb)
    o_dn, c_dn = gated_deltanet((dn_qkv, dn_a, dn_b, dn_gate), (cdn_qkv, cdn_a, cdn_b, cdn_gate), conv_w, a_log, dt_bias, dn_g)
    lq1, lk1, lq2, lk2 = df_lam.astype(F32)
    lam = jnp.exp(jnp.sum(lq1 * lk1)) - jnp.exp(jnp.sum(lq2 * lk2)) + lam_init
    q_d = apply_rope(diff_heads(df_q), cos, sin)
    k_d = apply_rope(diff_heads(df_k), cos, sin)
    kc_d, vc_d = diff_heads(cdf_k), split_heads(cdf_v)
    k_all = jnp.concatenate([kc_d, k_d], axis=1)
    v_all = jnp.concatenate([vc_d, split_heads(df_v)], axis=1)
    q_blocks = jnp.swapaxes(q_d.reshape(B, S // Q_BLOCK, Q_BLOCK, GROUP_HEADS, 2, DIFF_DIM), 0, 1)
    o_df = lax.map(lambda qb: diff_core(qb, k_all, v_all, lam), q_blocks)
    o_df = jnp.swapaxes(o_df, 0, 1).reshape(B, S, GROUP_HEADS, HEAD_DIM)
    diff_out = lambda o: rmsnorm(o, df_g) * (1.0 - lam_init)
    o_ft = fourier_mix(ft_u, ft_w)
    mix_lat = jnp.concatenate([o_na.reshape(B, S, GROUP_W).astype(dt), o_dn.astype(dt), diff_out(o_df).reshape(B, S, GROUP_W).astype(dt), o_ft.astype(dt)], axis=-1)
    if not ctx_out:
        return mix_lat, None
    c_na = softmax_attention(split_heads(cna_q), split_heads(cna_k), split_heads(cna_v))
    c_df = diff_out(diff_core(diff_heads(cdf_q), kc_d, vc_d, lam))
    c_ft = fourier_mix(cft_u, ft_w)
    mix_ctx = jnp.concatenate([c_na.reshape(B, L, GROUP_W).astype(dt), c_dn.astype(dt), c_df.reshape(B, L, GROUP_W).astype(dt), c_ft.astype(dt)], axis=-1)
    return mix_lat, mix_ctx


def setup_inputs(seed: int = 0) -> dict:
    key = jax.random.key(seed)
    ks = jax.random.split(key, 24)
    nrm = lambda k, shape, s: jax.random.normal(k, shape, F32) * s
    H = GROUP_HEADS
    a_init = jax.random.uniform(ks[9], (DEPTH, 2, H), F32, 1.0, 16.0)
    dt_init = jnp.exp(jax.random.uniform(ks[10], (DEPTH, 2, H), F32, math.log(1e-3), math.log(1e-1)))
    return {
        'x': nrm(ks[0], (BATCH, SEQ, D_MODEL), 1.0),
        'c': nrm(ks[1], (BATCH, D_MODEL), 1.0),
        'ctx': nrm(ks[2], (BATCH, CTX_LEN, D_MODEL), 1.0),
        'c_ctx': nrm(ks[3], (D_MODEL,), 1.0),
        'w_mod': nrm(ks[4], (DEPTH, D_MODEL, 6 * D_MODEL), 0.5 * D_MODEL ** -0.5),
        'b_mod': nrm(ks[5], (DEPTH, 6 * D_MODEL), 0.01),
        'norm1_g': 1.0 + nrm(ks[6], (DEPTH, D_MODEL), 0.02),
        'w_in': nrm(ks[7], (DEPTH, D_MODEL, IN_W), D_MODEL ** -0.5),
        'na_rpb': nrm(ks[8], (DEPTH, H, 2 * NA_WIN_H - 1, 2 * NA_WIN_W - 1), 0.02),
        'dn_conv_w': nrm(ks[11], (DEPTH, DN_CONV_K, 3 * GROUP_W), DN_CONV_K ** -0.5),
        'dn_a_log': jnp.log(a_init),
        'dn_dt_bias': dt_init + jnp.log(-jnp.expm1(-dt_init)),
        'dn_norm_g': 1.0 + nrm(ks[12], (DEPTH, HEAD_DIM), 0.02),
        'df_lambda': nrm(ks[13], (DEPTH, 4, DIFF_DIM), 0.1),
        'df_norm_g': 1.0 + nrm(ks[14], (DEPTH, HEAD_DIM), 0.02),
        'ft_w': nrm(ks[15], (DEPTH, GROUP_W, GROUP_W), GROUP_W ** -0.5),
        'w_out': nrm(ks[16], (DEPTH, D_MODEL, D_MODEL), D_MODEL ** -0.5),
        'norm2_g': 1.0 + nrm(ks[17], (DEPTH, D_MODEL), 0.02),
        'w_router': nrm(ks[18], (DEPTH, D_MODEL, N_EXPERTS), D_MODEL ** -0.5),
        'w_gate': nrm(ks[19], (DEPTH, N_EXPERTS, D_MODEL, D_EXPERT), D_MODEL ** -0.5),
        'w_up': nrm(ks[20], (DEPTH, N_EXPERTS, D_MODEL, D_EXPERT), D_MODEL ** -0.5),
        'w_down': nrm(ks[21], (DEPTH, N_EXPERTS, D_EXPERT, D_MODEL), D_EXPERT ** -0.5),
        'final_norm_g': 1.0 + nrm(ks[22], (D_MODEL,), 0.02),
    }


def reference(x, c, ctx, c_ctx, w_mod, b_mod, norm1_g, w_in, na_rpb, dn_conv_w, dn_a_log, dn_dt_bias, dn_norm_g, df_lambda, df_norm_g, ft_w, w_out, norm2_g, w_router, w_gate, w_up, w_down, final_norm_g):
    S = x.shape[1]
    cos, sin = axial_rope(S, DIFF_DIM)
    cos, sin = cos[:, None, None, :], sin[:, None, None, :]
    s_lat = jax.nn.silu(c)
    s_ctx = jax.nn.silu(c_ctx)
    x_lat, x_ctx = x, ctx
    for l in range(DEPTH):
        ctx_out = l < DEPTH - 1
        lam_init = 0.8 - 0.6 * math.exp(-0.3 * l)
        m_lat = jnp.split((s_lat @ w_mod[l] + b_mod[l])[:, None, :], 6, axis=-1)
        m_ctx = jnp.split(s_ctx @ w_mod[l] + b_mod[l], 6, axis=-1)
        h_lat = modulate(rmsnorm(x_lat, norm1_g[l]), m_lat[0], m_lat[1])
        h_ctx = modulate(rmsnorm(x_ctx, norm1_g[l]), m_ctx[0], m_ctx[1])
        mix_lat, mix_ctx = token_mixers(h_lat, h_ctx, w_in[l], na_rpb[l], dn_conv_w[l], dn_a_log[l], dn_dt_bias[l], dn_norm_g[l], df_lambda[l], lam_init, df_norm_g[l], ft_w[l], cos, sin, ctx_out)
        x_lat = x_lat + m_lat[2] * (mix_lat @ w_out[l])
        h_lat = modulate(rmsnorm(x_lat, norm2_g[l]), m_lat[3], m_lat[4])
        x_lat = x_lat + m_lat[5] * expert_choice_ffn(h_lat, w_router[l], w_gate[l], w_up[l], w_down[l])
        if ctx_out:
            x_ctx = x_ctx + m_ctx[2] * (mix_ctx @ w_out[l])
            h_ctx = modulate(rmsnorm(x_ctx, norm2_g[l]), m_ctx[3], m_ctx[4])
            x_ctx = x_ctx + m_ctx[5] * expert_choice_ffn(h_ctx, w_router[l], w_gate[l], w_up[l], w_down[l])
    return rmsnorm(x_lat, final_norm_g)
```

```python
import math
import os
TR = int(os.environ.get('K_TR', '9'))
TRG = float(os.environ.get('K_TRG', '9'))
from contextlib import ExitStack

import numpy as np
import ml_dtypes
import concourse.bass as bass
import concourse.mybir as mybir
from concourse.bass_utils import run_bass_kernel_spmd

F32 = mybir.dt.float32
BF16 = mybir.dt.bfloat16
I32 = mybir.dt.int32
ALU = mybir.AluOpType
AF = mybir.ActivationFunctionType
AX = mybir.AxisListType

D = 1024
S = 4096
L = 256
T = S + L
NT = T // 128
DEPTH = 4
NE = 16
CAP_L = 512
CAP_C = 32
ESL = CAP_L + CAP_C
TRASH = NE * ESL
GW = 64
VW = 72
EPOCH = 30000
NDMASEM = 8


class Buf:
    __slots__ = ("t", "lw", "rd", "name", "excl", "pe_rg", "pe_last")

    def __init__(self, t, name=""):
        self.t = t
        self.lw = None
        self.rd = {}
        self.name = name
        self.excl = False
        self.pe_rg = None
        self.pe_last = None

    def __getitem__(self, k):
        return self.t[k]


class Op:
    __slots__ = ("eng", "fn", "deps", "kind", "idx", "signal", "sigval")

    def __init__(self, eng, fn, deps, kind, idx):
        self.eng, self.fn, self.deps, self.kind, self.idx = eng, fn, deps, kind, idx
        self.signal = False
        self.sigval = None


class Prog:
    ENGS = ("pe", "act", "dve", "pool", "sp")

    def __init__(self, nc):
        self.nc = nc
        self.es = ExitStack()
        self.ph = ExitStack()
        self.mid = ExitStack()
        self.bufs = []
        self.ops = {e: [] for e in self.ENGS}
        self.nsig = {e: 0 for e in self.ENGS}
        self.ndma = {e: 0 for e in self.ENGS}
        self.cmp_sems = {}
        self.dma_sems = {}
        self.bar_sems = {}
        self.nbar = 0
        self.n = 0
        self.ninst = 0
        self.handles = {"pe": nc.tensor, "act": nc.scalar, "dve": nc.vector, "pool": nc.gpsimd, "sp": nc.sync}

    def _reg(self, t, name):
        b = Buf(t, name)
        self.bufs.append(b)
        return b

    def sb(self, shape, dt, persist=False):
        self.n += 1
        name = f"sb{self.n}"
        st = self.mid if persist == "mid" else (self.es if persist else self.ph)
        return self._reg(st.enter_context(self.nc.sbuf_tensor(name, list(shape), dt)), name)

    def release_mid(self):
        self.mid.close()
        self.mid = ExitStack()

    def ps(self, shape, dt=F32):
        self.n += 1
        name = f"ps{self.n}"
        b = self._reg(self.es.enter_context(self.nc.psum_tensor(name, list(shape), dt)), name)
        b.excl = True
        return b

    def dram(self, name, shape, dt, kind="Internal"):
        return self._reg(self.nc.dram_tensor(name, list(shape), dt, kind=kind), name)

    def view(self, name=""):
        return self._reg(None, name)

    def op(self, eng, fn, R=(), W=(), kind="cmp", rg=None):
        lst = self.ops[eng]
        idx = len(lst)
        deps = set()
        W = list(W) + [b for b in R if b.excl]
        R = [b for b in R if not b.excl]
        extra = set()
        if eng == "pe":
            if rg is None:
                rg = frozenset((0, 1, 2, 3))
            for b in W:
                if b.excl:
                    if b.pe_rg is not None and b.pe_last is not None and not (b.pe_rg & rg):
                        extra.add(b.pe_last)
                    b.pe_rg = rg
                    b.pe_last = ("pe", idx)
        for b in R:
            if b.lw is not None:
                deps.add(b.lw)
        for b in W:
            if b.lw is not None:
                deps.add(b.lw)
            for e2, i2 in b.rd.items():
                deps.add((e2, i2))
        if eng == "pe":
            deps = {d for d in deps if d[0] != "pe"}
        deps |= extra
        o = Op(eng, fn, deps, kind, idx)
        lst.append(o)
        for b in R:
            b.rd[eng] = idx
        for b in W:
            b.lw = (eng, idx)
            b.rd = {}
        return o

    def dma(self, eng, out, in_, R=(), W=(), **kw):
        return self.op(eng, lambda e: e.dma_start(out=out, in_=in_, **kw), R=R, W=W, kind="dma")

    def _csem(self, e, ep):
        if (e, ep) not in self.cmp_sems:
            self.cmp_sems[(e, ep)] = self.es.enter_context(self.nc.semaphore(f"c_{e}_{ep}"))
        return self.cmp_sems[(e, ep)]

    def _dsem(self, e, j):
        if (e, j) not in self.dma_sems:
            self.dma_sems[(e, j)] = self.es.enter_context(self.nc.semaphore(f"d_{e}_{j}"))
        return self.dma_sems[(e, j)]

    def _bsem(self, e):
        if e not in self.bar_sems:
            self.bar_sems[e] = self.es.enter_context(self.nc.semaphore(f"b_{e}"))
        return self.bar_sems[e]

    def flush(self):
        nc = self.nc
        ops = self.ops
        for e in self.ENGS:
            for o in ops[e]:
                for (e2, i2) in o.deps:
                    ops[e2][i2].signal = True
            for o in reversed(ops[e]):
                if o.kind == "cmp":
                    o.signal = True
                    break
        for e in self.ENGS:
            for o in ops[e]:
                if o.kind == "dma":
                    o.sigval = ("dma", self.ndma[e])
                    self.ndma[e] += 1
                elif o.signal:
                    o.sigval = ("cmp", self.nsig[e])
                    self.nsig[e] += 1
        self.nbar += 1
        nbar = self.nbar

        def run(ename, eh):
            waited_cmp = {}
            waited_dma = set()
            last_cmp = None
            my_dmas = []
            for o in ops[ename]:
                best = {}
                for (e2, i2) in o.deps:
                    d = ops[e2][i2]
                    kind, n = d.sigval
                    if kind == "dma":
                        if (e2, n) in waited_dma:
                            continue
                        waited_dma.add((e2, n))
                        eh.wait_ge(self._dsem(e2, n % NDMASEM), 16 * (n // NDMASEM + 1))
                        self.ninst += 1
                    else:
                        if waited_cmp.get(e2, -1) >= n:
                            continue
                        if best.get(e2, -1) < n:
                            best[e2] = n
                for e2, n in best.items():
                    waited_cmp[e2] = n
                    eh.wait_ge(self._csem(e2, n // EPOCH), (n % EPOCH) + 1)
                    self.ninst += 1
                if o.kind == "dma":
                    n = o.sigval[1]
                    if n >= NDMASEM and (ename, n - NDMASEM) not in waited_dma:
                        eh.wait_ge(self._dsem(ename, n % NDMASEM), 16 * (n // NDMASEM))
                        waited_dma.add((ename, n - NDMASEM))
                    ins = o.fn(eh)
                    ins.then_inc(self._dsem(ename, n % NDMASEM), 16)
                    my_dmas.append(n)
                else:
                    ins = o.fn(eh)
                    if o.signal:
                        n = o.sigval[1]
                        ins.then_inc(self._csem(ename, n // EPOCH), 1)
                        last_cmp = n
                self.ninst += 1
            for n in my_dmas[-NDMASEM:]:
                if (ename, n) not in waited_dma:
                    eh.wait_ge(self._dsem(ename, n % NDMASEM), 16 * (n // NDMASEM + 1))
            if last_cmp is not None and waited_cmp.get(ename, -1) < last_cmp:
                eh.wait_ge(self._csem(ename, last_cmp // EPOCH), (last_cmp % EPOCH) + 1)
            eh.sem_inc(self._bsem(ename), 1)
            for e2 in self.ENGS:
                if e2 != ename:
                    eh.wait_ge(self._bsem(e2), nbar)

        with nc.Block() as block:
            @block.sync
            def _(e):
                run("sp", e)

            @block.scalar
            def _(e):
                run("act", e)

            @block.vector
            def _(e):
                run("dve", e)

            @block.tensor
            def _(e):
                run("pe", e)

            @block.gpsimd
            def _(e):
                run("pool", e)
        self.ops = {e: [] for e in self.ENGS}
        for b in self.bufs:
            b.lw = None
            b.rd = {}
            b.pe_rg = None
            b.pe_last = None
        self.ph.close()
        self.ph = ExitStack()

    def close(self):
        self.ph.close()
        self.es.close()


IN_OFF = {"naq": 0, "nak": 256, "nav": 512, "dn": 768, "dna": 1536, "dnb": 1544, "dng": 1552,
          "dfq": 1808, "dfk": 2064, "dfv": 2320, "ftu": 2576}


def _swap_cols(base):
    idx = []
    for hm in range(8):
        for f in range(32):
            idx.append(base + hm * 32 + (f ^ 1))
    return idx


def in_proj_perm():
    p = []
    p += list(range(IN_OFF["naq"], IN_OFF["naq"] + 256))
    p += list(range(IN_OFF["nak"], IN_OFF["nak"] + 256))
    p += list(range(IN_OFF["dn"], IN_OFF["dn"] + 768))
    p += list(range(IN_OFF["dfq"], IN_OFF["dfq"] + 256))
    p += _swap_cols(IN_OFF["dfq"])
    p += list(range(IN_OFF["dfk"], IN_OFF["dfk"] + 256))
    p += _swap_cols(IN_OFF["dfk"])
    p += list(range(IN_OFF["ftu"], IN_OFF["ftu"] + 256))
    assert len(p) == 2560
    p += list(range(IN_OFF["nav"], IN_OFF["nav"] + 256))
    p += list(range(IN_OFF["dfv"], IN_OFF["dfv"] + 256))
    p += list(range(IN_OFF["dng"], IN_OFF["dng"] + 256))
    p += list(range(IN_OFF["dna"], IN_OFF["dna"] + 16))
    assert len(p) == 3344
    return np.array(p)


NWIN = 3344
A_PLAIN = {0: 0, 1: 128, 2: 256, 3: 384, 4: 512, 5: 640, 6: 768, 7: 896, 8: 1024, 9: 1152, 18: 1792, 19: 1920}
A_ROPE = {10: (12, 1280), 11: (13, 1408), 14: (16, 1536), 15: (17, 1664)}


def const_tables():
    c = {}
    c["ident"] = np.eye(128, dtype=np.float32)
    t = np.arange(S)
    pos = np.stack([t // GW, t % GW], -1).astype(np.float32)
    inv = (10000.0 ** (-np.arange(8, dtype=np.float32) / 8)).astype(np.float32)
    ang = (pos[:, :, None] * inv).reshape(S, 16)
    cosf = np.repeat(np.cos(ang), 2, axis=1)
    sinf = np.repeat(np.sin(ang), 2, axis=1)
    sgn = np.tile(np.array([-1.0, 1.0], np.float32), 16)
    sinf = sinf * sgn
    cosT = np.ones((32, T), np.float32)
    sinT = np.zeros((32, T), np.float32)
    cosT[:, L:] = cosf.T
    sinT[:, L:] = sinf.T
    c["cosT"] = np.tile(cosT, (4, 1)).astype(np.float32)
    c["sinT"] = np.tile(sinT, (4, 1)).astype(np.float32)
    cq = np.arange(GW)
    c0 = np.clip(cq - 8, 0, GW - 16)
    inwin = (cq[None, :] >= c0[:, None]) & (cq[None, :] < c0[:, None] + 16)
    c["colwinT"] = np.tile(inwin.T.astype(np.float32), (2, 1))
    def dft(n, scale):
        k = np.arange(n)
        ph = (np.outer(k, k) % n).astype(np.float64) * (2 * np.pi / n)
        return (np.cos(ph) * scale), (np.sin(ph) * scale)
    c64, s64 = dft(64, 1.0 / 8)
    bdc = np.zeros((128, 128)); bds = np.zeros((128, 128))
    for i in range(2):
        bdc[i * 64:(i + 1) * 64, i * 64:(i + 1) * 64] = c64
        bds[i * 64:(i + 1) * 64, i * 64:(i + 1) * 64] = s64
    c["bdc"] = bdc.astype(np.float32)
    c["bds"] = bds.astype(np.float32)
    cS, sS = dft(S, 1.0 / 64)
    def tile_tab(m, n):
        nt = n // 128
        return np.ascontiguousarray(m.reshape(nt, 128, nt, 128).transpose(2, 1, 0, 3)).astype(ml_dtypes.bfloat16)
    c["dftc"] = tile_tab(cS, S)
    c["dfts"] = tile_tab(sS, S)
    cL, sL = dft(L, 1.0 / 16)
    c["dftc_c"] = tile_tab(cL, L)
    c["dfts_c"] = tile_tab(sL, L)
    c["tri"] = np.triu(np.ones((128, 128), np.float32))
    c["ones"] = np.ones((128, 128), np.float32)
    base = np.zeros((128, NT, NE), np.float32)
    for e in range(NE):
        base[:, :2, e] = e * ESL + CAP_L
        base[:, 2:, e] = e * ESL
    c["slotbase"] = base.reshape(128, NT * NE) - TRASH
    ii = np.arange(128)
    same = (ii[:, None] // 64) == (ii[None, :] // 64)
    Uf = (same & (ii[:, None] <= ii[None, :])).astype(np.float32)
    Ub = (same & (ii[:, None] >= ii[None, :])).astype(np.float32)
    NEG = -30000.0
    g = {}
    g["U"] = np.stack([Uf, Ub])
    g["negU"] = -g["U"]
    nml_f = np.where(same & (ii[:, None] > ii[None, :]), 0.0, NEG)
    nml_b = np.where(same & (ii[:, None] < ii[None, :]), 0.0, NEG)
    nmq_f = np.where(same & (ii[None, :] >= ii[:, None]), 0.0, NEG)
    nmq_b = np.where(same & (ii[None, :] <= ii[:, None]), 0.0, NEG)
    g["NML"] = np.stack([nml_f, nml_b])
    g["NMQ"] = np.stack([nmq_f, nmq_b])
    ind = np.zeros((2, 128, 64), np.float32)
    ind[0, :64, :] = 1.0
    ind[1, 64:, :] = 1.0
    c["gdn_U"] = g["U"].astype(np.float32)
    c["gdn_negU"] = g["negU"].astype(np.float32)
    c["gdn_NML"] = g["NML"].astype(np.float32)
    c["gdn_NMQ"] = g["NMQ"].astype(np.float32)
    c["gdn_ind"] = ind
    c["gdn_ob"] = same.astype(np.float32)
    return c


class Builder:
    def __init__(self, nlayers=DEPTH, dbg=False, stages=None):
        self.nlayers = nlayers
        self.dbg = dbg
        self.stages = stages
        self.nc = bass.Bass("TRN2", target_bir_lowering=False)
        self.P = Prog(self.nc)
        self.inputs = {}

    def want(self, s):
        return self.stages is None or s in self.stages

    def din(self, name, shape, dt=F32):
        b = self.P.dram(name, shape, dt, kind="ExternalInput")
        self.inputs[name] = b
        return b

    def dscr(self, name, shape, dt, out=False):
        return self.P.dram(name, shape, dt, kind="ExternalOutput" if (out or self.dbg) else "Internal")

    def declare(self):
        P = self.P
        nl = DEPTH
        self.xc = self.din("xc", [T, D])
        self.cc = self.din("cc", [2, D])
        self.w_mod = self.din("w_mod", [nl, D, 6 * D])
        self.b_mod = self.din("b_mod", [nl, 6 * D])
        self.norm1_g = self.din("norm1_g", [nl, D])
        self.norm2_g = self.din("norm2_g", [nl, D])
        self.w_in = self.din("w_in_p", [nl, D, NWIN])
        self.rpbpad = self.din("rpbpad", [nl, 4, 15, 128])
        self.ft_w = self.din("ft_w", [nl, 256, 256])
        self.w_out = self.din("w_out", [nl, D, D])
        self.w_router = self.din("w_router", [nl, D, NE])
        if self.want("moe"):
            self.w_gate = self.din("w_gate", [nl, NE, D, 2 * D])
            self.w_up = self.din("w_up", [nl, NE, D, 2 * D])
            self.w_down = self.din("w_down", [nl, NE, 2 * D, D])
        self.final_g = self.din("final_norm_g", [D])
        self.df_lambda = self.din("df_lambda", [nl, 128])
        self.df_norm_g = self.din("df_norm_g", [nl, 64])
        self.dn_norm_g = self.din("dn_norm_g", [nl, 64])
        self.dn_conv_w = self.din("dn_conv_w", [nl, 5, 768])
        self.dn_a_log = self.din("dn_a_log", [nl, 8])
        self.dn_dt_bias = self.din("dn_dt_bias", [nl, 8])
        self.c_ident = self.din("ident", [128, 128])
        self.c_cosT = self.din("cosT", [128, T])
        self.c_sinT = self.din("sinT", [128, T])
        self.c_colwinT = self.din("colwinT", [128, 64])
        self.c_bdc = self.din("bdc", [128, 128])
        self.c_bds = self.din("bds", [128, 128])
        if self.want("fft"):
            self.c_dftc = self.din("dftc", [32, 128, 32, 128], BF16)
            self.c_dfts = self.din("dfts", [32, 128, 32, 128], BF16)
        self.c_dftc_c = self.din("dftc_c", [2, 128, 2, 128], BF16)
        self.c_dfts_c = self.din("dfts_c", [2, 128, 2, 128], BF16)
        self.c_tri = self.din("tri", [128, 128])
        self.c_ones = self.din("ones", [128, 128])
        self.c_slotbase = self.din("slotbase", [128, NT * NE])
        self.c_gU = self.din("gdn_U", [2, 128, 128])
        self.c_gnegU = self.din("gdn_negU", [2, 128, 128])
        self.c_gNML = self.din("gdn_NML", [2, 128, 128])
        self.c_gNMQ = self.din("gdn_NMQ", [2, 128, 128])
        self.c_gind = self.din("gdn_ind", [2, 128, 64])
        self.c_gob = self.din("gdn_ob", [128, 128])
        self.out = P.dram("out", [S, D], F32, kind="ExternalOutput")
        self.xres = self.dscr("xres", [T, D], F32)
        self.projT = self.dscr("projT", [2048, T], BF16)
        self.navd = self.dscr("navd", [T, 4 * VW], BF16)
        self.dfvd = self.dscr("dfvd", [T, 4 * VW], BF16)
        self.gated = self.dscr("gated", [T, 256], BF16)
        self.abd = self.dscr("abd", [T, 16], F32)
        self.mix = self.dscr("mix", [T, D], BF16)
        self.h2d = self.dscr("h2d", [T, D], BF16)
        self.xg = self.dscr("xg", [TRASH + 128, D], BF16)
        self.ybuf = self.dscr("ybuf", [TRASH + 128, D], F32)
        self.rpbz = self.dscr("rpbz", [60 * 64 * 129 + 256], F32)
        self.ident = P.sb([128, 128], BF16, persist=True)
        self.identf = P.sb([128, 128], F32, persist=True)
        self.onesf = P.sb([128, 128], F32, persist=True)
        self.trif = P.sb([128, 128], F32, persist=True)
        self.mT = P.sb([128, 48, 2], F32, persist=True)
        self.gs1T = P.sb([128, 8, 2], F32, persist=True)
        self.g1T = P.sb([128, 8], F32, persist=True)
        self.g2T = P.sb([128, 8], F32, persist=True)
        self.aff = P.sb([128, NT, NE], F32, persist=True)
        self.desti = P.sb([128, NT, NE], I32, persist=True)
        self.gatev = P.sb([128, NT, NE], F32, persist=True)
        self.pb = [P.ps([128, 512], F32) for _ in range(8)]

    def stage_init(self):
        P = self.P
        P.dma("pool", self.ident[:], self.c_ident[:], R=[self.c_ident], W=[self.ident])
        P.dma("sp", self.identf[:], self.c_ident[:], R=[self.c_ident], W=[self.identf])
        P.dma("sp", self.onesf[:], self.c_ones[:], R=[self.c_ones], W=[self.onesf])
        P.dma("sp", self.trif[:], self.c_tri[:], R=[self.c_tri], W=[self.trif])
        for i in range(2):
            r0 = i * (T // 2)
            P.dma("sp", self.xres[r0:r0 + T // 2, :], self.xc[r0:r0 + T // 2, :], R=[self.xc], W=[self.xres])
        z = P.sb([128, D], F32)
        P.op("dve", lambda e: e.memset(z[:], 0.0), W=[z])
        P.dma("sp", self.ybuf[TRASH:TRASH + 128, :], z[:], R=[z], W=[self.ybuf])
        P.flush()

    def stage_mod(self, l):
        P = self.P
        pb = self.pb
        sT = P.sb([128, 8, 2], F32)
        for r in range(2):
            P.dma("sp", sT[:, :, r], self.cc.t.ap()[r].rearrange("(c p) -> p c", p=128), R=[self.cc], W=[sT],
                  allow_slow_non_contiguous=True)
        P.op("act", lambda e: e.activation(sT[:], sT[:], AF.Silu), R=[sT], W=[sT])
        bT = P.sb([128, 48], F32)
        P.dma("sp", bT[:], self.b_mod.t.ap()[l].rearrange("(c p) -> p c", p=128), R=[self.b_mod], W=[bT],
              allow_slow_non_contiguous=True)
        P.dma("sp", self.g1T[:], self.norm1_g.t.ap()[l].rearrange("(c p) -> p c", p=128), R=[self.norm1_g],
              W=[self.g1T], allow_slow_non_contiguous=True)
        P.dma("sp", self.g2T[:], self.norm2_g.t.ap()[l].rearrange("(c p) -> p c", p=128), R=[self.norm2_g],
              W=[self.g2T], allow_slow_non_contiguous=True)
        wt = [P.sb([128, 8, 512], F32) for _ in range(2)]
        acc = pb[0]
        for n in range(12):
            w = wt[n % 2]
            P.dma("sp", w[:], self.w_mod.t.ap()[l, :, n * 512:(n + 1) * 512].rearrange("(k p) n -> p k n", p=128),
                  R=[self.w_mod], W=[w])
            for j in range(4):
                col = n * 4 + j
                for k in range(8):
                    P.op("pe", lambda e, w=w, j=j, k=k, col=col: e.matmul(
                        acc[:, col * 2:col * 2 + 2], w[:, k, j * 128:(j + 1) * 128], sT[:, k, :],
                        start=(k == 0), stop=(k == 7)), R=[w, sT], W=[acc])
        P.op("dve", lambda e: e.tensor_tensor(
            self.mT[:], acc[:, 0:96].rearrange("p (c r) -> p c r", r=2),
            bT[:].unsqueeze(2).to_broadcast([128, 48, 2]), ALU.add), R=[acc, bT], W=[self.mT])
        P.op("dve", lambda e: e.tensor_scalar(self.gs1T[:], self.mT[:, 8:16, :], 1.0, None, ALU.add),
             R=[self.mT], W=[self.gs1T])
        P.op("dve", lambda e: e.tensor_tensor(self.gs1T[:], self.gs1T[:],
                                              self.g1T[:].unsqueeze(2).to_broadcast([128, 8, 2]), ALU.mult),
             R=[self.gs1T, self.g1T], W=[self.gs1T])
        if self.dbg:
            dm = self.dscr(f"dbg_mT{l}", [128, 96], F32)
            P.dma("sp", dm[:], self.mT[:].rearrange("p c r -> p (c r)"), R=[self.mT], W=[dm])
        P.flush()

    def bcast_vec(self, dst, srcT_fn, R):
        P = self.P
        dg = P.sb([128, 128], F32)
        for c in range(8):
            bank = self.pb[4 + (c % 2)]
            P.op("dve", lambda e, c=c: e.tensor_scalar(dg[:], self.identf[:], srcT_fn(c), None, ALU.mult),
                 R=[self.identf] + R, W=[dg])
            P.op("pe", lambda e, bank=bank: e.matmul(bank[:, 0:128], self.onesf[:], dg[:], start=True, stop=True),
                 R=[dg, self.onesf], W=[bank])
            P.op("act", lambda e, c=c, bank=bank: e.copy(dst[:, c * 128:(c + 1) * 128], bank[:, 0:128]),
                 R=[bank], W=[dst])

    def stage_inproj(self, l):
        P = self.P
        pb = self.pb
        win = P.sb([128, 8, NWIN], BF16)
        for k in range(8):
            P.dma("pool", win[:, k, :], self.w_in.t.ap()[l, k * 128:(k + 1) * 128, :], R=[self.w_in], W=[win])
        xt = [P.sb([128, D], F32) for _ in range(4)]
        xn = [P.sb([128, D], BF16) for _ in range(4)]
        junk = P.sb([128, D], BF16)
        ss = [P.sb([128, 2], F32) for _ in range(4)]
        hT = P.sb([128, 8, 512], BF16)
        cosb = P.sb([128, 512], F32)
        sinb = P.sb([128, 512], F32)
        stg = [P.sb([128, 512], BF16) for _ in range(4)]
        r1 = [P.sb([128, 512], F32) for _ in range(2)]
        r2 = [P.sb([128, 512], F32) for _ in range(2)]
        vst = [P.sb([128, 4, VW], BF16) for _ in range(2)]
        fst = [P.sb([128, 4, VW], BF16) for _ in range(2)]
        gst = [P.sb([128, 256], BF16) for _ in range(2)]
        ast = [P.sb([128, 16], F32) for _ in range(2)]
        for b_ in vst + fst:
            P.op("dve", lambda e, b_=b_: e.memset(b_[:], 1.0), W=[b_])
        blocks = [(0, 2, 1)] + [(2 + 4 * i, 4, 0) for i in range(8)]
        nst = 0
        for (t0, nt, s) in blocks:
            ntok = nt * 128
            c0 = t0 * 128
            P.dma("sp", cosb[:, :ntok], self.c_cosT[:, c0:c0 + ntok], R=[self.c_cosT], W=[cosb])
            P.dma("sp", sinb[:, :ntok], self.c_sinT[:, c0:c0 + ntok], R=[self.c_sinT], W=[sinb])
            for i in range(nt):
                t = t0 + i
                P.dma("sp", xt[i][:], self.xres[t * 128:(t + 1) * 128, :], R=[self.xres], W=[xt[i]])
                P.op("dve", lambda e, i=i: e.memset(ss[i][:], 0.0), W=[ss[i]])
                P.op("act", lambda e, i=i: e.activation(junk[:], xt[i][:], AF.Square, accum_out=ss[i][:, 0:1]),
                     R=[xt[i], ss[i]], W=[junk, ss[i]])
                P.op("act", lambda e, i=i: e.activation(ss[i][:, 1:2], ss[i][:, 0:1], AF.Sqrt, bias=1e-6, scale=1.0 / D),
                     R=[ss[i]], W=[ss[i]])
                P.op("dve", lambda e, i=i: e.reciprocal(ss[i][:, 1:2], ss[i][:, 1:2]), R=[ss[i]], W=[ss[i]])
                P.op("act", lambda e, i=i: e.activation(xn[i][:], xt[i][:], AF.Copy, scale=ss[i][:, 1:2]),
                     R=[xt[i], ss[i]], W=[xn[i]])
            if TR < 2:
                continue
            for c in range(8):
                bank = pb[c % 2]
                pT = bank[:].bitcast(BF16)
                for i in range(nt):
                    P.op("pe", lambda e, i=i, c=c, pT=pT: e.transpose(
                        pT[:, i * 128:(i + 1) * 128], xn[i][:, c * 128:(c + 1) * 128], self.ident[:]),
                        R=[xn[i], self.ident], W=[bank])
                P.op("act", lambda e, c=c, pT=pT, s=s, ntok=ntok: e.activation(
                    hT[:, c, :ntok], pT[:, :ntok], AF.Identity, scale=self.gs1T[:, c, s:s + 1],
                    bias=self.mT[:, c, s:s + 1]), R=[bank, self.gs1T, self.mT], W=[hT])
            if TR < 3:
                continue
            def mm_chunk(j, bank):
                for k in range(8):
                    P.op("pe", lambda e, j=j, k=k, bank=bank: e.matmul(
                        bank[:, :ntok], win[:, k, j * 128:(j + 1) * 128], hT[:, k, :ntok],
                        start=(k == 0), stop=(k == 7)), R=[win, hT], W=[bank])
            for j in range(20):
                if j in A_PLAIN:
                    bank = pb[2 + (j % 2)]
                    mm_chunk(j, bank)
                    st = stg[nst % 4]
                    nst += 1
                    if j % 2 == 0:
                        P.op("act", lambda e, st=st, bank=bank: e.copy(st[:, :ntok], bank[:, :ntok]), R=[bank], W=[st])
                    else:
                        P.op("dve", lambda e, st=st, bank=bank: e.tensor_copy(st[:, :ntok], bank[:, :ntok]),
                             R=[bank], W=[st])
                    r0 = A_PLAIN[j]
                    P.dma("pool", self.projT[r0:r0 + 128, c0:c0 + ntok], st[:, :ntok], R=[st], W=[self.projT])
                elif j in A_ROPE:
                    j2, r0 = A_ROPE[j]
                    b1, b2 = pb[4 + (j % 2) * 2], pb[5 + (j % 2) * 2]
                    mm_chunk(j, b1)
                    mm_chunk(j2, b2)
                    a1, a2 = r1[j % 2], r2[j % 2]
                    P.op("dve", lambda e, a1=a1, b1=b1: e.tensor_tensor(a1[:, :ntok], b1[:, :ntok], cosb[:, :ntok], ALU.mult),
                         R=[b1, cosb], W=[a1])
                    P.op("dve", lambda e, a2=a2, b2=b2: e.tensor_tensor(a2[:, :ntok], b2[:, :ntok], sinb[:, :ntok], ALU.mult),
                         R=[b2, sinb], W=[a2])
                    st = stg[nst % 4]
                    nst += 1
                    P.op("pool", lambda e, st=st, a1=a1, a2=a2: e.tensor_tensor(st[:, :ntok], a1[:, :ntok], a2[:, :ntok], ALU.add),
                         R=[a1, a2], W=[st])
                    P.dma("pool", self.projT[r0:r0 + 128, c0:c0 + ntok], st[:, :ntok], R=[st], W=[self.projT])
            if TR < 4:
                continue
            for i in range(nt):
                t = t0 + i
                b1, b2 = pb[2 + (i % 2)], pb[4 + (i % 2)]
                for k in range(8):
                    P.op("pe", lambda e, i=i, k=k, b1=b1: e.matmul(
                        b1[:, :], hT[:, k, i * 128:(i + 1) * 128], win[:, k, 2560:3072],
                        start=(k == 0), stop=(k == 7)), R=[win, hT], W=[b1])
                for k in range(8):
                    P.op("pe", lambda e, i=i, k=k, b2=b2: e.matmul(
                        b2[:, :272], hT[:, k, i * 128:(i + 1) * 128], win[:, k, 3072:3344],
                        start=(k == 0), stop=(k == 7)), R=[win, hT], W=[b2])
                v, f, g, a = vst[i % 2], fst[i % 2], gst[i % 2], ast[i % 2]
                P.op("act", lambda e, v=v, b1=b1: e.copy(v[:, :, 0:64], b1[:, 0:256].rearrange("p (h d) -> p h d", d=64)),
                     R=[b1], W=[v])
                P.op("dve", lambda e, f=f, b1=b1: e.tensor_copy(f[:, :, 0:64], b1[:, 256:512].rearrange("p (h d) -> p h d", d=64)),
                     R=[b1], W=[f])
                P.op("act", lambda e, g=g, b2=b2: e.copy(g[:], b2[:, 0:256]), R=[b2], W=[g])
                P.op("dve", lambda e, a=a, b2=b2: e.tensor_copy(a[:], b2[:, 256:272]), R=[b2], W=[a])
                rs = slice(t * 128, (t + 1) * 128)
                if TR < 5:
                    continue
                P.dma("pool", self.navd[rs, :], v[:].rearrange("p h d -> p (h d)"), R=[v], W=[self.navd])
                P.dma("pool", self.dfvd[rs, :], f[:].rearrange("p h d -> p (h d)"), R=[f], W=[self.dfvd])
                P.dma("pool", self.gated[rs, :], g[:], R=[g], W=[self.gated])
                P.dma("pool", self.abd[rs, :], a[:], R=[a], W=[self.abd])
        P.flush()


    def mm(self, out, lhsT, rhs, start, stop, R, W, **kw):
        bp = lhsT.base_partition()
        kk = lhsT.shape[0]
        rg = frozenset(range(bp // 32, (bp + kk - 1) // 32 + 1))
        self.P.op("pe", lambda e: e.matmul(out, lhsT, rhs, start=start, stop=stop, **kw), R=R, W=W, rg=rg)

    def act(self, out, in_, func, R, W, **kw):
        self.P.op("act", lambda e: e.activation(out, in_, func, **kw), R=R, W=W)

    def tt(self, eng, out, in0, in1, op, R, W):
        self.P.op(eng, lambda e: e.tensor_tensor(out, in0, in1, op), R=R, W=W)

    def ts(self, eng, out, in0, s1, s2, op0, op1, R, W):
        if op1 is None:
            self.P.op(eng, lambda e: e.tensor_scalar(out, in0, s1, s2, op0), R=R, W=W)
        else:
            self.P.op(eng, lambda e: e.tensor_scalar(out, in0, s1, s2, op0, op1), R=R, W=W)

    def stt(self, eng, out, in0, scalar, in1, op0, op1, R, W):
        self.P.op(eng, lambda e: e.scalar_tensor_tensor(out, in0, scalar, in1, op0, op1), R=R, W=W)

    def cp(self, eng, out, in_, R, W):
        if eng == "act":
            self.P.op("act", lambda e: e.copy(out, in_), R=R, W=W)
        else:
            self.P.op(eng, lambda e: e.tensor_copy(out, in_), R=R, W=W)

    def rsqrt_mean(self, out, in_, n, eps, R, W):
        self.act(out, in_, AF.Sqrt, R=R, W=W, bias=eps, scale=1.0 / n)
        self.P.op("dve", lambda e: e.reciprocal(out, out), R=W, W=W)

    def stage_na(self, l, ctx_out):
        P = self.P
        pb = self.pb
        zdst = self.rpbz.t.ap()[0:60 * 8256].rearrange("(a b c) -> a b c", b=64, c=129)[:, :, 0:128]
        zsrc = self.rpbpad.t.ap()[l].rearrange("h j i -> (h j) i").unsqueeze(1).to_broadcast([60, 64, 128])
        P.dma("sp", zdst, zsrc, R=[self.rpbpad], W=[self.rpbz])
        zv = self.rpbz.t.ap()[63:63 + 60 * 8256].rearrange("(a r) -> a r", r=8256)[:, 0:8192] \
            .rearrange("a (k q) -> k a q", q=128)[:, :, 0:64]
        bank = P.sb([128, 60, 64], F32)
        colw = P.sb([128, 64], F32)
        P.dma("sp", colw[:], self.c_colwinT[:], R=[self.c_colwinT], W=[colw])
        P.dma("sp", bank[0:64], zv, R=[self.rpbz], W=[bank])
        P.dma("sp", bank[64:128], zv, R=[self.rpbz], W=[bank])
        self.act(bank[:], bank[:], AF.Exp, R=[bank], W=[bank])
        self.tt("dve", bank[:], bank[:], colw[:].unsqueeze(1).to_broadcast([128, 60, 64]), ALU.mult,
                R=[bank, colw], W=[bank])
        qT = [P.sb([128, T], BF16) for _ in range(2)]
        kT = [P.sb([128, T], BF16) for _ in range(2)]
        for hp in range(2):
            P.dma("sp", qT[hp][:], self.projT[hp * 128:(hp + 1) * 128, :], R=[self.projT], W=[qT[hp]])
            P.dma("sp", kT[hp][:], self.projT[256 + hp * 128:256 + (hp + 1) * 128, :], R=[self.projT], W=[kT[hp]])
        vsb = P.sb([128, NT, 4 * VW], BF16)
        P.dma("sp", vsb[:], self.navd.t.ap().rearrange("(n p) c -> p n c", p=128), R=[self.navd], W=[vsb])
        E = [P.sb([128, 8, 128], F32) for _ in range(2)]
        PT = [P.sb([128, 8, 128], BF16) for _ in range(2)]
        mst = [P.sb([128, 256], BF16) for _ in range(2)]
        rd = [P.sb([128, 1], F32) for _ in range(2)]

        def start_row(r):
            return min(max(r - 4, 0), 56)

        n = 0
        pend_tail = None
        qtiles = ([(-2, 0), (-1, 1)] if ctx_out else []) + [(m, 2 + m) for m in range(32)]
        for (m, qt) in qtiles:
            ms = mst[qt % 2]
            for h in range(4):
                hp, hl = h // 2, h % 2
                psl = slice(hl * 64, hl * 64 + 64)
                if m < 0:
                    chunks = [0, 1]
                else:
                    c_lo = start_row(2 * m) // 2
                    c_hi = (start_row(2 * m + 1) + 7) // 2
                    chunks = [0, 1] + [2 + c for c in range(c_lo, c_hi + 1)]
                nch = len(chunks)
                bA, bB = pb[2 * (n % 2)], pb[2 * (n % 2) + 1]
                Eb, PTb = E[n % 2], PT[n % 2]
                for ci, kt in enumerate(chunks):
                    bk = bA if ci < 4 else bB
                    off = (ci % 4) * 128
                    self.mm(bk[:, off:off + 128], kT[hp][psl, kt * 128:(kt + 1) * 128], qT[hp][psl, qt * 128:(qt + 1) * 128],
                            True, True, R=[kT[hp], qT[hp]], W=[bk])
                self.act(PTb[:, 0:2, :], bA[:, 0:256].rearrange("p (c q) -> p c q", q=128), AF.Exp,
                         R=[bA], W=[PTb], scale=0.125)
                if nch > 2:
                    na = min(nch, 4) - 2
                    self.act(Eb[:, 2:2 + na, :], bA[:, 256:256 + na * 128].rearrange("p (c q) -> p c q", q=128), AF.Exp,
                             R=[bA], W=[Eb], scale=0.125)
                if nch > 4:
                    nb = nch - 4
                    self.act(Eb[:, 4:4 + nb, :], bB[:, 0:nb * 128].rearrange("p (c q) -> p c q", q=128), AF.Exp,
                             R=[bB], W=[Eb], scale=0.125)
                for ci in range(2, nch):
                    c = chunks[ci] - 2
                    for kr in range(2):
                        krow = 2 * c + kr
                        ks = slice(kr * 64, kr * 64 + 64)
                        jj = []
                        for rr in range(2):
                            qrow = 2 * m + rr
                            st = start_row(qrow)
                            if st <= krow < st + 8:
                                jj.append(14 - (krow - qrow + 7))
                            else:
                                jj.append(None)
                        eng = "dve" if (ci + kr) % 2 == 0 else "pool"
                        if jj[0] is not None and jj[1] is not None:
                            assert jj[1] == jj[0] + 1
                            j0 = h * 15 + jj[0]
                            self.tt(eng, PTb[ks, ci, :].rearrange("p (r q) -> p r q", q=64),
                                    Eb[ks, ci, :].rearrange("p (r q) -> p r q", q=64),
                                    bank[ks, j0:j0 + 2, :], ALU.mult, R=[Eb, bank], W=[PTb])
                        else:
                            for rr in range(2):
                                if jj[rr] is None:
                                    P.op(eng, lambda e, ks=ks, ci=ci, rr=rr, PTb=PTb: e.memset(
                                        PTb[ks, ci, rr * 64:(rr + 1) * 64], 0.0), W=[PTb])
                                else:
                                    j0 = h * 15 + jj[rr]
                                    self.tt(eng, PTb[ks, ci, rr * 64:(rr + 1) * 64], Eb[ks, ci, rr * 64:(rr + 1) * 64],
                                            bank[ks, j0, :], ALU.mult, R=[Eb, bank], W=[PTb])
                def tail(n=n, chunks=chunks, nch=nch, PTb=PTb, h=h, ms=ms, qt=qt):
                    ob = pb[4 + (n % 2)]
                    for ci, kt in enumerate(chunks):
                        self.mm(ob[:, 0:65], PTb[:, ci, :], vsb[:, kt, h * VW:h * VW + 65], ci == 0, ci == nch - 1,
                                R=[PTb, vsb], W=[ob])
                    r_ = rd[n % 2]
                    P.op("dve", lambda e, r_=r_, ob=ob: e.reciprocal(r_[:], ob[:, 64:65]), R=[ob], W=[r_])
                    self.ts("dve", ms[:, h * 64:(h + 1) * 64], ob[:, 0:64], r_[:, 0:1], None, ALU.mult, None,
                            R=[ob, r_], W=[ms])
                    if h == 3:
                        P.dma("pool", self.mix[qt * 128:(qt + 1) * 128, 0:256], ms[:], R=[ms], W=[self.mix])
                if pend_tail is not None:
                    pend_tail()
                pend_tail = tail
                n += 1
        pend_tail()
        P.flush()

    def stage_diff(self, l, ctx_out):
        P = self.P
        pb = self.pb
        lam_init = 0.8 - 0.6 * math.exp(-0.3 * l)
        lv = P.sb([1, 128], F32)
        P.dma("sp", lv[:], self.df_lambda.t.ap()[l].unsqueeze(0), R=[self.df_lambda], W=[lv])
        pr = P.sb([1, 64], F32)
        lvv = lv[:].rearrange("p (a b) -> p a b", b=32)
        self.tt("dve", pr[:].rearrange("p (a b) -> p a b", b=32), lvv[:, 0:4:2, :], lvv[:, 1:4:2, :], ALU.mult, R=[lv], W=[pr])
        sm = P.sb([1, 4], F32)
        P.op("dve", lambda e: e.reduce_sum(sm[:, 0:2], pr[:].rearrange("p (a b) -> p a b", b=32), AX.X), R=[pr], W=[sm])
        self.act(sm[:, 0:2], sm[:, 0:2], AF.Exp, R=[sm], W=[sm])
        self.tt("dve", sm[:, 2:3], sm[:, 1:2], sm[:, 0:1], ALU.subtract, R=[sm], W=[sm])
        self.ts("dve", sm[:, 3:4], sm[:, 2:3], -lam_init, None, ALU.add, None, R=[sm], W=[sm])
        self.mm(pb[7][:, 0:1], self.onesf[0:1, :], sm[0:1, 3:4], True, True, R=[self.onesf, sm], W=[pb[7]])
        neglam = P.sb([128, 1], F32)
        self.cp("dve", neglam[:], pb[7][:, 0:1], R=[pb[7]], W=[neglam])
        gdf = P.sb([128, 64], F32)
        P.dma("sp", gdf[:], self.df_norm_g.t.ap()[l].partition_broadcast(128), R=[self.df_norm_g], W=[gdf])
        self.ts("dve", gdf[:], gdf[:], 1.0 - lam_init, None, ALU.mult, None, R=[gdf], W=[gdf])
        kT = [P.sb([128, T], BF16) for _ in range(2)]
        qA = [P.sb([128, T], BF16) for _ in range(2)]
        qB = [P.sb([128, T], BF16) for _ in range(2)]
        for hp in range(2):
            P.dma("sp", kT[hp][:], self.projT[1536 + hp * 128:1536 + (hp + 1) * 128, :], R=[self.projT], W=[kT[hp]])
            P.op("pool", lambda e, hp=hp: e.memset(qA[hp][:], 0.0), W=[qA[hp]])
            P.op("pool", lambda e, hp=hp: e.memset(qB[hp][:], 0.0), W=[qB[hp]])
            for hl in range(2):
                r0 = 1280 + hp * 128 + hl * 64
                P.dma("sp", qA[hp][hl * 64:hl * 64 + 32, :], self.projT[r0:r0 + 32, :], R=[self.projT], W=[qA[hp]])
                P.dma("sp", qB[hp][hl * 64 + 32:hl * 64 + 64, :], self.projT[r0 + 32:r0 + 64, :], R=[self.projT], W=[qB[hp]])
        vsb = P.sb([128, NT, 4 * VW], BF16)
        P.dma("sp", vsb[:], self.dfvd.t.ap().rearrange("(n p) c -> p n c", p=128), R=[self.dfvd], W=[vsb])
        P1 = [P.sb([128, 512], BF16) for _ in range(3)]
        P2 = [P.sb([128, 512], BF16) for _ in range(3)]
        mst = [P.sb([128, 4, 256], BF16) for _ in range(2)]
        rc = P.sb([128, 8], F32)
        av = P.sb([128, 4, 64], F32)
        bv = P.sb([128, 4, 64], F32)
        sq = P.sb([128, 4, 64], F32)
        s4 = P.sb([128, 8], F32)
        oT = P.sb([65, 2, 512], F32)
        scale = 1.0 / math.sqrt(32.0)
        qblocks = ([(0, 256, list(range(2)))] if ctx_out else []) + [(L + i * 512, 512, list(range(NT))) for i in range(8)]
        n = 0
        for bi, (q0, nq, kcs) in enumerate(qblocks):
            nqs = nq // 128
            ms = mst[bi % 2]
            for h in range(4):
                hp, hl = h // 2, h % 2
                psl = slice(hl * 64, hl * 64 + 64)
                o1, o2 = pb[6], pb[7]
                def score(kc):
                    nn_ = n
                    s1, s2 = pb[(2 * nn_) % 6], pb[(2 * nn_ + 1) % 6]
                    p1, p2 = P1[nn_ % 3], P2[nn_ % 3]
                    lhs = kT[hp][psl, kc * 128:(kc + 1) * 128]
                    self.mm(s1[:, :nq], lhs, qA[hp][psl, q0:q0 + nq], True, True, R=[kT[hp], qA[hp]], W=[s1])
                    self.mm(s2[:, :nq], lhs, qB[hp][psl, q0:q0 + nq], True, True, R=[kT[hp], qB[hp]], W=[s2])
                    self.act(p1[:, :nq], s1[:, :nq], AF.Exp, R=[s1], W=[p1], scale=scale)
                    self.act(p2[:, :nq], s2[:, :nq], AF.Exp, R=[s2], W=[p2], scale=scale)
                    return p1, p2

                def pv(ki, kc, p1, p2):
                    vst_ = vsb[:, kc, h * VW:h * VW + 65]
                    self.mm(o1[0:65, :nq], vst_, p1[:, :nq], ki == 0, ki == len(kcs) - 1, R=[p1, vsb], W=[o1])
                    self.mm(o2[0:65, :nq], vst_, p2[:, :nq], ki == 0, ki == len(kcs) - 1, R=[p2, vsb], W=[o2])

                pend = []
                for ki, kc in enumerate(kcs):
                    pp = score(kc)
                    n += 1
                    pend.append((ki, kc, pp[0], pp[1]))
                    if len(pend) > 2:
                        pv(*pend.pop(0))
                while pend:
                    pv(*pend.pop(0))
                self.cp("act", oT[:, 0, :nq], o1[0:65, :nq], R=[o1], W=[oT])
                self.cp("dve", oT[:, 1, :nq], o2[0:65, :nq], R=[o2], W=[oT])
                if self.dbg and bi == 0 and h == 0:
                    dd1 = self.dscr(f"dbg_oT{l}", [65, 1024], F32)
                    P.dma("sp", dd1[:], oT[:].rearrange("p a b -> p (a b)"), R=[oT], W=[dd1])
                t1b, t2b = pb[0], pb[1]
                for mi, tb_ in ((0, t1b), (1, t2b)):
                    for qs in range(nqs):
                        P.op("pe", lambda e, mi=mi, tb_=tb_, qs=qs: e.transpose(
                            tb_[:, qs * 65:(qs + 1) * 65], oT[:, mi, qs * 128:(qs + 1) * 128], self.identf[0:65, 0:65]),
                            R=[oT, self.identf], W=[tb_])
                if self.dbg and bi == 0 and h == 0:
                    dd2 = self.dscr(f"dbg_t1b{l}", [128, 512], F32)
                    dtmp = P.sb([128, 512], F32)
                    self.cp("dve", dtmp[:], t1b[:, :], R=[t1b], W=[dtmp])
                    P.dma("sp", dd2[:], dtmp[:], R=[dtmp], W=[dd2])
                o1v = t1b[:, 0:260].rearrange("p (a b) -> p a b", b=65)[:, 0:nqs, :]
                o2v = t2b[:, 0:260].rearrange("p (a b) -> p a b", b=65)[:, 0:nqs, :]
                o1, o2 = t1b, t2b
                P.op("dve", lambda e, o1v=o1v, nqs=nqs: e.reciprocal(rc[:, 0:nqs], o1v[:, :, 64]), R=[o1], W=[rc])
                P.op("dve", lambda e, o2v=o2v, nqs=nqs: e.reciprocal(rc[:, 4:4 + nqs], o2v[:, :, 64]), R=[o2], W=[rc])
                self.tt("dve", av[:, 0:nqs, :], o1v[:, :, 0:64], rc[:, 0:nqs].unsqueeze(2).to_broadcast([128, nqs, 64]),
                        ALU.mult, R=[o1, rc], W=[av])
                self.tt("dve", bv[:, 0:nqs, :], o2v[:, :, 0:64], rc[:, 4:4 + nqs].unsqueeze(2).to_broadcast([128, nqs, 64]),
                        ALU.mult, R=[o2, rc], W=[bv])
                self.stt("dve", av[:, 0:nqs, :], bv[:, 0:nqs, :], neglam[:, 0:1], av[:, 0:nqs, :], ALU.mult, ALU.add,
                         R=[bv, av, neglam], W=[av])
                self.tt("pool", sq[:, 0:nqs, :], av[:, 0:nqs, :], av[:, 0:nqs, :], ALU.mult, R=[av], W=[sq])
                P.op("dve", lambda e, nqs=nqs: e.reduce_sum(s4[:, 0:nqs], sq[:, 0:nqs, :], AX.X), R=[sq], W=[s4])
                self.rsqrt_mean(s4[:, 4:4 + nqs], s4[:, 0:nqs], 64.0, 1e-6, R=[s4], W=[s4])
                self.tt("dve", av[:, 0:nqs, :], av[:, 0:nqs, :], s4[:, 4:4 + nqs].unsqueeze(2).to_broadcast([128, nqs, 64]),
                        ALU.mult, R=[av, s4], W=[av])
                self.tt("dve", ms[:, 0:nqs, h * 64:(h + 1) * 64], av[:, 0:nqs, :],
                        gdf[:].unsqueeze(1).to_broadcast([128, nqs, 64]), ALU.mult, R=[av, gdf], W=[ms])
            P.dma("pool", self.mix[q0:q0 + nq, 512:768].rearrange("(a p) c -> p a c", p=128), ms[:, 0:nqs, :],
                  R=[ms], W=[self.mix])
        P.flush()

    def stage_fft(self, l, ctx_out):
        P = self.P
        pb = self.pb
        ftw = P.sb([128, 2, 256], BF16)
        P.dma("pool", ftw[:], self.ft_w.t.ap()[l].rearrange("(c p) n -> p c n", p=128), R=[self.ft_w], W=[ftw])
        bdc = P.sb([128, 128], BF16)
        bds = P.sb([128, 128], BF16)
        P.dma("pool", bdc[:], self.c_bdc[:], R=[self.c_bdc], W=[bdc])
        P.dma("pool", bds[:], self.c_bds[:], R=[self.c_bds], W=[bds])
        M12 = P.sb([128, 2, 512], BF16)
        for j in range(2):
            self.mm(pb[0][:, 0:256], bdc[:], ftw[:, j, :], True, True, R=[bdc, ftw], W=[pb[0]])
            self.mm(pb[1][:, 0:256], bds[:], ftw[:, j, :], True, True, R=[bds, ftw], W=[pb[1]])
            self.cp("act", M12[:, j, 0:256], pb[0][:, 0:256], R=[pb[0]], W=[M12])
            P.op("act", lambda e, j=j: e.mul(M12[:, j, 256:512], pb[1][:, 0:256], -1.0), R=[pb[1]], W=[M12])
        uT = P.sb([128, 2, T], BF16)
        P.dma("sp", uT[:], self.projT[1792:2048, :].rearrange("(c p) t -> p c t", p=128), R=[self.projT], W=[uT])
        uM = P.sb([128, NT, 512], BF16)
        for t in range(NT):
            bk = pb[t % 2]
            for c in range(2):
                self.mm(bk[:, :], uT[:, c, t * 128:(t + 1) * 128], M12[:, c, :], c == 0, c == 1, R=[uT, M12], W=[bk])
            self.cp("act" if t % 2 == 0 else "dve", uM[:, t, :], bk[:, :], R=[bk], W=[uM])
        ct = [P.sb([128, 32, 128], BF16) for _ in range(2)]
        st = [P.sb([128, 32, 128], BF16) for _ in range(2)]
        og = [P.sb([128, 256], BF16) for _ in range(2)]
        jobs = ([("c", ti) for ti in range(2)] if ctx_out else []) + [("l", ti) for ti in range(32)]
        for n, (kind, ti) in enumerate(jobs):
            c_, s_ = ct[n % 2], st[n % 2]
            if kind == "l":
                ntc, tb = 32, 2
                P.dma("sp", c_[:], self.c_dftc[ti], R=[self.c_dftc], W=[c_])
                P.dma("sp", s_[:], self.c_dfts[ti], R=[self.c_dfts], W=[s_])
            else:
                ntc, tb = 2, 0
                P.dma("sp", c_[:, 0:2, :], self.c_dftc_c[ti], R=[self.c_dftc_c], W=[c_])
                P.dma("sp", s_[:, 0:2, :], self.c_dfts_c[ti], R=[self.c_dfts_c], W=[s_])
            bk = pb[2 + n % 2]
            for tc in range(ntc):
                self.mm(bk[:, 0:256], c_[:, tc, :], uM[:, tb + tc, 0:256], tc == 0, False, R=[c_, uM], W=[bk])
                self.mm(bk[:, 0:256], s_[:, tc, :], uM[:, tb + tc, 256:512], False, tc == ntc - 1, R=[s_, uM], W=[bk])
            o_ = og[n % 2]
            self.cp("act" if n % 2 == 0 else "dve", o_[:], bk[:, 0:256], R=[bk], W=[o_])
            t = tb + ti
            P.dma("pool", self.mix[t * 128:(t + 1) * 128, 768:1024], o_[:], R=[o_], W=[self.mix])
        P.flush()

    def stage_outproj(self, l, ctx_out):
        P = self.P
        pb = self.pb
        wout = P.sb([128, 8, D], BF16)
        for k in range(8):
            P.dma("pool", wout[:, k, :], self.w_out.t.ap()[l, k * 128:(k + 1) * 128, :], R=[self.w_out], W=[wout])
        wr = P.sb([128, 8, NE], BF16)
        P.dma("pool", wr[:], self.w_router.t.ap()[l].rearrange("(k p) n -> p k n", p=128), R=[self.w_router], W=[wr])
        gs2T = P.sb([128, 8, 2], F32)
        self.ts("dve", gs2T[:], self.mT[:, 32:40, :], 1.0, None, ALU.add, None, R=[self.mT], W=[gs2T])
        self.tt("dve", gs2T[:], gs2T[:], self.g2T[:].unsqueeze(2).to_broadcast([128, 8, 2]), ALU.mult,
                R=[gs2T, self.g2T], W=[gs2T])
        streams = [0, 1] if ctx_out else [0]
        m2b, gs2b, sh2b = {}, {}, {}
        for s_ in streams:
            m2b[s_] = P.sb([128, D], F32)
            gs2b[s_] = P.sb([128, D], F32)
            sh2b[s_] = P.sb([128, D], F32)
            self.bcast_vec(m2b[s_], lambda c, s_=s_: self.mT[:, 16 + c, s_:s_ + 1], [self.mT])
            self.bcast_vec(gs2b[s_], lambda c, s_=s_: gs2T[:, c, s_:s_ + 1], [gs2T])
            self.bcast_vec(sh2b[s_], lambda c, s_=s_: self.mT[:, 24 + c, s_:s_ + 1], [self.mT])
        mx = [P.sb([128, D], BF16) for _ in range(2)]
        mxT = [P.sb([128, 8, 128], BF16) for _ in range(2)]
        xt = [P.sb([128, D], F32) for _ in range(2)]
        tmp = [P.sb([128, D], F32) for _ in range(2)]
        xn = [P.sb([128, D], F32) for _ in range(2)]
        junk = P.sb([128, D], BF16)
        ss = [P.sb([128, 2], F32) for _ in range(2)]
        h2 = [P.sb([128, D], BF16) for _ in range(2)]
        h2T = [P.sb([128, 8, 128], BF16) for _ in range(2)]
        sm = [P.sb([128, 4], F32) for _ in range(2)]
        ex = [P.sb([128, NE], F32) for _ in range(2)]
        tiles = list(range(0 if ctx_out else 2, NT))
        for n, t in enumerate(tiles):
            s_ = 1 if t < 2 else 0
            i = n % 2
            rs = slice(t * 128, (t + 1) * 128)
            P.dma("sp", mx[i][:], self.mix[rs, :], R=[self.mix], W=[mx[i]])
            P.dma("sp", xt[i][:], self.xres[rs, :], R=[self.xres], W=[xt[i]])
            bT = pb[0 + i]
            pT = bT[:].bitcast(BF16)
            for c in range(8):
                P.op("pe", lambda e, c=c, pT=pT, i=i: e.transpose(pT[:, c * 128:(c + 1) * 128], mx[i][:, c * 128:(c + 1) * 128],
                                                                   self.ident[:]), R=[mx[i], self.ident], W=[bT])
            self.cp("act", mxT[i][:].rearrange("p c t -> p (c t)"), pT[:, :], R=[bT], W=[mxT[i]])
            for nn in range(2):
                bk = pb[2 + nn]
                for k in range(8):
                    self.mm(bk[:, :], mxT[i][:, k, :], wout[:, k, nn * 512:(nn + 1) * 512], k == 0, k == 7,
                            R=[mxT[i], wout], W=[bk])
                self.tt("dve", tmp[i][:, nn * 512:(nn + 1) * 512], bk[:, :], m2b[s_][:, nn * 512:(nn + 1) * 512], ALU.mult,
                        R=[bk, m2b[s_]], W=[tmp[i]])
            self.tt("pool", xn[i][:], xt[i][:], tmp[i][:], ALU.add, R=[xt[i], tmp[i]], W=[xn[i]])
            P.dma("pool", self.xres[rs, :], xn[i][:], R=[xn[i]], W=[self.xres])
            P.op("dve", lambda e, i=i: e.memset(ss[i][:], 0.0), W=[ss[i]])
            self.act(junk[:], xn[i][:], AF.Square, R=[xn[i], ss[i]], W=[junk, ss[i]], accum_out=ss[i][:, 0:1])
            self.rsqrt_mean(ss[i][:, 1:2], ss[i][:, 0:1], float(D), 1e-6, R=[ss[i]], W=[ss[i]])
            self.stt("dve", tmp[i][:], xn[i][:], ss[i][:, 1:2], gs2b[s_][:], ALU.mult, ALU.mult,
                     R=[xn[i], ss[i], gs2b[s_]], W=[tmp[i]])
            self.tt("pool", h2[i][:], tmp[i][:], sh2b[s_][:], ALU.add, R=[tmp[i], sh2b[s_]], W=[h2[i]])
            P.dma("pool", self.h2d[rs, :], h2[i][:], R=[h2[i]], W=[self.h2d])
            bT2 = pb[4 + i]
            pT2 = bT2[:].bitcast(BF16)
            for c in range(8):
                P.op("pe", lambda e, c=c, pT2=pT2, i=i: e.transpose(pT2[:, c * 128:(c + 1) * 128], h2[i][:, c * 128:(c + 1) * 128],
                                                                     self.ident[:]), R=[h2[i], self.ident], W=[bT2])
            self.cp("act", h2T[i][:].rearrange("p c t -> p (c t)"), pT2[:, :], R=[bT2], W=[h2T[i]])
            bR = pb[6 + i]
            for k in range(8):
                self.mm(bR[:, 0:NE], h2T[i][:, k, :], wr[:, k, :], k == 0, k == 7, R=[h2T[i], wr], W=[bR])
            P.op("dve", lambda e, i=i, bR=bR: e.reduce_max(sm[i][:, 0:1], bR[:, 0:NE], AX.X), R=[bR], W=[sm[i]])
            self.ts("dve", sm[i][:, 1:2], sm[i][:, 0:1], -1.0, None, ALU.mult, None, R=[sm[i]], W=[sm[i]])
            P.op("dve", lambda e, i=i: e.memset(sm[i][:, 2:3], 0.0), W=[sm[i]])
            self.act(ex[i][:], bR[:, 0:NE], AF.Exp, R=[bR, sm[i]], W=[ex[i], sm[i]], bias=sm[i][:, 1:2], accum_out=sm[i][:, 2:3])
            P.op("dve", lambda e, i=i: e.reciprocal(sm[i][:, 3:4], sm[i][:, 2:3]), R=[sm[i]], W=[sm[i]])
            self.ts("dve", self.aff[:, t, :], ex[i][:], sm[i][:, 3:4], None, ALU.mult, None, R=[ex[i], sm[i]], W=[self.aff])
        if self.dbg:
            da = self.dscr(f"dbg_aff{l}", [128, NT * NE], F32)
            P.dma("sp", da[:], self.aff[:].rearrange("p t e -> p (t e)"), R=[self.aff], W=[da])
        P.flush()

    def stage_route(self, l, ctx_out):
        P = self.P
        pb = self.pb
        sbase = P.sb([128, NT, NE], F32)
        P.dma("sp", sbase[:].rearrange("p t e -> p (t e)"), self.c_slotbase[:], R=[self.c_slotbase], W=[sbase])
        streams = [(2, 32, CAP_L)] + ([(0, 2, CAP_C)] if ctx_out else [])
        cmpb = P.sb([128, 32, NE], F32)
        lo = P.sb([128, NE], F32)
        mid = P.sb([128, NE], F32)
        cnt = P.sb([128, NE], F32)
        ge = P.sb([128, NE], F32)
        tot = P.sb([128, 32, NE], F32)
        off = P.sb([128, 32, NE], F32)
        pos = P.sb([128, 32, NE], F32)
        m2 = P.sb([128, 32, NE], F32)
        dstf = P.sb([128, 32, NE], F32)
        for (t0, nt, cap) in streams:
            affv = self.aff[:, t0:t0 + nt, :]
            cv = cmpb[:, 0:nt, :]
            P.op("dve", lambda e: e.memset(lo[:], 0.0), W=[lo])
            for it in range(32):
                hstep = 2.0 ** (-(it + 1))
                self.ts("dve", mid[:], lo[:], hstep, None, ALU.add, None, R=[lo], W=[mid])
                self.tt("dve", cv, affv, mid[:].unsqueeze(1).to_broadcast([128, nt, NE]), ALU.is_gt,
                        R=[self.aff, mid], W=[cmpb])
                P.op("dve", lambda e, cv=cv: e.reduce_sum(cnt[:], cv.rearrange("p t e -> p e t"), AX.X), R=[cmpb], W=[cnt])
                self.mm(pb[0][:, 0:NE], self.onesf[:], cnt[:], True, True, R=[self.onesf, cnt], W=[pb[0]])
                self.ts("dve", ge[:], pb[0][:, 0:NE], cap - 0.5, None, ALU.is_ge, None, R=[pb[0]], W=[ge])
                self.stt("dve", lo[:], ge[:], hstep, lo[:], ALU.mult, ALU.add, R=[ge, lo], W=[lo])
            self.tt("dve", cv, affv, lo[:].unsqueeze(1).to_broadcast([128, nt, NE]), ALU.is_gt, R=[self.aff, lo], W=[cmpb])
            cvf = cv.rearrange("p t e -> p (t e)")
            self.mm(pb[1][:, 0:nt * NE], self.trif[:], cvf, True, True, R=[self.trif, cmpb], W=[pb[1]])
            self.mm(pb[2][:, 0:nt * NE], self.onesf[:], cvf, True, True, R=[self.onesf, cmpb], W=[pb[2]])
            self.cp("act", tot[:, 0:nt, :].rearrange("p t e -> p (t e)"), pb[2][:, 0:nt * NE], R=[pb[2]], W=[tot])
            P.op("dve", lambda e: e.memset(off[:, 0, :], 0.0), W=[off])
            for j in range(1, nt):
                self.tt("dve", off[:, j, :], off[:, j - 1, :], tot[:, j - 1, :], ALU.add, R=[off, tot], W=[off])
            self.tt("dve", pos[:, 0:nt, :].rearrange("p t e -> p (t e)"), pb[1][:, 0:nt * NE],
                    off[:, 0:nt, :].rearrange("p t e -> p (t e)"), ALU.add, R=[pb[1], off], W=[pos])
            self.ts("dve", m2[:, 0:nt, :], pos[:, 0:nt, :], cap + 0.5, None, ALU.is_lt, None, R=[pos], W=[m2])
            self.tt("dve", m2[:, 0:nt, :], m2[:, 0:nt, :], cv, ALU.mult, R=[m2, cmpb], W=[m2])
            self.stt("dve", dstf[:, 0:nt, :], pos[:, 0:nt, :], -1.0, sbase[:, t0:t0 + nt, :], ALU.add, ALU.add,
                     R=[pos, sbase], W=[dstf])
            self.tt("dve", dstf[:, 0:nt, :], dstf[:, 0:nt, :], m2[:, 0:nt, :], ALU.mult, R=[dstf, m2], W=[dstf])
            self.ts("dve", dstf[:, 0:nt, :], dstf[:, 0:nt, :], float(TRASH), None, ALU.add, None, R=[dstf], W=[dstf])
            self.cp("dve", self.desti[:, t0:t0 + nt, :], dstf[:, 0:nt, :], R=[dstf], W=[self.desti])
            self.tt("dve", self.gatev[:, t0:t0 + nt, :], affv, m2[:, 0:nt, :], ALU.mult, R=[self.aff, m2], W=[self.gatev])
        if self.dbg:
            dd = self.dscr(f"dbg_dest{l}", [128, NT * NE], I32)
            P.dma("sp", dd[:], self.desti[:].rearrange("p t e -> p (t e)"), R=[self.desti], W=[dd])
        ht = [P.sb([128, D], BF16) for _ in range(3)]
        tiles = list(range(0 if ctx_out else 2, NT))
        for n, t in enumerate(tiles):
            hb = ht[n % 3]
            P.dma("sp", hb[:], self.h2d[t * 128:(t + 1) * 128, :], R=[self.h2d], W=[hb])
            for e_ in range(NE):
                P.op("pool", lambda e, t=t, e_=e_, hb=hb: e.indirect_dma_start(
                    out=self.xg[:, :], out_offset=bass.IndirectOffsetOnAxis(ap=self.desti[:, t, e_:e_ + 1], axis=0),
                    in_=hb[:, :], in_offset=None),
                    R=[hb, self.desti], W=[], kind="dma")
        P.flush()

    def stage_ffn(self, l, ctx_out):
        P = self.P
        pb = self.pb
        NW = 6
        wbuf = [P.sb([128, 8 * 1024], BF16) for _ in range(NW)]
        nw = [0]

        def load_w(kind, e_, part):
            w = wbuf[nw[0] % NW]
            nw[0] += 1
            if kind == "g":
                src = self.w_gate.t.ap()[l, e_, :, part * 1024:(part + 1) * 1024].rearrange("(k p) n -> p k n", p=128)
                P.dma("pool", w[:].rearrange("p (k n) -> p k n", n=1024), src, R=[self.w_gate], W=[w])
            elif kind == "u":
                src = self.w_up.t.ap()[l, e_, :, part * 1024:(part + 1) * 1024].rearrange("(k p) n -> p k n", p=128)
                P.dma("pool", w[:].rearrange("p (k n) -> p k n", n=1024), src, R=[self.w_up], W=[w])
            else:
                src = self.w_down.t.ap()[l, e_, :, part * 512:(part + 1) * 512].rearrange("(k p) n -> p k n", p=128)
                P.dma("pool", w[:].rearrange("p (k n) -> p k n", n=512), src, R=[self.w_down], W=[w])
            return w

        nsl = ESL if ctx_out else CAP_L
        stiles = [(0, 128), (128, 128), (256, 128), (384, 128)] + ([(512, 32)] if ctx_out else [])
        xr = [P.sb([128, D], BF16) for _ in range(3)]
        xgT = [P.sb([128, 8, ESL], BF16) for _ in range(2)]
        hidT = P.sb([128, 16, ESL], BF16)
        sg = [P.sb([128, 512], F32) for _ in range(2)]
        sgc = P.sb([128, 32], F32)
        yst = [P.sb([128, 512], F32) for _ in range(3)]
        nx = 0
        ny = 0
        for e_ in range(NE):
            xT = xgT[e_ % 2]
            for si, (s0, rows) in enumerate(stiles):
                xb = xr[nx % 3]
                nx += 1
                r0 = e_ * ESL + s0
                P.dma("sp", xb[:rows, :], self.xg[r0:r0 + rows, :], R=[self.xg], W=[xb])
                bT = pb[6 + (si % 2)]
                pT = bT[:].bitcast(BF16)
                for c in range(8):
                    P.op("pe", lambda e, c=c, pT=pT, xb=xb, rows=rows: e.transpose(
                        pT[:, c * 128:c * 128 + rows], xb[:rows, c * 128:(c + 1) * 128], self.ident[:rows, :rows]),
                        R=[xb, self.ident], W=[bT])
                self.cp("act" if si % 2 == 0 else "dve", xT[:, :, s0:s0 + rows],
                        pT[:, :].rearrange("p (c t) -> p c t", t=128)[:, :, 0:rows], R=[bT], W=[xT])
            for fh in range(2):
                wg = load_w("g", e_, fh)
                wu = load_w("u", e_, fh)
                wgv = wg[:].rearrange("p (k n) -> p k n", n=1024)
                wuv = wu[:].rearrange("p (k n) -> p k n", n=1024)
                for fc in range(8):
                    fcg = fh * 8 + fc
                    gb, ub, cb = pb[0 + (fcg % 2)], pb[2 + (fcg % 2)], pb[4 + (fcg % 2)]
                    for k in range(8):
                        lw = wgv[:, k, fc * 128:(fc + 1) * 128]
                        self.mm(gb[:, :], lw, xT[:, k, 0:512], k == 0, k == 7, R=[wg, xT], W=[gb])
                        if ctx_out:
                            self.mm(cb[:, 0:32], lw, xT[:, k, 512:544], k == 0, k == 7, R=[wg, xT], W=[cb])
                    for k in range(8):
                        lw = wuv[:, k, fc * 128:(fc + 1) * 128]
                        self.mm(ub[:, :], lw, xT[:, k, 0:512], k == 0, k == 7, R=[wu, xT], W=[ub])
                        if ctx_out:
                            self.mm(cb[:, 32:64], lw, xT[:, k, 512:544], k == 0, k == 7, R=[wu, xT], W=[cb])
                    sgb = sg[fcg % 2]
                    self.act(sgb[:], gb[:, :], AF.Silu, R=[gb], W=[sgb])
                    self.tt("dve", hidT[:, fcg, 0:512], sgb[:], ub[:, :], ALU.mult, R=[sgb, ub], W=[hidT])
                    if ctx_out:
                        self.act(sgc[:], cb[:, 0:32], AF.Silu, R=[cb], W=[sgc])
                        self.tt("dve", hidT[:, fcg, 512:544], sgc[:], cb[:, 32:64], ALU.mult, R=[sgc, cb], W=[hidT])
            for nn in range(2):
                wd = load_w("d", e_, nn)
                wdv = wd[:].rearrange("p (k n) -> p k n", n=512)
                for si, (s0, rows) in enumerate(stiles):
                    yb = pb[6 + (si % 2)]
                    for fc in range(16):
                        self.mm(yb[:rows, :], hidT[:, fc, s0:s0 + rows], wdv[:, fc, :], fc == 0, fc == 15, R=[hidT, wd], W=[yb])
                    ys = yst[ny % 3]
                    ny += 1
                    self.cp("act" if si % 2 == 0 else "dve", ys[:rows, :], yb[:rows, :], R=[yb], W=[ys])
                    r0 = e_ * ESL + s0
                    P.dma("sp", self.ybuf[r0:r0 + rows, nn * 512:(nn + 1) * 512], ys[:rows, :], R=[ys], W=[])
        P.flush()

    def stage_combine(self, l, ctx_out, final):
        P = self.P
        streams = [0, 1] if ctx_out else [0]
        m5b = {}
        for s_ in streams:
            m5b[s_] = P.sb([128, D], F32)
            self.bcast_vec(m5b[s_], lambda c, s_=s_: self.mT[:, 40 + c, s_:s_ + 1], [self.mT])
        if final:
            gfT = P.sb([128, 8], F32)
            P.dma("sp", gfT[:], self.final_g.t.ap().rearrange("(c p) -> p c", p=128), R=[self.final_g], W=[gfT],
                  allow_slow_non_contiguous=True)
            gfb = P.sb([128, D], F32)
            self.bcast_vec(gfb, lambda c: gfT[:, c:c + 1], [gfT])
            junk = P.sb([128, D], BF16)
        G = [P.sb([128, D], F32) for _ in range(NE)]
        xt = [P.sb([128, D], F32) for _ in range(2)]
        acc = [P.sb([128, D], F32) for _ in range(2)]
        ss = [P.sb([128, 2], F32) for _ in range(2)]
        tiles = list(range(0 if ctx_out else 2, NT))
        for n, t in enumerate(tiles):
            s_ = 1 if t < 2 else 0
            i = n % 2
            rs = slice(t * 128, (t + 1) * 128)
            P.dma("sp", xt[i][:], self.xres[rs, :], R=[self.xres], W=[xt[i]])
            for e_ in range(NE):
                P.op("pool", lambda e, t=t, e_=e_: e.indirect_dma_start(
                    out=G[e_][:, :], out_offset=None, in_=self.ybuf[:, :],
                    in_offset=bass.IndirectOffsetOnAxis(ap=self.desti[:, t, e_:e_ + 1], axis=0)), R=[self.ybuf, self.desti], W=[G[e_]], kind="dma")
            a = acc[i]
            self.ts("dve", a[:], G[0][:], self.gatev[:, t, 0:1], None, ALU.mult, None, R=[G[0], self.gatev], W=[a])
            for e_ in range(1, NE):
                self.stt("dve", a[:], G[e_][:], self.gatev[:, t, e_:e_ + 1], a[:], ALU.mult, ALU.add,
                         R=[G[e_], self.gatev, a], W=[a])
            self.tt("dve", a[:], a[:], m5b[s_][:], ALU.mult, R=[a, m5b[s_]], W=[a])
            self.tt("dve", a[:], a[:], xt[i][:], ALU.add, R=[a, xt[i]], W=[a])
            if not final or t < 2:
                P.dma("sp", self.xres[rs, :], a[:], R=[a], W=[self.xres])
            else:
                if self.dbg:
                    P.dma("sp", self.xres[rs, :], a[:], R=[a], W=[self.xres])
                P.op("dve", lambda e, i=i: e.memset(ss[i][:], 0.0), W=[ss[i]])
                self.act(junk[:], a[:], AF.Square, R=[a, ss[i]], W=[junk, ss[i]], accum_out=ss[i][:, 0:1])
                self.rsqrt_mean(ss[i][:, 1:2], ss[i][:, 0:1], float(D), 1e-6, R=[ss[i]], W=[ss[i]])
                self.stt("dve", a[:], a[:], ss[i][:, 1:2], gfb[:], ALU.mult, ALU.mult, R=[a, ss[i], gfb], W=[a])
                P.dma("sp", self.out[(t - 2) * 128:(t - 1) * 128, :], a[:], R=[a], W=[self.out])
        P.flush()


    def _gdn_prep(self, l, qT, kT, qtok, ktok, vtok, gg, beta, nbeta, obb):
        P = self.P
        pb = self.pb
        identb_f = self.identf
        ab = P.sb([128, NT, 16], F32)
        P.dma("sp", ab[:], self.abd.t.ap().rearrange("(n p) c -> p n c", p=128), R=[self.abd], W=[ab])
        dtb = P.sb([128, 8], F32)
        nA = P.sb([128, 8], F32)
        P.dma("sp", dtb[:], self.dn_dt_bias.t.ap()[l].partition_broadcast(128), R=[self.dn_dt_bias], W=[dtb])
        P.dma("sp", nA[:], self.dn_a_log.t.ap()[l].partition_broadcast(128), R=[self.dn_a_log], W=[nA])
        self.act(nA[:], nA[:], AF.Exp, R=[nA], W=[nA])
        self.ts("dve", nA[:], nA[:], -1.0, None, ALU.mult, None, R=[nA], W=[nA])
        self.tt("dve", gg[:], ab[:, :, 0:8], dtb[:].unsqueeze(1).to_broadcast([128, NT, 8]), ALU.add, R=[ab, dtb], W=[gg])
        self.act(gg[:], gg[:], AF.Exp, R=[gg], W=[gg])
        self.act(gg[:], gg[:], AF.Ln, R=[gg], W=[gg], bias=1.0)
        self.tt("dve", gg[:], gg[:], nA[:].unsqueeze(1).to_broadcast([128, NT, 8]), ALU.mult, R=[gg, nA], W=[gg])
        self.act(beta[:], ab[:, :, 8:16], AF.Sigmoid, R=[ab], W=[beta])
        self.ts("dve", nbeta[:], beta[:], -1.0, None, ALU.mult, None, R=[beta], W=[nbeta])
        segs = [(0, L), (L, T)]
        xs = [P.sb([128, T], BF16) for _ in range(2)]
        sil = P.sb([128, T], BF16)
        sqv = P.sb([128, 512], BF16)
        rn = P.sb([128, 512], F32)
        cw = [P.sb([128, 5], F32) for _ in range(2)]
        dg = [P.sb([128, 5, 128], BF16) for _ in range(2)]
        blocks = [(0, L, 0, L)] + [(L + 512 * j, 512, L, T) for j in range(8)]
        nblk = 0
        for cc in range(6):
            x_ = xs[cc % 2]
            w_ = cw[cc % 2]
            dg_ = dg[cc % 2]
            P.dma("sp", x_[:], self.projT[512 + cc * 128:512 + (cc + 1) * 128, :], R=[self.projT], W=[x_])
            P.dma("sp", w_[:], self.dn_conv_w.t.ap()[l, :, cc * 128:(cc + 1) * 128].rearrange("k c -> c k"),
                  R=[self.dn_conv_w], W=[w_], allow_slow_non_contiguous=True)
            for k in range(5):
                self.ts("dve", dg_[:, k, :], self.identf[:], w_[:, k:k + 1], None, ALU.mult, None, R=[self.identf, w_], W=[dg_])
            for (b0, nb, s0, s1) in blocks:
                bk = pb[4 + (nblk % 2)]
                nblk += 1
                self.mm(bk[:, :nb], dg_[:, 2, :], x_[:, b0:b0 + nb], True, False, R=[dg_, x_], W=[bk])
                taps = (0, 1, 3, 4)
                for ti_, k in enumerate(taps):
                    sft = k - 2
                    a_ = max(b0, s0 - sft)
                    b_ = min(b0 + nb, s1 - sft)
                    self.mm(bk[:, a_ - b0:b_ - b0], dg_[:, k, :], x_[:, a_ + sft:b_ + sft], False, ti_ == len(taps) - 1,
                            R=[dg_, x_], W=[bk])
                self.act(sil[:, b0:b0 + nb], bk[:, :nb], AF.Silu, R=[bk], W=[sil])
            if cc < 4:
                dst = qT[cc] if cc < 2 else kT[cc - 2]
                scl = 0.125 if cc < 2 else 1.0
                for b0 in range(0, T, 512):
                    nb = min(512, T - b0)
                    bk = pb[(b0 // 512) % 2]
                    self.tt("pool", sqv[:, :nb], sil[:, b0:b0 + nb], sil[:, b0:b0 + nb], ALU.mult, R=[sil], W=[sqv])
                    self.mm(bk[:, :nb], obb[:], sqv[:, :nb], True, True, R=[obb, sqv], W=[bk])
                    self.act(rn[:, :nb], bk[:, :nb], AF.Sqrt, R=[bk], W=[rn], bias=1e-6, scale=1.0)
                    P.op("dve", lambda e, nb=nb: e.reciprocal(rn[:, :nb], rn[:, :nb]), R=[rn], W=[rn])
                    self.stt("dve", dst[:, b0:b0 + nb], sil[:, b0:b0 + nb], scl, rn[:, :nb], ALU.mult, ALU.mult,
                             R=[sil, rn], W=[dst])
                srcT = dst
            else:
                srcT = sil
            tok = qtok if cc < 2 else (ktok if cc < 4 else vtok)
            hp = cc % 2
            for t in range(NT):
                bT = pb[2 + (t % 2)]
                pT = bT[:].bitcast(BF16)
                P.op("pe", lambda e, t=t, pT=pT, srcT=srcT: e.transpose(pT[:, 0:128], srcT[:, t * 128:(t + 1) * 128], self.ident[:]),
                     R=[srcT, self.ident], W=[bT])
                self.cp("act" if t % 2 == 0 else "dve", tok[:, t, hp * 128:(hp + 1) * 128], pT[:, 0:128], R=[bT], W=[tok])

    def stage_gdn(self, l, ctx_out):
        P = self.P
        pb = self.pb
        qT = [P.sb([128, T], BF16, persist="mid") for _ in range(2)]
        kT = [P.sb([128, T], BF16, persist="mid") for _ in range(2)]
        qtok = P.sb([128, NT, 256], BF16, persist="mid")
        ktok = P.sb([128, NT, 256], BF16, persist="mid")
        vtok = P.sb([128, NT, 256], BF16, persist="mid")
        gg = P.sb([128, NT, 8], F32, persist="mid")
        beta = P.sb([128, NT, 8], F32, persist="mid")
        nbeta = P.sb([128, NT, 8], F32, persist="mid")
        obb = P.sb([128, 128], BF16)
        P.dma("pool", obb[:], self.c_gob[:], R=[self.c_gob], W=[obb])
        self._gdn_prep(l, qT, kT, qtok, ktok, vtok, gg, beta, nbeta, obb)
        P.flush()
        oacc = P.sb([128, NT, 256], F32)
        cst = {}
        for nm, src, shp in (("U", self.c_gU, [2, 128]), ("negU", self.c_gnegU, [2, 128]), ("NML", self.c_gNML, [2, 128]),
                             ("NMQ", self.c_gNMQ, [2, 128])):
            tl = P.sb([128, 2, 128], F32)
            P.dma("sp", tl[:], src.t.ap().rearrange("d p i -> p d i"), R=[src], W=[tl])
            cst[nm] = tl
        ind = P.sb([128, 2, 64], F32)
        P.dma("sp", ind[:], self.c_gind.t.ap().rearrange("c p m -> p c m"), R=[self.c_gind], W=[ind])
        obf = P.sb([128, 128], F32)
        P.dma("sp", obf[:], self.c_gob[:], R=[self.c_gob], W=[obf])
        negones = P.sb([128, 128], F32)
        self.ts("dve", negones[:], self.onesf[:], -1.0, None, ALU.mult, None, R=[self.onesf], W=[negones])
        S32 = P.sb([64, 4, 64], F32)
        S16 = P.sb([64, 4, 64], BF16)
        NB = 2
        rhs1 = [P.sb([128, 4, 128], F32) for _ in range(NB)]
        gbm = [P.sb([128, 4, 128], F32) for _ in range(NB)]
        t1 = [P.sb([128, 4, 128], F32) for _ in range(NB)]
        t2 = [P.sb([128, 4, 128], F32) for _ in range(NB)]
        XT = [P.sb([128, 4, 128], BF16) for _ in range(NB)]
        X = [P.sb([128, 4, 128], BF16) for _ in range(NB)]
        Rm = [P.sb([128, 4, 128], BF16) for _ in range(NB)]
        qkT = [P.sb([128, 4, 128], BF16) for _ in range(NB)]
        sml = [P.sb([128, 16], F32) for _ in range(NB)]
        GL = [P.sb([64, 8], F32) for _ in range(NB)]
        vb = [P.sb([128, 4, 64], BF16) for _ in range(NB)]
        kbg = [P.sb([128, 4, 64], BF16) for _ in range(NB)]
        kdec = [P.sb([128, 4, 64], BF16) for _ in range(NB)]
        qdec = [P.sb([128, 4, 64], BF16) for _ in range(NB)]
        uu = [P.sb([128, 4, 64], F32) for _ in range(NB)]
        wT = [P.sb([64, 4, 128], BF16) for _ in range(NB)]
        qdT = [P.sb([64, 4, 128], BF16) for _ in range(NB)]
        vn = [P.sb([128, 4, 64], BF16) for _ in range(2)]
        n = 0
        for d in range(2):
            order = list(range(NT)) if d == 0 else [1, 0] + list(range(NT - 1, 1, -1))
            P.op("dve", lambda e: e.memset(S32[:], 0.0), W=[S32])
            P.op("dve", lambda e: e.memset(S16[:], 0.0), W=[S16])
            U_ = cst["U"][:, d, :]
            nU_ = cst["negU"][:, d, :]
            RU = [cst["U"], cst["negU"]]
            for tl in order:
                if TRG < 2:
                    continue
                i = n % NB
                n += 1
                ts_ = slice(tl * 128, (tl + 1) * 128)
                gd = gg[:, tl, d * 4:(d + 1) * 4]
                bd = beta[:, tl, d * 4:(d + 1) * 4]
                nbd = nbeta[:, tl, d * 4:(d + 1) * 4]
                self.tt("dve", rhs1[i][:], U_.unsqueeze(1).to_broadcast([128, 4, 128]),
                        gd.unsqueeze(2).to_broadcast([128, 4, 128]), ALU.mult, R=[cst["U"], gg], W=[rhs1[i]])
                r1f = rhs1[i][:].rearrange("p h i -> p (h i)")
                self.mm(pb[0][:, :], self.onesf[:], r1f, True, True, R=[self.onesf, rhs1[i]], W=[pb[0]])
                if TRG < 2.2:
                    continue
                sb_ = pb[7]
                self.mm(sb_[:, 0:4], U_, gd, True, True, R=RU + [gg], W=[sb_])
                self.mm(sb_[:, 4:8], obf[:], gd, True, True, R=[obf, gg], W=[sb_])
                self.mm(sb_[0:64, 8:12], ind[:, 0, :], gd, True, True, R=[ind, gg], W=[sb_])
                self.mm(sb_[0:64, 12:16], ind[:, 1, :], gd, True, True, R=[ind, gg], W=[sb_])
                sm_ = sml[i]
                self.tt("dve", sm_[:, 4:8], sb_[:, 4:8], sb_[:, 0:4], ALU.subtract, R=[sb_], W=[sm_]) if False else None
                self.cp("dve", sm_[:, 0:8], sb_[:, 0:8], R=[sb_], W=[sm_])
                self.cp("dve", sm_[:, 12:16], sb_[:, 0:4], R=[sb_], W=[sm_])
                self.stt("dve", gbm[i][:], pb[0][:, :].rearrange("p (h j) -> p h j", j=128), -1.0,
                         sm_[:, 12:16].unsqueeze(2).to_broadcast([128, 4, 128]), ALU.mult, ALU.add,
                         R=[pb[0], sm_], W=[gbm[i]])
                self.tt("dve", sm_[:, 4:8], sm_[:, 4:8], sm_[:, 0:4], ALU.subtract, R=[sm_], W=[sm_])
                self.act(sm_[:, 0:8], sm_[:, 0:8], AF.Exp, R=[sm_], W=[sm_])
                self.act(GL[i][:], sb_[0:64, 8:16], AF.Exp, R=[sb_], W=[GL[i]])
                self.tt("dve", sm_[:, 8:12], sm_[:, 0:4], bd, ALU.mult, R=[sm_, beta], W=[sm_])
                if TRG < 2.3:
                    continue
                for h in range(4):
                    hp, hl = h // 2, h % 2
                    hs = slice(hl * 64, hl * 64 + 64)
                    self.mm(pb[2][:, h * 128:(h + 1) * 128], kT[hp][hs, ts_], kT[hp][hs, ts_], True, True, R=[kT[hp]], W=[pb[2]])
                    self.mm(pb[3][:, h * 128:(h + 1) * 128], kT[hp][hs, ts_], qT[hp][hs, ts_], True, True, R=[kT[hp], qT[hp]], W=[pb[3]])
                if TRG < 2.4:
                    continue
                self.stt("dve", t1[i][:], gbm[i][:], 0.0,
                         cst["NML"][:, d, :].unsqueeze(1).to_broadcast([128, 4, 128]), ALU.min, ALU.add,
                         R=[gbm[i], cst["NML"]], W=[t1[i]])
                self.act(t1[i][:], t1[i][:], AF.Exp, R=[t1[i]], W=[t1[i]])
                self.tt("dve", t1[i][:], t1[i][:], pb[2][:, :].rearrange("p (h j) -> p h j", j=128), ALU.mult, R=[t1[i], pb[2]], W=[t1[i]])
                self.tt("pool", XT[i][:], t1[i][:], nbd.unsqueeze(2).to_broadcast([128, 4, 128]), ALU.mult, R=[t1[i], nbeta], W=[XT[i]])
                if TRG < 2.5:
                    continue
                self.stt("dve", t2[i][:], gbm[i][:], 0.0,
                         cst["NMQ"][:, d, :].unsqueeze(1).to_broadcast([128, 4, 128]), ALU.max, ALU.subtract,
                         R=[gbm[i], cst["NMQ"]], W=[t2[i]])
                self.act(t2[i][:], t2[i][:], AF.Exp, R=[t2[i]], W=[t2[i]], scale=-1.0)
                self.tt("dve", qkT[i][:], t2[i][:], pb[3][:, :].rearrange("p (h j) -> p h j", j=128), ALU.mult, R=[t2[i], pb[3]], W=[qkT[i]])
                if TRG < 3:
                    continue
                b4 = pb[4]
                p4 = b4[:].bitcast(BF16)
                for h in range(4):
                    P.op("pe", lambda e, h=h, p4=p4, i=i: e.transpose(p4[:, h * 128:(h + 1) * 128], XT[i][:, h, :], self.ident[:]),
                         R=[XT[i], self.ident], W=[b4])
                self.cp("act", X[i][:].rearrange("p h j -> p (h j)"), p4[:, 0:512], R=[b4], W=[X[i]])
                self.tt("pool", Rm[i][:], X[i][:], self.ident[:].unsqueeze(1).to_broadcast([128, 4, 128]), ALU.add,
                        R=[X[i], self.ident], W=[Rm[i]])
                Y, YT = X[i], XT[i]
                for kk in range(1, 6):
                    if kk < 5:
                        for h in range(4):
                            self.mm(pb[0][:, h * 128:(h + 1) * 128], YT[:, h, :], Y[:, h, :], True, True, R=[Y, YT], W=[pb[0]])
                    for h in range(4):
                        self.mm(pb[1][:, h * 128:(h + 1) * 128], Y[:, h, :], YT[:, h, :], True, True, R=[Y, YT], W=[pb[1]])
                    if kk < 5:
                        self.cp("act", Y[:].rearrange("p h j -> p (h j)"), pb[0][:, :], R=[pb[0]], W=[Y])
                    self.cp("dve", YT[:].rearrange("p h j -> p (h j)"), pb[1][:, :], R=[pb[1]], W=[YT])
                    for h in range(4):
                        self.mm(pb[2][:, h * 128:(h + 1) * 128], YT[:, h, :], Rm[i][:, h, :], True, True, R=[YT, Rm[i]], W=[pb[2]])
                    self.tt("dve", Rm[i][:].rearrange("p h j -> p (h j)"), Rm[i][:].rearrange("p h j -> p (h j)"), pb[2][:, :], ALU.add,
                            R=[Rm[i], pb[2]], W=[Rm[i]])
                kv = ktok[:, tl, :].rearrange("p (h d) -> p h d", d=64)
                qv = qtok[:, tl, :].rearrange("p (h d) -> p h d", d=64)
                vv = vtok[:, tl, :].rearrange("p (h d) -> p h d", d=64)
                self.tt("pool", vb[i][:], vv, bd.unsqueeze(2).to_broadcast([128, 4, 64]), ALU.mult, R=[vtok, beta], W=[vb[i]])
                self.tt("pool", kbg[i][:], kv, sm_[:, 8:12].unsqueeze(2).to_broadcast([128, 4, 64]), ALU.mult, R=[ktok, sm_], W=[kbg[i]])
                self.tt("pool", kdec[i][:], kv, sm_[:, 4:8].unsqueeze(2).to_broadcast([128, 4, 64]), ALU.mult, R=[ktok, sm_], W=[kdec[i]])
                self.tt("pool", qdec[i][:], qv, sm_[:, 0:4].unsqueeze(2).to_broadcast([128, 4, 64]), ALU.mult, R=[qtok, sm_], W=[qdec[i]])
                for h in range(4):
                    self.mm(pb[3][:, h * 64:(h + 1) * 64], Rm[i][:, h, :], vb[i][:, h, :], True, True, R=[Rm[i], vb[i]], W=[pb[3]])
                self.cp("act", uu[i][:].rearrange("p h d -> p (h d)"), pb[3][:, 0:256], R=[pb[3]], W=[uu[i]])
                for h in range(4):
                    self.mm(pb[4][0:64, h * 128:(h + 1) * 128], kbg[i][:, h, :], Rm[i][:, h, :], True, True, R=[kbg[i], Rm[i]], W=[pb[4]])
                self.cp("dve", wT[i][:].rearrange("p h j -> p (h j)"), pb[4][0:64, :], R=[pb[4]], W=[wT[i]])
                for h in range(4):
                    P.op("pe", lambda e, h=h, p4=p4, i=i: e.transpose(p4[0:64, h * 128:(h + 1) * 128], qdec[i][:, h, :], self.ident[:]),
                         R=[qdec[i], self.ident], W=[b4])
                self.cp("act", qdT[i][:].rearrange("p h j -> p (h j)"), p4[0:64, 0:512], R=[b4], W=[qdT[i]])
                for c in ([0, 1] if d == 0 else [1, 0]):
                    if TRG < 4:
                        continue
                    cs = slice(c * 64, (c + 1) * 64)
                    vn_ = vn[c]
                    for h in range(4):
                        self.mm(pb[5][:, h * 64:(h + 1) * 64], wT[i][:, h, :], S16[:, h, :], True, True, R=[wT[i], S16], W=[pb[5]])
                    self.tt("dve", vn_[cs].rearrange("p h d -> p (h d)"), uu[i][cs].rearrange("p h d -> p (h d)"), pb[5][cs, 0:256],
                            ALU.subtract, R=[uu[i], pb[5]], W=[vn_])
                    for h in range(4):
                        self.mm(pb[6][:, h * 64:(h + 1) * 64], qdT[i][:, h, :], S16[:, h, :], True, False, R=[qdT[i], S16], W=[pb[6]])
                        self.mm(pb[6][:, h * 64:(h + 1) * 64], qkT[i][cs, h, :], vn_[cs, h, :], False, True, R=[qkT[i], vn_], W=[pb[6]])
                    if d == 0:
                        self.cp("act", oacc[cs, tl, :], pb[6][cs, 0:256], R=[pb[6]], W=[oacc])
                    else:
                        self.tt("dve", oacc[cs, tl, :], oacc[cs, tl, :], pb[6][cs, 0:256], ALU.add, R=[oacc, pb[6]], W=[oacc])
                    for h in range(4):
                        self.mm(pb[7][0:64, 256 + h * 64:256 + (h + 1) * 64], kdec[i][cs, h, :], vn_[cs, h, :], True, True,
                                R=[kdec[i], vn_], W=[pb[7]])
                    self.tt("dve", S32[:], S32[:], GL[i][:, c * 4:(c + 1) * 4].unsqueeze(2).to_broadcast([64, 4, 64]), ALU.mult,
                            R=[S32, GL[i]], W=[S32])
                    self.tt("dve", S32[:].rearrange("p h d -> p (h d)"), S32[:].rearrange("p h d -> p (h d)"), pb[7][0:64, 256:512], ALU.add,
                            R=[S32, pb[7]], W=[S32])
                    self.cp("act", S16[:], S32[:], R=[S32], W=[S16])
        gdn_g = P.sb([128, 64], F32)
        P.dma("sp", gdn_g[:], self.dn_norm_g.t.ap()[l].partition_broadcast(128), R=[self.dn_norm_g], W=[gdn_g])
        gt = [P.sb([128, 256], BF16) for _ in range(2)]
        gs = [P.sb([128, 256], F32) for _ in range(2)]
        sq2 = [P.sb([128, 4, 64], F32) for _ in range(2)]
        s8 = [P.sb([128, 8], F32) for _ in range(2)]
        ob = [P.sb([128, 256], BF16) for _ in range(2)]
        tiles = list(range(0 if ctx_out else 2, NT))
        for nn, t in enumerate(tiles):
            i = nn % 2
            rs = slice(t * 128, (t + 1) * 128)
            P.dma("sp", gt[i][:], self.gated[rs, :], R=[self.gated], W=[gt[i]])
            self.act(gs[i][:], gt[i][:], AF.Silu, R=[gt[i]], W=[gs[i]])
            ov = oacc[:, t, :].rearrange("p (h d) -> p h d", d=64)
            self.tt("pool", sq2[i][:], ov, ov, ALU.mult, R=[oacc], W=[sq2[i]])
            P.op("dve", lambda e, i=i: e.reduce_sum(s8[i][:, 0:4], sq2[i][:], AX.X), R=[sq2[i]], W=[s8[i]])
            self.rsqrt_mean(s8[i][:, 4:8], s8[i][:, 0:4], 64.0, 1e-6, R=[s8[i]], W=[s8[i]])
            self.tt("dve", sq2[i][:], ov, s8[i][:, 4:8].unsqueeze(2).to_broadcast([128, 4, 64]), ALU.mult, R=[oacc, s8[i]], W=[sq2[i]])
            self.tt("dve", sq2[i][:], sq2[i][:], gdn_g[:].unsqueeze(1).to_broadcast([128, 4, 64]), ALU.mult, R=[sq2[i], gdn_g], W=[sq2[i]])
            self.tt("dve", ob[i][:], sq2[i][:].rearrange("p h d -> p (h d)"), gs[i][:], ALU.mult, R=[sq2[i], gs[i]], W=[ob[i]])
            P.dma("pool", self.mix[rs, 256:512], ob[i][:], R=[ob[i]], W=[self.mix])
        P.flush()
        P.release_mid()

def build(nlayers=DEPTH, dbg=False, stages=None):
    B = Builder(nlayers, dbg, stages)
    B.declare()
    B.stage_init()
    for l in range(nlayers):
        if stages is not None and "nomod" in stages:
            continue
        B.stage_mod(l)
        ctx_out = l < DEPTH - 1
        if B.want("inproj"):
            B.stage_inproj(l)
        if B.want("na"):
            B.stage_na(l, ctx_out)
        if B.want("diff"):
            B.stage_diff(l, ctx_out)
        if B.want("fft"):
            B.stage_fft(l, ctx_out)
        if B.want("gdn"):
            B.stage_gdn(l, ctx_out)
        if stages is not None and "inject_dn" in stages:
            inj = B.din("dn_inj", [T, 256])
            B.P.dma("pool", B.mix[:, 256:512], inj[:], R=[inj], W=[B.mix])
            B.P.flush()
        if B.want("outproj"):
            B.stage_outproj(l, ctx_out)
        if B.want("moe"):
            B.stage_route(l, ctx_out)
            B.stage_ffn(l, ctx_out)
            B.stage_combine(l, ctx_out, final=(l == nlayers - 1))
    B.P.close()
    return B


_CONST = None


def prep_shared(inputs):
    global _CONST
    if _CONST is None:
        _CONST = const_tables()
    sh = dict(_CONST)
    f = lambda k: np.ascontiguousarray(np.asarray(inputs[k], dtype=np.float32))
    for k in ("w_mod", "b_mod", "norm1_g", "norm2_g", "ft_w", "w_out", "w_router", "w_gate", "w_up", "w_down",
              "final_norm_g", "df_norm_g", "dn_norm_g", "dn_conv_w"):
        sh[k] = f(k)
    sh["w_in_p"] = np.ascontiguousarray(f("w_in")[:, :, in_proj_perm()])
    rp = f("na_rpb")
    pad = np.zeros((DEPTH, 4, 15, 128), np.float32)
    pad[..., 48:79] = rp[:, :, ::-1, ::-1]
    sh["rpbpad"] = pad
    sh["df_lambda"] = f("df_lambda").reshape(DEPTH, 128)
    sh["dn_a_log"] = f("dn_a_log").reshape(DEPTH, 8)
    sh["dn_dt_bias"] = f("dn_dt_bias").reshape(DEPTH, 8)
    return sh


def prep_sample(inputs, b):
    xc = np.concatenate([np.asarray(inputs["ctx"][b], np.float32), np.asarray(inputs["x"][b], np.float32)], 0)
    cc = np.stack([np.asarray(inputs["c"][b], np.float32), np.asarray(inputs["c_ctx"], np.float32)], 0)
    return {"xc": np.ascontiguousarray(xc), "cc": np.ascontiguousarray(cc)}


_BUILT = {}
NCORES = 4


def kernel(**inputs):
    if "B" not in _BUILT:
        _BUILT["B"] = build()
    B = _BUILT["B"]
    sh = prep_shared(inputs)
    sh = {k: v for k, v in sh.items() if k in B.inputs}
    in_maps = []
    for core in range(NCORES):
        m = dict(sh)
        m.update(prep_sample(inputs, core % 4))
        in_maps.append(m)
    res = run_bass_kernel_spmd(B.nc, in_maps, core_ids=list(range(NCORES)))
    out = np.stack([np.asarray(res.results[b]["out"], dtype=np.float32) for b in range(4)], 0)
    return out
```

```python
import math
import os
TR = int(os.environ.get('K_TR', '9'))
TRG = float(os.environ.get('K_TRG', '9'))
from contextlib import ExitStack

import numpy as np
import ml_dtypes
import concourse.bass as bass
import concourse.mybir as mybir
from concourse.bass_utils import run_bass_kernel_spmd

F32 = mybir.dt.float32
BF16 = mybir.dt.bfloat16
I32 = mybir.dt.int32
ALU = mybir.AluOpType
AF = mybir.ActivationFunctionType
AX = mybir.AxisListType

D = 1024
S = 4096
L = 256
T = S + L
NT = T // 128
DEPTH = 4
NE = 16
CAP_L = 512
CAP_C = 32
ESL = CAP_L + CAP_C
TRASH = NE * ESL
OOB = 1 << 20
GW = 64
VW = 72
EPOCH = 30000
NDMASEM = 8


class Buf:
    __slots__ = ("t", "lw", "rd", "name", "excl", "pe_rg", "pe_last")

    def __init__(self, t, name=""):
        self.t = t
        self.lw = None
        self.rd = {}
        self.name = name
        self.excl = False
        self.pe_rg = None
        self.pe_last = None

    def __getitem__(self, k):
        return self.t[k]


class Op:
    __slots__ = ("eng", "fn", "deps", "kind", "idx", "signal", "sigval")

    def __init__(self, eng, fn, deps, kind, idx):
        self.eng, self.fn, self.deps, self.kind, self.idx = eng, fn, deps, kind, idx
        self.signal = False
        self.sigval = None


class Prog:
    ENGS = ("pe", "act", "dve", "pool", "sp")

    def __init__(self, nc):
        self.nc = nc
        self.es = ExitStack()
        self.ph = ExitStack()
        self.mid = ExitStack()
        self.bufs = []
        self.ops = {e: [] for e in self.ENGS}
        self.nsig = {e: 0 for e in self.ENGS}
        self.ndma = {e: 0 for e in self.ENGS}
        self.cmp_sems = {}
        self.dma_sems = {}
        self.bar_sems = {}
        self.nbar = 0
        self.n = 0
        self.ninst = 0
        self.handles = {"pe": nc.tensor, "act": nc.scalar, "dve": nc.vector, "pool": nc.gpsimd, "sp": nc.sync}

    def _reg(self, t, name):
        b = Buf(t, name)
        self.bufs.append(b)
        return b

    def sb(self, shape, dt, persist=False):
        self.n += 1
        name = f"sb{self.n}"
        st = self.mid if persist == "mid" else (self.es if persist else self.ph)
        return self._reg(st.enter_context(self.nc.sbuf_tensor(name, list(shape), dt)), name)

    def release_mid(self):
        self.mid.close()
        self.mid = ExitStack()

    def ps(self, shape, dt=F32):
        self.n += 1
        name = f"ps{self.n}"
        b = self._reg(self.es.enter_context(self.nc.psum_tensor(name, list(shape), dt)), name)
        b.excl = True
        return b

    def dram(self, name, shape, dt, kind="Internal"):
        return self._reg(self.nc.dram_tensor(name, list(shape), dt, kind=kind), name)

    def view(self, name=""):
        return self._reg(None, name)

    def op(self, eng, fn, R=(), W=(), kind="cmp", rg=None):
        lst = self.ops[eng]
        idx = len(lst)
        deps = set()
        W = list(W) + [b for b in R if b.excl]
        R = [b for b in R if not b.excl]
        extra = set()
        if eng == "pe":
            if rg is None:
                rg = frozenset((0, 1, 2, 3))
            for b in W:
                if b.excl:
                    if b.pe_rg is not None and b.pe_last is not None and not (b.pe_rg & rg):
                        extra.add(b.pe_last)
                    b.pe_rg = rg
                    b.pe_last = ("pe", idx)
        for b in R:
            if b.lw is not None:
                deps.add(b.lw)
        for b in W:
            if b.lw is not None:
                deps.add(b.lw)
            for e2, i2 in b.rd.items():
                deps.add((e2, i2))
        if eng == "pe":
            deps = {d for d in deps if d[0] != "pe"}
        deps |= extra
        o = Op(eng, fn, deps, kind, idx)
        lst.append(o)
        for b in R:
            b.rd[eng] = idx
        for b in W:
            b.lw = (eng, idx)
            b.rd = {}
        return o

    def dma(self, eng, out, in_, R=(), W=(), **kw):
        return self.op(eng, lambda e: e.dma_start(out=out, in_=in_, **kw), R=R, W=W, kind="dma")

    def _csem(self, e, ep):
        if (e, ep) not in self.cmp_sems:
            self.cmp_sems[(e, ep)] = self.es.enter_context(self.nc.semaphore(f"c_{e}_{ep}"))
        return self.cmp_sems[(e, ep)]

    def _dsem(self, e, j):
        if (e, j) not in self.dma_sems:
            self.dma_sems[(e, j)] = self.es.enter_context(self.nc.semaphore(f"d_{e}_{j}"))
        return self.dma_sems[(e, j)]

    def _bsem(self, e):
        if e not in self.bar_sems:
            self.bar_sems[e] = self.es.enter_context(self.nc.semaphore(f"b_{e}"))
        return self.bar_sems[e]

    def flush(self):
        nc = self.nc
        ops = self.ops
        for e in self.ENGS:
            for o in ops[e]:
                for (e2, i2) in o.deps:
                    ops[e2][i2].signal = True
            for o in reversed(ops[e]):
                if o.kind == "cmp":
                    o.signal = True
                    break
        for e in self.ENGS:
            for o in ops[e]:
                if o.kind == "dma":
                    o.sigval = ("dma", self.ndma[e])
                    self.ndma[e] += 1
                elif o.signal:
                    o.sigval = ("cmp", self.nsig[e])
                    self.nsig[e] += 1
        self.nbar += 1
        nbar = self.nbar

        def run(ename, eh):
            waited_cmp = {}
            waited_dma = set()
            last_cmp = None
            my_dmas = []
            for o in ops[ename]:
                best = {}
                for (e2, i2) in o.deps:
                    d = ops[e2][i2]
                    kind, n = d.sigval
                    if kind == "dma":
                        if (e2, n) in waited_dma:
                            continue
                        waited_dma.add((e2, n))
                        eh.wait_ge(self._dsem(e2, n % NDMASEM), 16 * (n // NDMASEM + 1))
                        self.ninst += 1
                    else:
                        if waited_cmp.get(e2, -1) >= n:
                            continue
                        if best.get(e2, -1) < n:
                            best[e2] = n
                for e2, n in best.items():
                    waited_cmp[e2] = n
                    eh.wait_ge(self._csem(e2, n // EPOCH), (n % EPOCH) + 1)
                    self.ninst += 1
                if o.kind == "dma":
                    n = o.sigval[1]
                    if n >= NDMASEM and (ename, n - NDMASEM) not in waited_dma:
                        eh.wait_ge(self._dsem(ename, n % NDMASEM), 16 * (n // NDMASEM))
                        waited_dma.add((ename, n - NDMASEM))
                    ins = o.fn(eh)
                    ins.then_inc(self._dsem(ename, n % NDMASEM), 16)
                    my_dmas.append(n)
                else:
                    ins = o.fn(eh)
                    if o.signal:
                        n = o.sigval[1]
                        ins.then_inc(self._csem(ename, n // EPOCH), 1)
                        last_cmp = n
                self.ninst += 1
            for n in my_dmas[-NDMASEM:]:
                if (ename, n) not in waited_dma:
                    eh.wait_ge(self._dsem(ename, n % NDMASEM), 16 * (n // NDMASEM + 1))
            if last_cmp is not None and waited_cmp.get(ename, -1) < last_cmp:
                eh.wait_ge(self._csem(ename, last_cmp // EPOCH), (last_cmp % EPOCH) + 1)
            eh.sem_inc(self._bsem(ename), 1)
            for e2 in self.ENGS:
                if e2 != ename:
                    eh.wait_ge(self._bsem(e2), nbar)

        with nc.Block() as block:
            @block.sync
            def _(e):
                run("sp", e)

            @block.scalar
            def _(e):
                run("act", e)

            @block.vector
            def _(e):
                run("dve", e)

            @block.tensor
            def _(e):
                run("pe", e)

            @block.gpsimd
            def _(e):
                run("pool", e)
        self.ops = {e: [] for e in self.ENGS}
        for b in self.bufs:
            b.lw = None
            b.rd = {}
            b.pe_rg = None
            b.pe_last = None
        self.ph.close()
        self.ph = ExitStack()

    def close(self):
        self.ph.close()
        self.es.close()


IN_OFF = {"naq": 0, "nak": 256, "nav": 512, "dn": 768, "dna": 1536, "dnb": 1544, "dng": 1552,
          "dfq": 1808, "dfk": 2064, "dfv": 2320, "ftu": 2576}


def _swap_cols(base):
    idx = []
    for hm in range(8):
        for f in range(32):
            idx.append(base + hm * 32 + (f ^ 1))
    return idx


def in_proj_perm():
    p = []
    p += list(range(IN_OFF["naq"], IN_OFF["naq"] + 256))
    p += list(range(IN_OFF["nak"], IN_OFF["nak"] + 256))
    p += list(range(IN_OFF["dn"], IN_OFF["dn"] + 768))
    p += list(range(IN_OFF["dfq"], IN_OFF["dfq"] + 256))
    p += _swap_cols(IN_OFF["dfq"])
    p += list(range(IN_OFF["dfk"], IN_OFF["dfk"] + 256))
    p += _swap_cols(IN_OFF["dfk"])
    p += list(range(IN_OFF["ftu"], IN_OFF["ftu"] + 256))
    assert len(p) == 2560
    p += list(range(IN_OFF["nav"], IN_OFF["nav"] + 256))
    p += list(range(IN_OFF["dfv"], IN_OFF["dfv"] + 256))
    p += list(range(IN_OFF["dng"], IN_OFF["dng"] + 256))
    p += list(range(IN_OFF["dna"], IN_OFF["dna"] + 16))
    assert len(p) == 3344
    return np.array(p)


NWIN = 3344
A_PLAIN = {0: 0, 1: 128, 2: 256, 3: 384, 4: 512, 5: 640, 6: 768, 7: 896, 8: 1024, 9: 1152, 18: 1792, 19: 1920}
A_ROPE = {10: (12, 1280), 11: (13, 1408), 14: (16, 1536), 15: (17, 1664)}


def const_tables():
    c = {}
    c["ident"] = np.eye(128, dtype=np.float32)
    t = np.arange(S)
    pos = np.stack([t // GW, t % GW], -1).astype(np.float32)
    inv = (10000.0 ** (-np.arange(8, dtype=np.float32) / 8)).astype(np.float32)
    ang = (pos[:, :, None] * inv).reshape(S, 16)
    cosf = np.repeat(np.cos(ang), 2, axis=1)
    sinf = np.repeat(np.sin(ang), 2, axis=1)
    sgn = np.tile(np.array([-1.0, 1.0], np.float32), 16)
    sinf = sinf * sgn
    cosT = np.ones((32, T), np.float32)
    sinT = np.zeros((32, T), np.float32)
    cosT[:, L:] = cosf.T
    sinT[:, L:] = sinf.T
    c["cosT"] = np.tile(cosT, (4, 1)).astype(np.float32)
    c["sinT"] = np.tile(sinT, (4, 1)).astype(np.float32)
    cq = np.arange(GW)
    c0 = np.clip(cq - 8, 0, GW - 16)
    inwin = (cq[None, :] >= c0[:, None]) & (cq[None, :] < c0[:, None] + 16)
    c["colwinT"] = np.tile(inwin.T.astype(np.float32), (2, 1))
    def dft(n, scale):
        k = np.arange(n)
        ph = (np.outer(k, k) % n).astype(np.float64) * (2 * np.pi / n)
        return (np.cos(ph) * scale), (np.sin(ph) * scale)
    c64, s64 = dft(64, 1.0 / 8)
    bdc = np.zeros((128, 128)); bds = np.zeros((128, 128))
    for i in range(2):
        bdc[i * 64:(i + 1) * 64, i * 64:(i + 1) * 64] = c64
        bds[i * 64:(i + 1) * 64, i * 64:(i + 1) * 64] = s64
    c["bdc"] = bdc.astype(np.float32)
    c["bds"] = bds.astype(np.float32)
    cS, sS = dft(S, 1.0 / 64)
    def tile_tab(m, n):
        nt = n // 128
        return np.ascontiguousarray(m.reshape(nt, 128, nt, 128).transpose(2, 1, 0, 3)).astype(ml_dtypes.bfloat16)
    c["dftc"] = tile_tab(cS, S)
    c["dfts"] = tile_tab(sS, S)
    cL, sL = dft(L, 1.0 / 16)
    c["dftc_c"] = tile_tab(cL, L)
    c["dfts_c"] = tile_tab(sL, L)
    c["tri"] = np.triu(np.ones((128, 128), np.float32))
    c["ones"] = np.ones((128, 128), np.float32)
    base = np.zeros((128, NT, NE), np.float32)
    for e in range(NE):
        base[:, :2, e] = e * ESL + CAP_L
        base[:, 2:, e] = e * ESL
    c["slotbase"] = base.reshape(128, NT * NE) - OOB
    ii = np.arange(128)
    same = (ii[:, None] // 64) == (ii[None, :] // 64)
    Uf = (same & (ii[:, None] <= ii[None, :])).astype(np.float32)
    Ub = (same & (ii[:, None] >= ii[None, :])).astype(np.float32)
    NEG = -30000.0
    g = {}
    g["U"] = np.stack([Uf, Ub])
    g["negU"] = -g["U"]
    nml_f = np.where(same & (ii[:, None] > ii[None, :]), 0.0, NEG)
    nml_b = np.where(same & (ii[:, None] < ii[None, :]), 0.0, NEG)
    nmq_f = np.where(same & (ii[None, :] >= ii[:, None]), 0.0, NEG)
    nmq_b = np.where(same & (ii[None, :] <= ii[:, None]), 0.0, NEG)
    g["NML"] = np.stack([nml_f, nml_b])
    g["NMQ"] = np.stack([nmq_f, nmq_b])
    ind = np.zeros((2, 128, 64), np.float32)
    ind[0, :64, :] = 1.0
    ind[1, 64:, :] = 1.0
    c["gdn_U"] = g["U"].astype(np.float32)
    c["gdn_negU"] = g["negU"].astype(np.float32)
    c["gdn_NML"] = g["NML"].astype(np.float32)
    c["gdn_NMQ"] = g["NMQ"].astype(np.float32)
    c["gdn_ind"] = ind
    c["gdn_ob"] = same.astype(np.float32)
    return c


class Builder:
    def __init__(self, nlayers=DEPTH, dbg=False, stages=None):
        self.nlayers = nlayers
        self.dbg = dbg
        self.stages = stages
        self.nc = bass.Bass("TRN2", target_bir_lowering=False)
        self.P = Prog(self.nc)
        self.inputs = {}

    def want(self, s):
        return self.stages is None or s in self.stages

    def din(self, name, shape, dt=F32):
        b = self.P.dram(name, shape, dt, kind="ExternalInput")
        self.inputs[name] = b
        return b

    def dscr(self, name, shape, dt, out=False):
        return self.P.dram(name, shape, dt, kind="ExternalOutput" if (out or self.dbg) else "Internal")

    def declare(self):
        P = self.P
        nl = DEPTH
        self.xc = self.din("xc", [T, D])
        self.cc = self.din("cc", [2, D])
        self.w_mod = self.din("w_mod", [nl, D, 6 * D])
        self.b_mod = self.din("b_mod", [nl, 6 * D])
        self.norm1_g = self.din("norm1_g", [nl, D])
        self.norm2_g = self.din("norm2_g", [nl, D])
        self.w_in = self.din("w_in_p", [nl, D, NWIN])
        self.rpbpad = self.din("rpbpad", [nl, 4, 15, 128])
        self.ft_w = self.din("ft_w", [nl, 256, 256])
        self.w_out = self.din("w_out", [nl, D, D])
        self.w_router = self.din("w_router", [nl, D, NE])
        if self.want("moe"):
            self.w_gate = self.din("w_gate", [nl, NE, D, 2 * D])
            self.w_up = self.din("w_up", [nl, NE, D, 2 * D])
            self.w_down = self.din("w_down", [nl, NE, 2 * D, D])
        self.final_g = self.din("final_norm_g", [D])
        self.df_lambda = self.din("df_lambda", [nl, 128])
        self.df_norm_g = self.din("df_norm_g", [nl, 64])
        self.dn_norm_g = self.din("dn_norm_g", [nl, 64])
        self.dn_conv_w = self.din("dn_conv_w", [nl, 5, 768])
        self.dn_a_log = self.din("dn_a_log", [nl, 8])
        self.dn_dt_bias = self.din("dn_dt_bias", [nl, 8])
        self.c_ident = self.din("ident", [128, 128])
        self.c_cosT = self.din("cosT", [128, T])
        self.c_sinT = self.din("sinT", [128, T])
        self.c_colwinT = self.din("colwinT", [128, 64])
        self.c_bdc = self.din("bdc", [128, 128])
        self.c_bds = self.din("bds", [128, 128])
        if self.want("fft"):
            self.c_dftc = self.din("dftc", [32, 128, 32, 128], BF16)
            self.c_dfts = self.din("dfts", [32, 128, 32, 128], BF16)
        self.c_dftc_c = self.din("dftc_c", [2, 128, 2, 128], BF16)
        self.c_dfts_c = self.din("dfts_c", [2, 128, 2, 128], BF16)
        self.c_tri = self.din("tri", [128, 128])
        self.c_ones = self.din("ones", [128, 128])
        self.c_slotbase = self.din("slotbase", [128, NT * NE])
        self.c_gU = self.din("gdn_U", [2, 128, 128])
        self.c_gnegU = self.din("gdn_negU", [2, 128, 128])
        self.c_gNML = self.din("gdn_NML", [2, 128, 128])
        self.c_gNMQ = self.din("gdn_NMQ", [2, 128, 128])
        self.c_gind = self.din("gdn_ind", [2, 128, 64])
        self.c_gob = self.din("gdn_ob", [128, 128])
        self.out = P.dram("out", [S, D], F32, kind="ExternalOutput")
        self.xres = self.dscr("xres", [T, D], F32)
        self.projT = self.dscr("projT", [2048, T], BF16)
        self.navd = self.dscr("navd", [T, 4 * VW], BF16)
        self.dfvd = self.dscr("dfvd", [T, 4 * VW], BF16)
        self.gated = self.dscr("gated", [T, 256], BF16)
        self.abd = self.dscr("abd", [T, 16], F32)
        self.mix = self.dscr("mix", [T, D], BF16)
        self.h2d = self.dscr("h2d", [T, D], BF16)
        self.xg = self.dscr("xg", [TRASH + 128, D], BF16)
        self.ybuf = self.dscr("ybuf", [TRASH + 128, D], F32)
        self.rpbz = self.dscr("rpbz", [60 * 64 * 129 + 256], F32)
        self.ident = P.sb([128, 128], BF16, persist=True)
        self.identf = P.sb([128, 128], F32, persist=True)
        self.onesf = P.sb([128, 128], F32, persist=True)
        self.trif = P.sb([128, 128], F32, persist=True)
        self.mT = P.sb([128, 48, 2], F32, persist=True)
        self.gs1T = P.sb([128, 8, 2], F32, persist=True)
        self.g1T = P.sb([128, 8], F32, persist=True)
        self.g2T = P.sb([128, 8], F32, persist=True)
        self.aff = P.sb([128, NT, NE], F32, persist=True)
        self.desti = P.sb([128, NT, NE], I32, persist=True)
        self.gatev = P.sb([128, NT, NE], F32, persist=True)
        self.pb = [P.ps([128, 512], F32) for _ in range(8)]

    def stage_init(self):
        P = self.P
        P.dma("pool", self.ident[:], self.c_ident[:], R=[self.c_ident], W=[self.ident])
        P.dma("sp", self.identf[:], self.c_ident[:], R=[self.c_ident], W=[self.identf])
        P.dma("sp", self.onesf[:], self.c_ones[:], R=[self.c_ones], W=[self.onesf])
        P.dma("sp", self.trif[:], self.c_tri[:], R=[self.c_tri], W=[self.trif])
        for i in range(2):
            r0 = i * (T // 2)
            P.dma("sp", self.xres[r0:r0 + T // 2, :], self.xc[r0:r0 + T // 2, :], R=[self.xc], W=[self.xres])
        z = P.sb([128, D], F32)
        P.op("dve", lambda e: e.memset(z[:], 0.0), W=[z])
        P.dma("sp", self.ybuf[TRASH:TRASH + 128, :], z[:], R=[z], W=[self.ybuf])
        P.flush()

    def stage_mod(self, l):
        P = self.P
        pb = self.pb
        sT = P.sb([128, 8, 2], F32)
        for r in range(2):
            P.dma("sp", sT[:, :, r], self.cc.t.ap()[r].rearrange("(c p) -> p c", p=128), R=[self.cc], W=[sT],
                  allow_slow_non_contiguous=True)
        P.op("act", lambda e: e.activation(sT[:], sT[:], AF.Silu), R=[sT], W=[sT])
        bT = P.sb([128, 48], F32)
        P.dma("sp", bT[:], self.b_mod.t.ap()[l].rearrange("(c p) -> p c", p=128), R=[self.b_mod], W=[bT],
              allow_slow_non_contiguous=True)
        P.dma("sp", self.g1T[:], self.norm1_g.t.ap()[l].rearrange("(c p) -> p c", p=128), R=[self.norm1_g],
              W=[self.g1T], allow_slow_non_contiguous=True)
        P.dma("sp", self.g2T[:], self.norm2_g.t.ap()[l].rearrange("(c p) -> p c", p=128), R=[self.norm2_g],
              W=[self.g2T], allow_slow_non_contiguous=True)
        wt = [P.sb([128, 8, 512], F32) for _ in range(2)]
        acc = pb[0]
        for n in range(12):
            w = wt[n % 2]
            P.dma("sp", w[:], self.w_mod.t.ap()[l, :, n * 512:(n + 1) * 512].rearrange("(k p) n -> p k n", p=128),
                  R=[self.w_mod], W=[w])
            for j in range(4):
                col = n * 4 + j
                for k in range(8):
                    P.op("pe", lambda e, w=w, j=j, k=k, col=col: e.matmul(
                        acc[:, col * 2:col * 2 + 2], w[:, k, j * 128:(j + 1) * 128], sT[:, k, :],
                        start=(k == 0), stop=(k == 7)), R=[w, sT], W=[acc])
        P.op("dve", lambda e: e.tensor_tensor(
            self.mT[:], acc[:, 0:96].rearrange("p (c r) -> p c r", r=2),
            bT[:].unsqueeze(2).to_broadcast([128, 48, 2]), ALU.add), R=[acc, bT], W=[self.mT])
        P.op("dve", lambda e: e.tensor_scalar(self.gs1T[:], self.mT[:, 8:16, :], 1.0, None, ALU.add),
             R=[self.mT], W=[self.gs1T])
        P.op("dve", lambda e: e.tensor_tensor(self.gs1T[:], self.gs1T[:],
                                              self.g1T[:].unsqueeze(2).to_broadcast([128, 8, 2]), ALU.mult),
             R=[self.gs1T, self.g1T], W=[self.gs1T])
        if self.dbg:
            dm = self.dscr(f"dbg_mT{l}", [128, 96], F32)
            P.dma("sp", dm[:], self.mT[:].rearrange("p c r -> p (c r)"), R=[self.mT], W=[dm])
        P.flush()

    def bcast_vec(self, dst, srcT_fn, R):
        P = self.P
        dg = P.sb([128, 128], F32)
        for c in range(8):
            bank = self.pb[4 + (c % 2)]
            P.op("dve", lambda e, c=c: e.tensor_scalar(dg[:], self.identf[:], srcT_fn(c), None, ALU.mult),
                 R=[self.identf] + R, W=[dg])
            P.op("pe", lambda e, bank=bank: e.matmul(bank[:, 0:128], self.onesf[:], dg[:], start=True, stop=True),
                 R=[dg, self.onesf], W=[bank])
            P.op("act", lambda e, c=c, bank=bank: e.copy(dst[:, c * 128:(c + 1) * 128], bank[:, 0:128]),
                 R=[bank], W=[dst])

    def stage_inproj(self, l):
        P = self.P
        pb = self.pb
        win = P.sb([128, 8, NWIN], BF16)
        for k in range(8):
            P.dma("pool", win[:, k, :], self.w_in.t.ap()[l, k * 128:(k + 1) * 128, :], R=[self.w_in], W=[win])
        xt = [P.sb([128, D], F32) for _ in range(4)]
        xn = [P.sb([128, D], BF16) for _ in range(4)]
        junk = P.sb([128, D], BF16)
        ss = [P.sb([128, 2], F32) for _ in range(4)]
        hT = P.sb([128, 8, 512], BF16)
        cosb = P.sb([128, 512], F32)
        sinb = P.sb([128, 512], F32)
        stg = [P.sb([128, 512], BF16) for _ in range(4)]
        r1 = [P.sb([128, 512], F32) for _ in range(2)]
        r2 = [P.sb([128, 512], F32) for _ in range(2)]
        vst = [P.sb([128, 4, VW], BF16) for _ in range(2)]
        fst = [P.sb([128, 4, VW], BF16) for _ in range(2)]
        gst = [P.sb([128, 256], BF16) for _ in range(2)]
        ast = [P.sb([128, 16], F32) for _ in range(2)]
        for b_ in vst + fst:
            P.op("dve", lambda e, b_=b_: e.memset(b_[:], 1.0), W=[b_])
        blocks = [(0, 2, 1)] + [(2 + 4 * i, 4, 0) for i in range(8)]
        nst = 0
        for (t0, nt, s) in blocks:
            ntok = nt * 128
            c0 = t0 * 128
            P.dma("sp", cosb[:, :ntok], self.c_cosT[:, c0:c0 + ntok], R=[self.c_cosT], W=[cosb])
            P.dma("sp", sinb[:, :ntok], self.c_sinT[:, c0:c0 + ntok], R=[self.c_sinT], W=[sinb])
            for i in range(nt):
                t = t0 + i
                P.dma("sp", xt[i][:], self.xres[t * 128:(t + 1) * 128, :], R=[self.xres], W=[xt[i]])
                P.op("dve", lambda e, i=i: e.memset(ss[i][:], 0.0), W=[ss[i]])
                P.op("act", lambda e, i=i: e.activation(junk[:], xt[i][:], AF.Square, accum_out=ss[i][:, 0:1]),
                     R=[xt[i], ss[i]], W=[junk, ss[i]])
                P.op("act", lambda e, i=i: e.activation(ss[i][:, 1:2], ss[i][:, 0:1], AF.Sqrt, bias=1e-6, scale=1.0 / D),
                     R=[ss[i]], W=[ss[i]])
                P.op("dve", lambda e, i=i: e.reciprocal(ss[i][:, 1:2], ss[i][:, 1:2]), R=[ss[i]], W=[ss[i]])
                P.op("act", lambda e, i=i: e.activation(xn[i][:], xt[i][:], AF.Copy, scale=ss[i][:, 1:2]),
                     R=[xt[i], ss[i]], W=[xn[i]])
            if TR < 2:
                continue
            for c in range(8):
                bank = pb[c % 2]
                pT = bank[:].bitcast(BF16)
                for i in range(nt):
                    P.op("pe", lambda e, i=i, c=c, pT=pT: e.transpose(
                        pT[:, i * 128:(i + 1) * 128], xn[i][:, c * 128:(c + 1) * 128], self.ident[:]),
                        R=[xn[i], self.ident], W=[bank])
                P.op("act", lambda e, c=c, pT=pT, s=s, ntok=ntok: e.activation(
                    hT[:, c, :ntok], pT[:, :ntok], AF.Identity, scale=self.gs1T[:, c, s:s + 1],
                    bias=self.mT[:, c, s:s + 1]), R=[bank, self.gs1T, self.mT], W=[hT])
            if TR < 3:
                continue
            def mm_chunk(j, bank):
                for k in range(8):
                    P.op("pe", lambda e, j=j, k=k, bank=bank: e.matmul(
                        bank[:, :ntok], win[:, k, j * 128:(j + 1) * 128], hT[:, k, :ntok],
                        start=(k == 0), stop=(k == 7)), R=[win, hT], W=[bank])
            for j in range(20):
                if j in A_PLAIN:
                    bank = pb[2 + (j % 2)]
                    mm_chunk(j, bank)
                    st = stg[nst % 4]
                    nst += 1
                    if j % 2 == 0:
                        P.op("act", lambda e, st=st, bank=bank: e.copy(st[:, :ntok], bank[:, :ntok]), R=[bank], W=[st])
                    else:
                        P.op("dve", lambda e, st=st, bank=bank: e.tensor_copy(st[:, :ntok], bank[:, :ntok]),
                             R=[bank], W=[st])
                    r0 = A_PLAIN[j]
                    P.dma("pool", self.projT[r0:r0 + 128, c0:c0 + ntok], st[:, :ntok], R=[st], W=[self.projT])
                elif j in A_ROPE:
                    j2, r0 = A_ROPE[j]
                    b1, b2 = pb[4 + (j % 2) * 2], pb[5 + (j % 2) * 2]
                    mm_chunk(j, b1)
                    mm_chunk(j2, b2)
                    a1, a2 = r1[j % 2], r2[j % 2]
                    P.op("dve", lambda e, a1=a1, b1=b1: e.tensor_tensor(a1[:, :ntok], b1[:, :ntok], cosb[:, :ntok], ALU.mult),
                         R=[b1, cosb], W=[a1])
                    P.op("dve", lambda e, a2=a2, b2=b2: e.tensor_tensor(a2[:, :ntok], b2[:, :ntok], sinb[:, :ntok], ALU.mult),
                         R=[b2, sinb], W=[a2])
                    st = stg[nst % 4]
                    nst += 1
                    P.op("pool", lambda e, st=st, a1=a1, a2=a2: e.tensor_tensor(st[:, :ntok], a1[:, :ntok], a2[:, :ntok], ALU.add),
                         R=[a1, a2], W=[st])
                    P.dma("pool", self.projT[r0:r0 + 128, c0:c0 + ntok], st[:, :ntok], R=[st], W=[self.projT])
            if TR < 4:
                continue
            for i in range(nt):
                t = t0 + i
                b1, b2 = pb[2 + (i % 2)], pb[4 + (i % 2)]
                for k in range(8):
                    P.op("pe", lambda e, i=i, k=k, b1=b1: e.matmul(
                        b1[:, :], hT[:, k, i * 128:(i + 1) * 128], win[:, k, 2560:3072],
                        start=(k == 0), stop=(k == 7)), R=[win, hT], W=[b1])
                for k in range(8):
                    P.op("pe", lambda e, i=i, k=k, b2=b2: e.matmul(
                        b2[:, :272], hT[:, k, i * 128:(i + 1) * 128], win[:, k, 3072:3344],
                        start=(k == 0), stop=(k == 7)), R=[win, hT], W=[b2])
                v, f, g, a = vst[i % 2], fst[i % 2], gst[i % 2], ast[i % 2]
                P.op("act", lambda e, v=v, b1=b1: e.copy(v[:, :, 0:64], b1[:, 0:256].rearrange("p (h d) -> p h d", d=64)),
                     R=[b1], W=[v])
                P.op("dve", lambda e, f=f, b1=b1: e.tensor_copy(f[:, :, 0:64], b1[:, 256:512].rearrange("p (h d) -> p h d", d=64)),
                     R=[b1], W=[f])
                P.op("act", lambda e, g=g, b2=b2: e.copy(g[:], b2[:, 0:256]), R=[b2], W=[g])
                P.op("dve", lambda e, a=a, b2=b2: e.tensor_copy(a[:], b2[:, 256:272]), R=[b2], W=[a])
                rs = slice(t * 128, (t + 1) * 128)
                if TR < 5:
                    continue
                P.dma("pool", self.navd[rs, :], v[:].rearrange("p h d -> p (h d)"), R=[v], W=[self.navd])
                P.dma("pool", self.dfvd[rs, :], f[:].rearrange("p h d -> p (h d)"), R=[f], W=[self.dfvd])
                P.dma("pool", self.gated[rs, :], g[:], R=[g], W=[self.gated])
                P.dma("pool", self.abd[rs, :], a[:], R=[a], W=[self.abd])
        P.flush()


    def oob_reg(self, e):
        if getattr(self, "_oob", None) is None:
            self._oob = e.to_reg(TRASH - 1)
        return self._oob

    def mm(self, out, lhsT, rhs, start, stop, R, W, **kw):
        bp = lhsT.base_partition()
        kk = lhsT.shape[0]
        rg = frozenset(range(bp // 32, (bp + kk - 1) // 32 + 1))
        self.P.op("pe", lambda e: e.matmul(out, lhsT, rhs, start=start, stop=stop, **kw), R=R, W=W, rg=rg)

    def act(self, out, in_, func, R, W, **kw):
        self.P.op("act", lambda e: e.activation(out, in_, func, **kw), R=R, W=W)

    def tt(self, eng, out, in0, in1, op, R, W):
        self.P.op(eng, lambda e: e.tensor_tensor(out, in0, in1, op), R=R, W=W)

    def ts(self, eng, out, in0, s1, s2, op0, op1, R, W):
        if op1 is None:
            self.P.op(eng, lambda e: e.tensor_scalar(out, in0, s1, s2, op0), R=R, W=W)
        else:
            self.P.op(eng, lambda e: e.tensor_scalar(out, in0, s1, s2, op0, op1), R=R, W=W)

    def stt(self, eng, out, in0, scalar, in1, op0, op1, R, W):
        self.P.op(eng, lambda e: e.scalar_tensor_tensor(out, in0, scalar, in1, op0, op1), R=R, W=W)

    def cp(self, eng, out, in_, R, W):
        if eng == "act":
            self.P.op("act", lambda e: e.copy(out, in_), R=R, W=W)
        else:
            self.P.op(eng, lambda e: e.tensor_copy(out, in_), R=R, W=W)

    def rsqrt_mean(self, out, in_, n, eps, R, W):
        self.act(out, in_, AF.Sqrt, R=R, W=W, bias=eps, scale=1.0 / n)
        self.P.op("dve", lambda e: e.reciprocal(out, out), R=W, W=W)

    def stage_na(self, l, ctx_out):
        P = self.P
        pb = self.pb
        zdst = self.rpbz.t.ap()[0:60 * 8256].rearrange("(a b c) -> a b c", b=64, c=129)[:, :, 0:128]
        zsrc = self.rpbpad.t.ap()[l].rearrange("h j i -> (h j) i").unsqueeze(1).to_broadcast([60, 64, 128])
        P.dma("sp", zdst, zsrc, R=[self.rpbpad], W=[self.rpbz])
        zv = self.rpbz.t.ap()[63:63 + 60 * 8256].rearrange("(a r) -> a r", r=8256)[:, 0:8192] \
            .rearrange("a (k q) -> k a q", q=128)[:, :, 0:64]
        bank = P.sb([128, 60, 64], F32)
        colw = P.sb([128, 64], F32)
        P.dma("sp", colw[:], self.c_colwinT[:], R=[self.c_colwinT], W=[colw])
        P.dma("sp", bank[0:64], zv, R=[self.rpbz], W=[bank])
        P.dma("sp", bank[64:128], zv, R=[self.rpbz], W=[bank])
        self.act(bank[:], bank[:], AF.Exp, R=[bank], W=[bank])
        self.tt("dve", bank[:], bank[:], colw[:].unsqueeze(1).to_broadcast([128, 60, 64]), ALU.mult,
                R=[bank, colw], W=[bank])
        qT = [P.sb([128, T], BF16) for _ in range(2)]
        kT = [P.sb([128, T], BF16) for _ in range(2)]
        for hp in range(2):
            P.dma("sp", qT[hp][:], self.projT[hp * 128:(hp + 1) * 128, :], R=[self.projT], W=[qT[hp]])
            P.dma("sp", kT[hp][:], self.projT[256 + hp * 128:256 + (hp + 1) * 128, :], R=[self.projT], W=[kT[hp]])
        vsb = P.sb([128, NT, 4 * VW], BF16)
        P.dma("sp", vsb[:], self.navd.t.ap().rearrange("(n p) c -> p n c", p=128), R=[self.navd], W=[vsb])
        E = [P.sb([128, 8, 128], F32) for _ in range(2)]
        PT = [P.sb([128, 8, 128], BF16) for _ in range(2)]
        mst = [P.sb([128, 256], BF16) for _ in range(2)]
        rd = [P.sb([128, 1], F32) for _ in range(2)]

        def start_row(r):
            return min(max(r - 4, 0), 56)

        n = 0
        pend_tail = None
        qtiles = ([(-2, 0), (-1, 1)] if ctx_out else []) + [(m, 2 + m) for m in range(32)]
        for (m, qt) in qtiles:
            ms = mst[qt % 2]
            for h in range(4):
                hp, hl = h // 2, h % 2
                psl = slice(hl * 64, hl * 64 + 64)
                if m < 0:
                    chunks = [0, 1]
                else:
                    c_lo = start_row(2 * m) // 2
                    c_hi = (start_row(2 * m + 1) + 7) // 2
                    chunks = [0, 1] + [2 + c for c in range(c_lo, c_hi + 1)]
                nch = len(chunks)
                bA, bB = pb[2 * (n % 2)], pb[2 * (n % 2) + 1]
                Eb, PTb = E[n % 2], PT[n % 2]
                for ci, kt in enumerate(chunks):
                    bk = bA if ci < 4 else bB
                    off = (ci % 4) * 128
                    self.mm(bk[:, off:off + 128], kT[hp][psl, kt * 128:(kt + 1) * 128], qT[hp][psl, qt * 128:(qt + 1) * 128],
                            True, True, R=[kT[hp], qT[hp]], W=[bk])
                self.act(PTb[:, 0:2, :], bA[:, 0:256].rearrange("p (c q) -> p c q", q=128), AF.Exp,
                         R=[bA], W=[PTb], scale=0.125)
                if nch > 2:
                    na = min(nch, 4) - 2
                    self.act(Eb[:, 2:2 + na, :], bA[:, 256:256 + na * 128].rearrange("p (c q) -> p c q", q=128), AF.Exp,
                             R=[bA], W=[Eb], scale=0.125)
                if nch > 4:
                    nb = nch - 4
                    self.act(Eb[:, 4:4 + nb, :], bB[:, 0:nb * 128].rearrange("p (c q) -> p c q", q=128), AF.Exp,
                             R=[bB], W=[Eb], scale=0.125)
                for ci in range(2, nch):
                    c = chunks[ci] - 2
                    for kr in range(2):
                        krow = 2 * c + kr
                        ks = slice(kr * 64, kr * 64 + 64)
                        jj = []
                        for rr in range(2):
                            qrow = 2 * m + rr
                            st = start_row(qrow)
                            if st <= krow < st + 8:
                                jj.append(14 - (krow - qrow + 7))
                            else:
                                jj.append(None)
                        eng = "dve" if (ci + kr) % 2 == 0 else "pool"
                        if jj[0] is not None and jj[1] is not None:
                            assert jj[1] == jj[0] + 1
                            j0 = h * 15 + jj[0]
                            self.tt(eng, PTb[ks, ci, :].rearrange("p (r q) -> p r q", q=64),
                                    Eb[ks, ci, :].rearrange("p (r q) -> p r q", q=64),
                                    bank[ks, j0:j0 + 2, :], ALU.mult, R=[Eb, bank], W=[PTb])
                        else:
                            for rr in range(2):
                                if jj[rr] is None:
                                    P.op(eng, lambda e, ks=ks, ci=ci, rr=rr, PTb=PTb: e.memset(
                                        PTb[ks, ci, rr * 64:(rr + 1) * 64], 0.0), W=[PTb])
                                else:
                                    j0 = h * 15 + jj[rr]
                                    self.tt(eng, PTb[ks, ci, rr * 64:(rr + 1) * 64], Eb[ks, ci, rr * 64:(rr + 1) * 64],
                                            bank[ks, j0, :], ALU.mult, R=[Eb, bank], W=[PTb])
                def tail(n=n, chunks=chunks, nch=nch, PTb=PTb, h=h, ms=ms, qt=qt):
                    ob = pb[4 + (n % 2)]
                    for ci, kt in enumerate(chunks):
                        self.mm(ob[:, 0:65], PTb[:, ci, :], vsb[:, kt, h * VW:h * VW + 65], ci == 0, ci == nch - 1,
                                R=[PTb, vsb], W=[ob])
                    r_ = rd[n % 2]
                    P.op("dve", lambda e, r_=r_, ob=ob: e.reciprocal(r_[:], ob[:, 64:65]), R=[ob], W=[r_])
                    self.ts("dve", ms[:, h * 64:(h + 1) * 64], ob[:, 0:64], r_[:, 0:1], None, ALU.mult, None,
                            R=[ob, r_], W=[ms])
                    if h == 3:
                        P.dma("pool", self.mix[qt * 128:(qt + 1) * 128, 0:256], ms[:], R=[ms], W=[self.mix])
                if pend_tail is not None:
                    pend_tail()
                pend_tail = tail
                n += 1
        pend_tail()
        P.flush()

    def stage_diff(self, l, ctx_out):
        P = self.P
        pb = self.pb
        lam_init = 0.8 - 0.6 * math.exp(-0.3 * l)
        lv = P.sb([1, 128], F32)
        P.dma("sp", lv[:], self.df_lambda.t.ap()[l].unsqueeze(0), R=[self.df_lambda], W=[lv])
        pr = P.sb([1, 64], F32)
        lvv = lv[:].rearrange("p (a b) -> p a b", b=32)
        self.tt("dve", pr[:].rearrange("p (a b) -> p a b", b=32), lvv[:, 0:4:2, :], lvv[:, 1:4:2, :], ALU.mult, R=[lv], W=[pr])
        sm = P.sb([1, 4], F32)
        P.op("dve", lambda e: e.reduce_sum(sm[:, 0:2], pr[:].rearrange("p (a b) -> p a b", b=32), AX.X), R=[pr], W=[sm])
        self.act(sm[:, 0:2], sm[:, 0:2], AF.Exp, R=[sm], W=[sm])
        self.tt("dve", sm[:, 2:3], sm[:, 1:2], sm[:, 0:1], ALU.subtract, R=[sm], W=[sm])
        self.ts("dve", sm[:, 3:4], sm[:, 2:3], -lam_init, None, ALU.add, None, R=[sm], W=[sm])
        self.mm(pb[7][:, 0:1], self.onesf[0:1, :], sm[0:1, 3:4], True, True, R=[self.onesf, sm], W=[pb[7]])
        neglam = P.sb([128, 1], F32)
        self.cp("dve", neglam[:], pb[7][:, 0:1], R=[pb[7]], W=[neglam])
        gdf = P.sb([128, 64], F32)
        P.dma("sp", gdf[:], self.df_norm_g.t.ap()[l].partition_broadcast(128), R=[self.df_norm_g], W=[gdf])
        self.ts("dve", gdf[:], gdf[:], 1.0 - lam_init, None, ALU.mult, None, R=[gdf], W=[gdf])
        kT = [P.sb([128, T], BF16) for _ in range(2)]
        qA = [P.sb([128, T], BF16) for _ in range(2)]
        qB = [P.sb([128, T], BF16) for _ in range(2)]
        for hp in range(2):
            P.dma("sp", kT[hp][:], self.projT[1536 + hp * 128:1536 + (hp + 1) * 128, :], R=[self.projT], W=[kT[hp]])
            P.op("pool", lambda e, hp=hp: e.memset(qA[hp][:], 0.0), W=[qA[hp]])
            P.op("pool", lambda e, hp=hp: e.memset(qB[hp][:], 0.0), W=[qB[hp]])
            for hl in range(2):
                r0 = 1280 + hp * 128 + hl * 64
                P.dma("sp", qA[hp][hl * 64:hl * 64 + 32, :], self.projT[r0:r0 + 32, :], R=[self.projT], W=[qA[hp]])
                P.dma("sp", qB[hp][hl * 64 + 32:hl * 64 + 64, :], self.projT[r0 + 32:r0 + 64, :], R=[self.projT], W=[qB[hp]])
        vsb = P.sb([128, NT, 4 * VW], BF16)
        P.dma("sp", vsb[:], self.dfvd.t.ap().rearrange("(n p) c -> p n c", p=128), R=[self.dfvd], W=[vsb])
        P1 = [P.sb([128, 512], BF16) for _ in range(3)]
        P2 = [P.sb([128, 512], BF16) for _ in range(3)]
        mst = [P.sb([128, 4, 256], BF16) for _ in range(2)]
        rc = P.sb([128, 8], F32)
        av = P.sb([128, 4, 64], F32)
        bv = P.sb([128, 4, 64], F32)
        sq = P.sb([128, 4, 64], F32)
        s4 = P.sb([128, 8], F32)
        oT = P.sb([65, 2, 512], F32)
        scale = 1.0 / math.sqrt(32.0)
        qblocks = ([(0, 256, list(range(2)))] if ctx_out else []) + [(L + i * 512, 512, list(range(NT))) for i in range(8)]
        n = 0
        for bi, (q0, nq, kcs) in enumerate(qblocks):
            nqs = nq // 128
            ms = mst[bi % 2]
            for h in range(4):
                hp, hl = h // 2, h % 2
                psl = slice(hl * 64, hl * 64 + 64)
                o1, o2 = pb[6], pb[7]
                def score(kc):
                    nn_ = n
                    s1, s2 = pb[(2 * nn_) % 6], pb[(2 * nn_ + 1) % 6]
                    p1, p2 = P1[nn_ % 3], P2[nn_ % 3]
                    lhs = kT[hp][psl, kc * 128:(kc + 1) * 128]
                    self.mm(s1[:, :nq], lhs, qA[hp][psl, q0:q0 + nq], True, True, R=[kT[hp], qA[hp]], W=[s1])
                    self.mm(s2[:, :nq], lhs, qB[hp][psl, q0:q0 + nq], True, True, R=[kT[hp], qB[hp]], W=[s2])
                    self.act(p1[:, :nq], s1[:, :nq], AF.Exp, R=[s1], W=[p1], scale=scale)
                    self.act(p2[:, :nq], s2[:, :nq], AF.Exp, R=[s2], W=[p2], scale=scale)
                    return p1, p2

                def pv(ki, kc, p1, p2):
                    vst_ = vsb[:, kc, h * VW:h * VW + 65]
                    self.mm(o1[0:65, :nq], vst_, p1[:, :nq], ki == 0, ki == len(kcs) - 1, R=[p1, vsb], W=[o1])
                    self.mm(o2[0:65, :nq], vst_, p2[:, :nq], ki == 0, ki == len(kcs) - 1, R=[p2, vsb], W=[o2])

                pend = []
                for ki, kc in enumerate(kcs):
                    pp = score(kc)
                    n += 1
                    pend.append((ki, kc, pp[0], pp[1]))
                    if len(pend) > 2:
                        pv(*pend.pop(0))
                while pend:
                    pv(*pend.pop(0))
                self.cp("act", oT[:, 0, :nq], o1[0:65, :nq], R=[o1], W=[oT])
                self.cp("dve", oT[:, 1, :nq], o2[0:65, :nq], R=[o2], W=[oT])
                if self.dbg and bi == 0 and h == 0:
                    dd1 = self.dscr(f"dbg_oT{l}", [65, 1024], F32)
                    P.dma("sp", dd1[:], oT[:].rearrange("p a b -> p (a b)"), R=[oT], W=[dd1])
                t1b, t2b = pb[0], pb[1]
                for mi, tb_ in ((0, t1b), (1, t2b)):
                    for qs in range(nqs):
                        P.op("pe", lambda e, mi=mi, tb_=tb_, qs=qs: e.transpose(
                            tb_[:, qs * 65:(qs + 1) * 65], oT[:, mi, qs * 128:(qs + 1) * 128], self.identf[0:65, 0:65]),
                            R=[oT, self.identf], W=[tb_])
                if self.dbg and bi == 0 and h == 0:
                    dd2 = self.dscr(f"dbg_t1b{l}", [128, 512], F32)
                    dtmp = P.sb([128, 512], F32)
                    self.cp("dve", dtmp[:], t1b[:, :], R=[t1b], W=[dtmp])
                    P.dma("sp", dd2[:], dtmp[:], R=[dtmp], W=[dd2])
                o1v = t1b[:, 0:260].rearrange("p (a b) -> p a b", b=65)[:, 0:nqs, :]
                o2v = t2b[:, 0:260].rearrange("p (a b) -> p a b", b=65)[:, 0:nqs, :]
                o1, o2 = t1b, t2b
                P.op("dve", lambda e, o1v=o1v, nqs=nqs: e.reciprocal(rc[:, 0:nqs], o1v[:, :, 64]), R=[o1], W=[rc])
                P.op("dve", lambda e, o2v=o2v, nqs=nqs: e.reciprocal(rc[:, 4:4 + nqs], o2v[:, :, 64]), R=[o2], W=[rc])
                self.tt("dve", av[:, 0:nqs, :], o1v[:, :, 0:64], rc[:, 0:nqs].unsqueeze(2).to_broadcast([128, nqs, 64]),
                        ALU.mult, R=[o1, rc], W=[av])
                self.tt("dve", bv[:, 0:nqs, :], o2v[:, :, 0:64], rc[:, 4:4 + nqs].unsqueeze(2).to_broadcast([128, nqs, 64]),
                        ALU.mult, R=[o2, rc], W=[bv])
                self.stt("dve", av[:, 0:nqs, :], bv[:, 0:nqs, :], neglam[:, 0:1], av[:, 0:nqs, :], ALU.mult, ALU.add,
                         R=[bv, av, neglam], W=[av])
                self.tt("pool", sq[:, 0:nqs, :], av[:, 0:nqs, :], av[:, 0:nqs, :], ALU.mult, R=[av], W=[sq])
                P.op("dve", lambda e, nqs=nqs: e.reduce_sum(s4[:, 0:nqs], sq[:, 0:nqs, :], AX.X), R=[sq], W=[s4])
                self.rsqrt_mean(s4[:, 4:4 + nqs], s4[:, 0:nqs], 64.0, 1e-6, R=[s4], W=[s4])
                self.tt("dve", av[:, 0:nqs, :], av[:, 0:nqs, :], s4[:, 4:4 + nqs].unsqueeze(2).to_broadcast([128, nqs, 64]),
                        ALU.mult, R=[av, s4], W=[av])
                self.tt("dve", ms[:, 0:nqs, h * 64:(h + 1) * 64], av[:, 0:nqs, :],
                        gdf[:].unsqueeze(1).to_broadcast([128, nqs, 64]), ALU.mult, R=[av, gdf], W=[ms])
            P.dma("pool", self.mix[q0:q0 + nq, 512:768].rearrange("(a p) c -> p a c", p=128), ms[:, 0:nqs, :],
                  R=[ms], W=[self.mix])
        P.flush()

    def stage_fft(self, l, ctx_out):
        P = self.P
        pb = self.pb
        ftw = P.sb([128, 2, 256], BF16)
        P.dma("pool", ftw[:], self.ft_w.t.ap()[l].rearrange("(c p) n -> p c n", p=128), R=[self.ft_w], W=[ftw])
        bdc = P.sb([128, 128], BF16)
        bds = P.sb([128, 128], BF16)
        P.dma("pool", bdc[:], self.c_bdc[:], R=[self.c_bdc], W=[bdc])
        P.dma("pool", bds[:], self.c_bds[:], R=[self.c_bds], W=[bds])
        M12 = P.sb([128, 2, 512], BF16)
        for j in range(2):
            self.mm(pb[0][:, 0:256], bdc[:], ftw[:, j, :], True, True, R=[bdc, ftw], W=[pb[0]])
            self.mm(pb[1][:, 0:256], bds[:], ftw[:, j, :], True, True, R=[bds, ftw], W=[pb[1]])
            self.cp("act", M12[:, j, 0:256], pb[0][:, 0:256], R=[pb[0]], W=[M12])
            P.op("act", lambda e, j=j: e.mul(M12[:, j, 256:512], pb[1][:, 0:256], -1.0), R=[pb[1]], W=[M12])
        uT = P.sb([128, 2, T], BF16)
        P.dma("sp", uT[:], self.projT[1792:2048, :].rearrange("(c p) t -> p c t", p=128), R=[self.projT], W=[uT])
        uM = P.sb([128, NT, 512], BF16)
        for t in range(NT):
            bk = pb[t % 2]
            for c in range(2):
                self.mm(bk[:, :], uT[:, c, t * 128:(t + 1) * 128], M12[:, c, :], c == 0, c == 1, R=[uT, M12], W=[bk])
            self.cp("act" if t % 2 == 0 else "dve", uM[:, t, :], bk[:, :], R=[bk], W=[uM])
        ct = [P.sb([128, 32, 128], BF16) for _ in range(2)]
        st = [P.sb([128, 32, 128], BF16) for _ in range(2)]
        og = [P.sb([128, 256], BF16) for _ in range(2)]
        jobs = ([("c", ti) for ti in range(2)] if ctx_out else []) + [("l", ti) for ti in range(32)]
        for n, (kind, ti) in enumerate(jobs):
            c_, s_ = ct[n % 2], st[n % 2]
            if kind == "l":
                ntc, tb = 32, 2
                P.dma("sp", c_[:], self.c_dftc[ti], R=[self.c_dftc], W=[c_])
                P.dma("sp", s_[:], self.c_dfts[ti], R=[self.c_dfts], W=[s_])
            else:
                ntc, tb = 2, 0
                P.dma("sp", c_[:, 0:2, :], self.c_dftc_c[ti], R=[self.c_dftc_c], W=[c_])
                P.dma("sp", s_[:, 0:2, :], self.c_dfts_c[ti], R=[self.c_dfts_c], W=[s_])
            bk = pb[2 + n % 2]
            for tc in range(ntc):
                self.mm(bk[:, 0:256], c_[:, tc, :], uM[:, tb + tc, 0:256], tc == 0, False, R=[c_, uM], W=[bk])
                self.mm(bk[:, 0:256], s_[:, tc, :], uM[:, tb + tc, 256:512], False, tc == ntc - 1, R=[s_, uM], W=[bk])
            o_ = og[n % 2]
            self.cp("act" if n % 2 == 0 else "dve", o_[:], bk[:, 0:256], R=[bk], W=[o_])
            t = tb + ti
            P.dma("pool", self.mix[t * 128:(t + 1) * 128, 768:1024], o_[:], R=[o_], W=[self.mix])
        P.flush()

    def stage_outproj(self, l, ctx_out):
        P = self.P
        pb = self.pb
        wout = P.sb([128, 8, D], BF16)
        for k in range(8):
            P.dma("pool", wout[:, k, :], self.w_out.t.ap()[l, k * 128:(k + 1) * 128, :], R=[self.w_out], W=[wout])
        wr = P.sb([128, 8, NE], BF16)
        P.dma("pool", wr[:], self.w_router.t.ap()[l].rearrange("(k p) n -> p k n", p=128), R=[self.w_router], W=[wr])
        gs2T = P.sb([128, 8, 2], F32)
        self.ts("dve", gs2T[:], self.mT[:, 32:40, :], 1.0, None, ALU.add, None, R=[self.mT], W=[gs2T])
        self.tt("dve", gs2T[:], gs2T[:], self.g2T[:].unsqueeze(2).to_broadcast([128, 8, 2]), ALU.mult,
                R=[gs2T, self.g2T], W=[gs2T])
        streams = [0, 1] if ctx_out else [0]
        m2b, gs2b, sh2b = {}, {}, {}
        for s_ in streams:
            m2b[s_] = P.sb([128, D], F32)
            gs2b[s_] = P.sb([128, D], F32)
            sh2b[s_] = P.sb([128, D], F32)
            self.bcast_vec(m2b[s_], lambda c, s_=s_: self.mT[:, 16 + c, s_:s_ + 1], [self.mT])
            self.bcast_vec(gs2b[s_], lambda c, s_=s_: gs2T[:, c, s_:s_ + 1], [gs2T])
            self.bcast_vec(sh2b[s_], lambda c, s_=s_: self.mT[:, 24 + c, s_:s_ + 1], [self.mT])
        mx = [P.sb([128, D], BF16) for _ in range(2)]
        mxT = [P.sb([128, 8, 128], BF16) for _ in range(2)]
        xt = [P.sb([128, D], F32) for _ in range(2)]
        tmp = [P.sb([128, D], F32) for _ in range(2)]
        xn = [P.sb([128, D], F32) for _ in range(2)]
        junk = P.sb([128, D], BF16)
        ss = [P.sb([128, 2], F32) for _ in range(2)]
        h2 = [P.sb([128, D], BF16) for _ in range(2)]
        h2T = [P.sb([128, 8, 128], BF16) for _ in range(2)]
        sm = [P.sb([128, 4], F32) for _ in range(2)]
        ex = [P.sb([128, NE], F32) for _ in range(2)]
        tiles = list(range(0 if ctx_out else 2, NT))
        for n, t in enumerate(tiles):
            s_ = 1 if t < 2 else 0
            i = n % 2
            rs = slice(t * 128, (t + 1) * 128)
            P.dma("sp", mx[i][:], self.mix[rs, :], R=[self.mix], W=[mx[i]])
            P.dma("sp", xt[i][:], self.xres[rs, :], R=[self.xres], W=[xt[i]])
            bT = pb[0 + i]
            pT = bT[:].bitcast(BF16)
            for c in range(8):
                P.op("pe", lambda e, c=c, pT=pT, i=i: e.transpose(pT[:, c * 128:(c + 1) * 128], mx[i][:, c * 128:(c + 1) * 128],
                                                                   self.ident[:]), R=[mx[i], self.ident], W=[bT])
            self.cp("act", mxT[i][:].rearrange("p c t -> p (c t)"), pT[:, :], R=[bT], W=[mxT[i]])
            for nn in range(2):
                bk = pb[2 + nn]
                for k in range(8):
                    self.mm(bk[:, :], mxT[i][:, k, :], wout[:, k, nn * 512:(nn + 1) * 512], k == 0, k == 7,
                            R=[mxT[i], wout], W=[bk])
                self.tt("dve", tmp[i][:, nn * 512:(nn + 1) * 512], bk[:, :], m2b[s_][:, nn * 512:(nn + 1) * 512], ALU.mult,
                        R=[bk, m2b[s_]], W=[tmp[i]])
            self.tt("pool", xn[i][:], xt[i][:], tmp[i][:], ALU.add, R=[xt[i], tmp[i]], W=[xn[i]])
            P.dma("pool", self.xres[rs, :], xn[i][:], R=[xn[i]], W=[self.xres])
            P.op("dve", lambda e, i=i: e.memset(ss[i][:], 0.0), W=[ss[i]])
            self.act(junk[:], xn[i][:], AF.Square, R=[xn[i], ss[i]], W=[junk, ss[i]], accum_out=ss[i][:, 0:1])
            self.rsqrt_mean(ss[i][:, 1:2], ss[i][:, 0:1], float(D), 1e-6, R=[ss[i]], W=[ss[i]])
            self.stt("dve", tmp[i][:], xn[i][:], ss[i][:, 1:2], gs2b[s_][:], ALU.mult, ALU.mult,
                     R=[xn[i], ss[i], gs2b[s_]], W=[tmp[i]])
            self.tt("pool", h2[i][:], tmp[i][:], sh2b[s_][:], ALU.add, R=[tmp[i], sh2b[s_]], W=[h2[i]])
            P.dma("pool", self.h2d[rs, :], h2[i][:], R=[h2[i]], W=[self.h2d])
            bT2 = pb[4 + i]
            pT2 = bT2[:].bitcast(BF16)
            for c in range(8):
                P.op("pe", lambda e, c=c, pT2=pT2, i=i: e.transpose(pT2[:, c * 128:(c + 1) * 128], h2[i][:, c * 128:(c + 1) * 128],
                                                                     self.ident[:]), R=[h2[i], self.ident], W=[bT2])
            self.cp("act", h2T[i][:].rearrange("p c t -> p (c t)"), pT2[:, :], R=[bT2], W=[h2T[i]])
            bR = pb[6 + i]
            for k in range(8):
                self.mm(bR[:, 0:NE], h2T[i][:, k, :], wr[:, k, :], k == 0, k == 7, R=[h2T[i], wr], W=[bR])
            P.op("dve", lambda e, i=i, bR=bR: e.reduce_max(sm[i][:, 0:1], bR[:, 0:NE], AX.X), R=[bR], W=[sm[i]])
            self.ts("dve", sm[i][:, 1:2], sm[i][:, 0:1], -1.0, None, ALU.mult, None, R=[sm[i]], W=[sm[i]])
            P.op("dve", lambda e, i=i: e.memset(sm[i][:, 2:3], 0.0), W=[sm[i]])
            self.act(ex[i][:], bR[:, 0:NE], AF.Exp, R=[bR, sm[i]], W=[ex[i], sm[i]], bias=sm[i][:, 1:2], accum_out=sm[i][:, 2:3])
            P.op("dve", lambda e, i=i: e.reciprocal(sm[i][:, 3:4], sm[i][:, 2:3]), R=[sm[i]], W=[sm[i]])
            self.ts("dve", self.aff[:, t, :], ex[i][:], sm[i][:, 3:4], None, ALU.mult, None, R=[ex[i], sm[i]], W=[self.aff])
        if self.dbg:
            da = self.dscr(f"dbg_aff{l}", [128, NT * NE], F32)
            P.dma("sp", da[:], self.aff[:].rearrange("p t e -> p (t e)"), R=[self.aff], W=[da])
        P.flush()

    def stage_route(self, l, ctx_out):
        P = self.P
        pb = self.pb
        sbase = P.sb([128, NT, NE], F32)
        P.dma("sp", sbase[:].rearrange("p t e -> p (t e)"), self.c_slotbase[:], R=[self.c_slotbase], W=[sbase])
        streams = [(2, 32, CAP_L)] + ([(0, 2, CAP_C)] if ctx_out else [])
        cmpb = P.sb([128, 32, NE], F32)
        lo = P.sb([128, NE], F32)
        mid = P.sb([128, NE], F32)
        cnt = P.sb([128, NE], F32)
        ge = P.sb([128, NE], F32)
        tot = P.sb([128, 32, NE], F32)
        off = P.sb([128, 32, NE], F32)
        pos = P.sb([128, 32, NE], F32)
        m2 = P.sb([128, 32, NE], F32)
        dstf = P.sb([128, 32, NE], F32)
        for (t0, nt, cap) in streams:
            affv = self.aff[:, t0:t0 + nt, :]
            cv = cmpb[:, 0:nt, :]
            P.op("dve", lambda e: e.memset(lo[:], 0.0), W=[lo])
            for it in range(32):
                hstep = 2.0 ** (-(it + 1))
                self.ts("dve", mid[:], lo[:], hstep, None, ALU.add, None, R=[lo], W=[mid])
                self.tt("dve", cv, affv, mid[:].unsqueeze(1).to_broadcast([128, nt, NE]), ALU.is_gt,
                        R=[self.aff, mid], W=[cmpb])
                P.op("dve", lambda e, cv=cv: e.reduce_sum(cnt[:], cv.rearrange("p t e -> p e t"), AX.X), R=[cmpb], W=[cnt])
                self.mm(pb[0][:, 0:NE], self.onesf[:], cnt[:], True, True, R=[self.onesf, cnt], W=[pb[0]])
                self.ts("dve", ge[:], pb[0][:, 0:NE], cap - 0.5, None, ALU.is_ge, None, R=[pb[0]], W=[ge])
                self.stt("dve", lo[:], ge[:], hstep, lo[:], ALU.mult, ALU.add, R=[ge, lo], W=[lo])
            self.tt("dve", cv, affv, lo[:].unsqueeze(1).to_broadcast([128, nt, NE]), ALU.is_gt, R=[self.aff, lo], W=[cmpb])
            cvf = cv.rearrange("p t e -> p (t e)")
            self.mm(pb[1][:, 0:nt * NE], self.trif[:], cvf, True, True, R=[self.trif, cmpb], W=[pb[1]])
            self.mm(pb[2][:, 0:nt * NE], self.onesf[:], cvf, True, True, R=[self.onesf, cmpb], W=[pb[2]])
            self.cp("act", tot[:, 0:nt, :].rearrange("p t e -> p (t e)"), pb[2][:, 0:nt * NE], R=[pb[2]], W=[tot])
            P.op("dve", lambda e: e.memset(off[:, 0, :], 0.0), W=[off])
            for j in range(1, nt):
                self.tt("dve", off[:, j, :], off[:, j - 1, :], tot[:, j - 1, :], ALU.add, R=[off, tot], W=[off])
            self.tt("dve", pos[:, 0:nt, :].rearrange("p t e -> p (t e)"), pb[1][:, 0:nt * NE],
                    off[:, 0:nt, :].rearrange("p t e -> p (t e)"), ALU.add, R=[pb[1], off], W=[pos])
            self.ts("dve", m2[:, 0:nt, :], pos[:, 0:nt, :], cap + 0.5, None, ALU.is_lt, None, R=[pos], W=[m2])
            self.tt("dve", m2[:, 0:nt, :], m2[:, 0:nt, :], cv, ALU.mult, R=[m2, cmpb], W=[m2])
            self.stt("dve", dstf[:, 0:nt, :], pos[:, 0:nt, :], -1.0, sbase[:, t0:t0 + nt, :], ALU.add, ALU.add,
                     R=[pos, sbase], W=[dstf])
            self.tt("dve", dstf[:, 0:nt, :], dstf[:, 0:nt, :], m2[:, 0:nt, :], ALU.mult, R=[dstf, m2], W=[dstf])
            self.ts("dve", dstf[:, 0:nt, :], dstf[:, 0:nt, :], float(OOB), None, ALU.add, None, R=[dstf], W=[dstf])
            self.cp("dve", self.desti[:, t0:t0 + nt, :], dstf[:, 0:nt, :], R=[dstf], W=[self.desti])
            self.tt("dve", self.gatev[:, t0:t0 + nt, :], affv, m2[:, 0:nt, :], ALU.mult, R=[self.aff, m2], W=[self.gatev])
        if self.dbg:
            dd = self.dscr(f"dbg_dest{l}", [128, NT * NE], I32)
            P.dma("sp", dd[:], self.desti[:].rearrange("p t e -> p (t e)"), R=[self.desti], W=[dd])
        ht = [P.sb([128, D], BF16) for _ in range(3)]
        tiles = list(range(0 if ctx_out else 2, NT))
        for n, t in enumerate(tiles):
            hb = ht[n % 3]
            P.dma("sp", hb[:], self.h2d[t * 128:(t + 1) * 128, :], R=[self.h2d], W=[hb])
            for e_ in range(NE):
                P.op("pool", lambda e, t=t, e_=e_, hb=hb: e.indirect_dma_start(
                    out=self.xg[:, :], out_offset=bass.IndirectOffsetOnAxis(ap=self.desti[:, t, e_:e_ + 1], axis=0),
                    in_=hb[:, :], in_offset=None, bounds_check=self.oob_reg(e), oob_is_err=False),
                    R=[hb, self.desti], W=[], kind="dma")
        P.flush()

    def stage_ffn(self, l, ctx_out):
        P = self.P
        pb = self.pb
        NW = 6
        wbuf = [P.sb([128, 8 * 1024], BF16) for _ in range(NW)]
        nw = [0]

        def load_w(kind, e_, part):
            w = wbuf[nw[0] % NW]
            nw[0] += 1
            if kind == "g":
                src = self.w_gate.t.ap()[l, e_, :, part * 1024:(part + 1) * 1024].rearrange("(k p) n -> p k n", p=128)
                P.dma("pool", w[:].rearrange("p (k n) -> p k n", n=1024), src, R=[self.w_gate], W=[w])
            elif kind == "u":
                src = self.w_up.t.ap()[l, e_, :, part * 1024:(part + 1) * 1024].rearrange("(k p) n -> p k n", p=128)
                P.dma("pool", w[:].rearrange("p (k n) -> p k n", n=1024), src, R=[self.w_up], W=[w])
            else:
                src = self.w_down.t.ap()[l, e_, :, part * 512:(part + 1) * 512].rearrange("(k p) n -> p k n", p=128)
                P.dma("pool", w[:].rearrange("p (k n) -> p k n", n=512), src, R=[self.w_down], W=[w])
            return w

        nsl = ESL if ctx_out else CAP_L
        stiles = [(0, 128), (128, 128), (256, 128), (384, 128)] + ([(512, 32)] if ctx_out else [])
        xr = [P.sb([128, D], BF16) for _ in range(3)]
        xgT = [P.sb([128, 8, ESL], BF16) for _ in range(2)]
        hidT = P.sb([128, 16, ESL], BF16)
        sg = [P.sb([128, 512], F32) for _ in range(2)]
        sgc = P.sb([128, 32], F32)
        yst = [P.sb([128, 512], F32) for _ in range(3)]
        nx = 0
        ny = 0
        for e_ in range(NE):
            xT = xgT[e_ % 2]
            for si, (s0, rows) in enumerate(stiles):
                xb = xr[nx % 3]
                nx += 1
                r0 = e_ * ESL + s0
                P.dma("sp", xb[:rows, :], self.xg[r0:r0 + rows, :], R=[self.xg], W=[xb])
                bT = pb[6 + (si % 2)]
                pT = bT[:].bitcast(BF16)
                for c in range(8):
                    P.op("pe", lambda e, c=c, pT=pT, xb=xb, rows=rows: e.transpose(
                        pT[:, c * 128:c * 128 + rows], xb[:rows, c * 128:(c + 1) * 128], self.ident[:rows, :rows]),
                        R=[xb, self.ident], W=[bT])
                self.cp("act" if si % 2 == 0 else "dve", xT[:, :, s0:s0 + rows],
                        pT[:, :].rearrange("p (c t) -> p c t", t=128)[:, :, 0:rows], R=[bT], W=[xT])
            for fh in range(2):
                wg = load_w("g", e_, fh)
                wu = load_w("u", e_, fh)
                wgv = wg[:].rearrange("p (k n) -> p k n", n=1024)
                wuv = wu[:].rearrange("p (k n) -> p k n", n=1024)
                for fc in range(8):
                    fcg = fh * 8 + fc
                    gb, ub, cb = pb[0 + (fcg % 2)], pb[2 + (fcg % 2)], pb[4 + (fcg % 2)]
                    for k in range(8):
                        lw = wgv[:, k, fc * 128:(fc + 1) * 128]
                        self.mm(gb[:, :], lw, xT[:, k, 0:512], k == 0, k == 7, R=[wg, xT], W=[gb])
                        if ctx_out:
                            self.mm(cb[:, 0:32], lw, xT[:, k, 512:544], k == 0, k == 7, R=[wg, xT], W=[cb])
                    for k in range(8):
                        lw = wuv[:, k, fc * 128:(fc + 1) * 128]
                        self.mm(ub[:, :], lw, xT[:, k, 0:512], k == 0, k == 7, R=[wu, xT], W=[ub])
                        if ctx_out:
                            self.mm(cb[:, 32:64], lw, xT[:, k, 512:544], k == 0, k == 7, R=[wu, xT], W=[cb])
                    sgb = sg[fcg % 2]
                    self.act(sgb[:], gb[:, :], AF.Silu, R=[gb], W=[sgb])
                    self.tt("dve", hidT[:, fcg, 0:512], sgb[:], ub[:, :], ALU.mult, R=[sgb, ub], W=[hidT])
                    if ctx_out:
                        self.act(sgc[:], cb[:, 0:32], AF.Silu, R=[cb], W=[sgc])
                        self.tt("dve", hidT[:, fcg, 512:544], sgc[:], cb[:, 32:64], ALU.mult, R=[sgc, cb], W=[hidT])
            for nn in range(2):
                wd = load_w("d", e_, nn)
                wdv = wd[:].rearrange("p (k n) -> p k n", n=512)
                for si, (s0, rows) in enumerate(stiles):
                    yb = pb[6 + (si % 2)]
                    for fc in range(16):
                        self.mm(yb[:rows, :], hidT[:, fc, s0:s0 + rows], wdv[:, fc, :], fc == 0, fc == 15, R=[hidT, wd], W=[yb])
                    ys = yst[ny % 3]
                    ny += 1
                    self.cp("act" if si % 2 == 0 else "dve", ys[:rows, :], yb[:rows, :], R=[yb], W=[ys])
                    r0 = e_ * ESL + s0
                    P.dma("sp", self.ybuf[r0:r0 + rows, nn * 512:(nn + 1) * 512], ys[:rows, :], R=[ys], W=[])
        P.flush()

    def stage_combine(self, l, ctx_out, final):
        P = self.P
        streams = [0, 1] if ctx_out else [0]
        m5b = {}
        for s_ in streams:
            m5b[s_] = P.sb([128, D], F32)
            self.bcast_vec(m5b[s_], lambda c, s_=s_: self.mT[:, 40 + c, s_:s_ + 1], [self.mT])
        if final:
            gfT = P.sb([128, 8], F32)
            P.dma("sp", gfT[:], self.final_g.t.ap().rearrange("(c p) -> p c", p=128), R=[self.final_g], W=[gfT],
                  allow_slow_non_contiguous=True)
            gfb = P.sb([128, D], F32)
            self.bcast_vec(gfb, lambda c: gfT[:, c:c + 1], [gfT])
            junk = P.sb([128, D], BF16)
        G = [P.sb([128, D], F32) for _ in range(NE)]
        for e_ in range(NE):
            P.op("dve" if e_ % 2 == 0 else "pool", lambda e, e_=e_: e.memset(G[e_][:], 0.0), W=[G[e_]])
        xt = [P.sb([128, D], F32) for _ in range(2)]
        acc = [P.sb([128, D], F32) for _ in range(2)]
        ss = [P.sb([128, 2], F32) for _ in range(2)]
        tiles = list(range(0 if ctx_out else 2, NT))
        for n, t in enumerate(tiles):
            s_ = 1 if t < 2 else 0
            i = n % 2
            rs = slice(t * 128, (t + 1) * 128)
            P.dma("sp", xt[i][:], self.xres[rs, :], R=[self.xres], W=[xt[i]])
            for e_ in range(NE):
                P.op("pool", lambda e, t=t, e_=e_: e.indirect_dma_start(
                    out=G[e_][:, :], out_offset=None, in_=self.ybuf[:, :],
                    in_offset=bass.IndirectOffsetOnAxis(ap=self.desti[:, t, e_:e_ + 1], axis=0),
                    bounds_check=self.oob_reg(e), oob_is_err=False), R=[self.ybuf, self.desti], W=[G[e_]], kind="dma")
            a = acc[i]
            self.ts("dve", a[:], G[0][:], self.gatev[:, t, 0:1], None, ALU.mult, None, R=[G[0], self.gatev], W=[a])
            for e_ in range(1, NE):
                self.stt("dve", a[:], G[e_][:], self.gatev[:, t, e_:e_ + 1], a[:], ALU.mult, ALU.add,
                         R=[G[e_], self.gatev, a], W=[a])
            self.tt("dve", a[:], a[:], m5b[s_][:], ALU.mult, R=[a, m5b[s_]], W=[a])
            self.tt("dve", a[:], a[:], xt[i][:], ALU.add, R=[a, xt[i]], W=[a])
            if not final or t < 2:
                P.dma("sp", self.xres[rs, :], a[:], R=[a], W=[self.xres])
            else:
                if self.dbg:
                    P.dma("sp", self.xres[rs, :], a[:], R=[a], W=[self.xres])
                P.op("dve", lambda e, i=i: e.memset(ss[i][:], 0.0), W=[ss[i]])
                self.act(junk[:], a[:], AF.Square, R=[a, ss[i]], W=[junk, ss[i]], accum_out=ss[i][:, 0:1])
                self.rsqrt_mean(ss[i][:, 1:2], ss[i][:, 0:1], float(D), 1e-6, R=[ss[i]], W=[ss[i]])
                self.stt("dve", a[:], a[:], ss[i][:, 1:2], gfb[:], ALU.mult, ALU.mult, R=[a, ss[i], gfb], W=[a])
                P.dma("sp", self.out[(t - 2) * 128:(t - 1) * 128, :], a[:], R=[a], W=[self.out])
        P.flush()


    def _gdn_prep(self, l, qT, kT, qtok, ktok, vtok, gg, beta, nbeta, obb):
        P = self.P
        pb = self.pb
        identb_f = self.identf
        ab = P.sb([128, NT, 16], F32)
        P.dma("sp", ab[:], self.abd.t.ap().rearrange("(n p) c -> p n c", p=128), R=[self.abd], W=[ab])
        dtb = P.sb([128, 8], F32)
        nA = P.sb([128, 8], F32)
        P.dma("sp", dtb[:], self.dn_dt_bias.t.ap()[l].partition_broadcast(128), R=[self.dn_dt_bias], W=[dtb])
        P.dma("sp", nA[:], self.dn_a_log.t.ap()[l].partition_broadcast(128), R=[self.dn_a_log], W=[nA])
        self.act(nA[:], nA[:], AF.Exp, R=[nA], W=[nA])
        self.ts("dve", nA[:], nA[:], -1.0, None, ALU.mult, None, R=[nA], W=[nA])
        self.tt("dve", gg[:], ab[:, :, 0:8], dtb[:].unsqueeze(1).to_broadcast([128, NT, 8]), ALU.add, R=[ab, dtb], W=[gg])
        self.act(gg[:], gg[:], AF.Exp, R=[gg], W=[gg])
        self.act(gg[:], gg[:], AF.Ln, R=[gg], W=[gg], bias=1.0)
        self.tt("dve", gg[:], gg[:], nA[:].unsqueeze(1).to_broadcast([128, NT, 8]), ALU.mult, R=[gg, nA], W=[gg])
        self.act(beta[:], ab[:, :, 8:16], AF.Sigmoid, R=[ab], W=[beta])
        self.ts("dve", nbeta[:], beta[:], -1.0, None, ALU.mult, None, R=[beta], W=[nbeta])
        segs = [(0, L), (L, T)]
        xs = [P.sb([128, T], BF16) for _ in range(2)]
        sil = P.sb([128, T], BF16)
        sqv = P.sb([128, 512], BF16)
        rn = P.sb([128, 512], F32)
        cw = [P.sb([128, 5], F32) for _ in range(2)]
        dg = [P.sb([128, 5, 128], BF16) for _ in range(2)]
        blocks = [(0, L, 0, L)] + [(L + 512 * j, 512, L, T) for j in range(8)]
        nblk = 0
        for cc in range(6):
            x_ = xs[cc % 2]
            w_ = cw[cc % 2]
            dg_ = dg[cc % 2]
            P.dma("sp", x_[:], self.projT[512 + cc * 128:512 + (cc + 1) * 128, :], R=[self.projT], W=[x_])
            P.dma("sp", w_[:], self.dn_conv_w.t.ap()[l, :, cc * 128:(cc + 1) * 128].rearrange("k c -> c k"),
                  R=[self.dn_conv_w], W=[w_], allow_slow_non_contiguous=True)
            for k in range(5):
                self.ts("dve", dg_[:, k, :], self.identf[:], w_[:, k:k + 1], None, ALU.mult, None, R=[self.identf, w_], W=[dg_])
            for (b0, nb, s0, s1) in blocks:
                bk = pb[4 + (nblk % 2)]
                nblk += 1
                self.mm(bk[:, :nb], dg_[:, 2, :], x_[:, b0:b0 + nb], True, False, R=[dg_, x_], W=[bk])
                taps = (0, 1, 3, 4)
                for ti_, k in enumerate(taps):
                    sft = k - 2
                    a_ = max(b0, s0 - sft)
                    b_ = min(b0 + nb, s1 - sft)
                    self.mm(bk[:, a_ - b0:b_ - b0], dg_[:, k, :], x_[:, a_ + sft:b_ + sft], False, ti_ == len(taps) - 1,
                            R=[dg_, x_], W=[bk])
                self.act(sil[:, b0:b0 + nb], bk[:, :nb], AF.Silu, R=[bk], W=[sil])
            if cc < 4:
                dst = qT[cc] if cc < 2 else kT[cc - 2]
                scl = 0.125 if cc < 2 else 1.0
                for b0 in range(0, T, 512):
                    nb = min(512, T - b0)
                    bk = pb[(b0 // 512) % 2]
                    self.tt("pool", sqv[:, :nb], sil[:, b0:b0 + nb], sil[:, b0:b0 + nb], ALU.mult, R=[sil], W=[sqv])
                    self.mm(bk[:, :nb], obb[:], sqv[:, :nb], True, True, R=[obb, sqv], W=[bk])
                    self.act(rn[:, :nb], bk[:, :nb], AF.Sqrt, R=[bk], W=[rn], bias=1e-6, scale=1.0)
                    P.op("dve", lambda e, nb=nb: e.reciprocal(rn[:, :nb], rn[:, :nb]), R=[rn], W=[rn])
                    self.stt("dve", dst[:, b0:b0 + nb], sil[:, b0:b0 + nb], scl, rn[:, :nb], ALU.mult, ALU.mult,
                             R=[sil, rn], W=[dst])
                srcT = dst
            else:
                srcT = sil
            tok = qtok if cc < 2 else (ktok if cc < 4 else vtok)
            hp = cc % 2
            for t in range(NT):
                bT = pb[2 + (t % 2)]
                pT = bT[:].bitcast(BF16)
                P.op("pe", lambda e, t=t, pT=pT, srcT=srcT: e.transpose(pT[:, 0:128], srcT[:, t * 128:(t + 1) * 128], self.ident[:]),
                     R=[srcT, self.ident], W=[bT])
                self.cp("act" if t % 2 == 0 else "dve", tok[:, t, hp * 128:(hp + 1) * 128], pT[:, 0:128], R=[bT], W=[tok])

    def stage_gdn(self, l, ctx_out):
        P = self.P
        pb = self.pb
        qT = [P.sb([128, T], BF16, persist="mid") for _ in range(2)]
        kT = [P.sb([128, T], BF16, persist="mid") for _ in range(2)]
        qtok = P.sb([128, NT, 256], BF16, persist="mid")
        ktok = P.sb([128, NT, 256], BF16, persist="mid")
        vtok = P.sb([128, NT, 256], BF16, persist="mid")
        gg = P.sb([128, NT, 8], F32, persist="mid")
        beta = P.sb([128, NT, 8], F32, persist="mid")
        nbeta = P.sb([128, NT, 8], F32, persist="mid")
        obb = P.sb([128, 128], BF16)
        P.dma("pool", obb[:], self.c_gob[:], R=[self.c_gob], W=[obb])
        self._gdn_prep(l, qT, kT, qtok, ktok, vtok, gg, beta, nbeta, obb)
        P.flush()
        oacc = P.sb([128, NT, 256], F32)
        cst = {}
        for nm, src, shp in (("U", self.c_gU, [2, 128]), ("negU", self.c_gnegU, [2, 128]), ("NML", self.c_gNML, [2, 128]),
                             ("NMQ", self.c_gNMQ, [2, 128])):
            tl = P.sb([128, 2, 128], F32)
            P.dma("sp", tl[:], src.t.ap().rearrange("d p i -> p d i"), R=[src], W=[tl])
            cst[nm] = tl
        ind = P.sb([128, 2, 64], F32)
        P.dma("sp", ind[:], self.c_gind.t.ap().rearrange("c p m -> p c m"), R=[self.c_gind], W=[ind])
        obf = P.sb([128, 128], F32)
        P.dma("sp", obf[:], self.c_gob[:], R=[self.c_gob], W=[obf])
        negones = P.sb([128, 128], F32)
        self.ts("dve", negones[:], self.onesf[:], -1.0, None, ALU.mult, None, R=[self.onesf], W=[negones])
        S32 = P.sb([64, 4, 64], F32)
        S16 = P.sb([64, 4, 64], BF16)
        NB = 2
        rhs1 = [P.sb([128, 4, 128], F32) for _ in range(NB)]
        gbm = [P.sb([128, 4, 128], F32) for _ in range(NB)]
        t1 = [P.sb([128, 4, 128], F32) for _ in range(NB)]
        t2 = [P.sb([128, 4, 128], F32) for _ in range(NB)]
        XT = [P.sb([128, 4, 128], BF16) for _ in range(NB)]
        X = [P.sb([128, 4, 128], BF16) for _ in range(NB)]
        Rm = [P.sb([128, 4, 128], BF16) for _ in range(NB)]
        qkT = [P.sb([128, 4, 128], BF16) for _ in range(NB)]
        sml = [P.sb([128, 16], F32) for _ in range(NB)]
        GL = [P.sb([64, 8], F32) for _ in range(NB)]
        vb = [P.sb([128, 4, 64], BF16) for _ in range(NB)]
        kbg = [P.sb([128, 4, 64], BF16) for _ in range(NB)]
        kdec = [P.sb([128, 4, 64], BF16) for _ in range(NB)]
        qdec = [P.sb([128, 4, 64], BF16) for _ in range(NB)]
        uu = [P.sb([128, 4, 64], F32) for _ in range(NB)]
        wT = [P.sb([64, 4, 128], BF16) for _ in range(NB)]
        qdT = [P.sb([64, 4, 128], BF16) for _ in range(NB)]
        vn = [P.sb([128, 4, 64], BF16) for _ in range(2)]
        n = 0
        for d in range(2):
            order = list(range(NT)) if d == 0 else [1, 0] + list(range(NT - 1, 1, -1))
            P.op("dve", lambda e: e.memset(S32[:], 0.0), W=[S32])
            P.op("dve", lambda e: e.memset(S16[:], 0.0), W=[S16])
            U_ = cst["U"][:, d, :]
            nU_ = cst["negU"][:, d, :]
            RU = [cst["U"], cst["negU"]]
            for tl in order:
                if TRG < 2:
                    continue
                i = n % NB
                n += 1
                ts_ = slice(tl * 128, (tl + 1) * 128)
                gd = gg[:, tl, d * 4:(d + 1) * 4]
                bd = beta[:, tl, d * 4:(d + 1) * 4]
                nbd = nbeta[:, tl, d * 4:(d + 1) * 4]
                self.tt("dve", rhs1[i][:], U_.unsqueeze(1).to_broadcast([128, 4, 128]),
                        gd.unsqueeze(2).to_broadcast([128, 4, 128]), ALU.mult, R=[cst["U"], gg], W=[rhs1[i]])
                r1f = rhs1[i][:].rearrange("p h i -> p (h i)")
                self.mm(pb[0][:, :], self.onesf[:], r1f, True, True, R=[self.onesf, rhs1[i]], W=[pb[0]])
                if TRG < 2.2:
                    continue
                sb_ = pb[7]
                self.mm(sb_[:, 0:4], U_, gd, True, True, R=RU + [gg], W=[sb_])
                self.mm(sb_[:, 4:8], obf[:], gd, True, True, R=[obf, gg], W=[sb_])
                self.mm(sb_[0:64, 8:12], ind[:, 0, :], gd, True, True, R=[ind, gg], W=[sb_])
                self.mm(sb_[0:64, 12:16], ind[:, 1, :], gd, True, True, R=[ind, gg], W=[sb_])
                sm_ = sml[i]
                self.tt("dve", sm_[:, 4:8], sb_[:, 4:8], sb_[:, 0:4], ALU.subtract, R=[sb_], W=[sm_]) if False else None
                self.cp("dve", sm_[:, 0:8], sb_[:, 0:8], R=[sb_], W=[sm_])
                self.cp("dve", sm_[:, 12:16], sb_[:, 0:4], R=[sb_], W=[sm_])
                self.stt("dve", gbm[i][:], pb[0][:, :].rearrange("p (h j) -> p h j", j=128), -1.0,
                         sm_[:, 12:16].unsqueeze(2).to_broadcast([128, 4, 128]), ALU.mult, ALU.add,
                         R=[pb[0], sm_], W=[gbm[i]])
                self.tt("dve", sm_[:, 4:8], sm_[:, 4:8], sm_[:, 0:4], ALU.subtract, R=[sm_], W=[sm_])
                self.act(sm_[:, 0:8], sm_[:, 0:8], AF.Exp, R=[sm_], W=[sm_])
                self.act(GL[i][:], sb_[0:64, 8:16], AF.Exp, R=[sb_], W=[GL[i]])
                self.tt("dve", sm_[:, 8:12], sm_[:, 0:4], bd, ALU.mult, R=[sm_, beta], W=[sm_])
                if TRG < 2.3:
                    continue
                for h in range(4):
                    hp, hl = h // 2, h % 2
                    hs = slice(hl * 64, hl * 64 + 64)
                    self.mm(pb[2][:, h * 128:(h + 1) * 128], kT[hp][hs, ts_], kT[hp][hs, ts_], True, True, R=[kT[hp]], W=[pb[2]])
                    self.mm(pb[3][:, h * 128:(h + 1) * 128], kT[hp][hs, ts_], qT[hp][hs, ts_], True, True, R=[kT[hp], qT[hp]], W=[pb[3]])
                if TRG < 2.4:
                    continue
                self.stt("dve", t1[i][:], gbm[i][:], 0.0,
                         cst["NML"][:, d, :].unsqueeze(1).to_broadcast([128, 4, 128]), ALU.min, ALU.add,
                         R=[gbm[i], cst["NML"]], W=[t1[i]])
                self.act(t1[i][:], t1[i][:], AF.Exp, R=[t1[i]], W=[t1[i]])
                self.tt("dve", t1[i][:], t1[i][:], pb[2][:, :].rearrange("p (h j) -> p h j", j=128), ALU.mult, R=[t1[i], pb[2]], W=[t1[i]])
                self.tt("pool", XT[i][:], t1[i][:], nbd.unsqueeze(2).to_broadcast([128, 4, 128]), ALU.mult, R=[t1[i], nbeta], W=[XT[i]])
                if TRG < 2.5:
                    continue
                self.stt("dve", t2[i][:], gbm[i][:], 0.0,
                         cst["NMQ"][:, d, :].unsqueeze(1).to_broadcast([128, 4, 128]), ALU.max, ALU.subtract,
                         R=[gbm[i], cst["NMQ"]], W=[t2[i]])
                self.act(t2[i][:], t2[i][:], AF.Exp, R=[t2[i]], W=[t2[i]], scale=-1.0)
                self.tt("dve", qkT[i][:], t2[i][:], pb[3][:, :].rearrange("p (h j) -> p h j", j=128), ALU.mult, R=[t2[i], pb[3]], W=[qkT[i]])
                if TRG < 3:
                    continue
                b4 = pb[4]
                p4 = b4[:].bitcast(BF16)
                for h in range(4):
                    P.op("pe", lambda e, h=h, p4=p4, i=i: e.transpose(p4[:, h * 128:(h + 1) * 128], XT[i][:, h, :], self.ident[:]),
                         R=[XT[i], self.ident], W=[b4])
                self.cp("act", X[i][:].rearrange("p h j -> p (h j)"), p4[:, 0:512], R=[b4], W=[X[i]])
                self.tt("pool", Rm[i][:], X[i][:], self.ident[:].unsqueeze(1).to_broadcast([128, 4, 128]), ALU.add,
                        R=[X[i], self.ident], W=[Rm[i]])
                Y, YT = X[i], XT[i]
                for kk in range(1, 6):
                    if kk < 5:
                        for h in range(4):
                            self.mm(pb[0][:, h * 128:(h + 1) * 128], YT[:, h, :], Y[:, h, :], True, True, R=[Y, YT], W=[pb[0]])
                    for h in range(4):
                        self.mm(pb[1][:, h * 128:(h + 1) * 128], Y[:, h, :], YT[:, h, :], True, True, R=[Y, YT], W=[pb[1]])
                    if kk < 5:
                        self.cp("act", Y[:].rearrange("p h j -> p (h j)"), pb[0][:, :], R=[pb[0]], W=[Y])
                    self.cp("dve", YT[:].rearrange("p h j -> p (h j)"), pb[1][:, :], R=[pb[1]], W=[YT])
                    for h in range(4):
                        self.mm(pb[2][:, h * 128:(h + 1) * 128], YT[:, h, :], Rm[i][:, h, :], True, True, R=[YT, Rm[i]], W=[pb[2]])
                    self.tt("dve", Rm[i][:].rearrange("p h j -> p (h j)"), Rm[i][:].rearrange("p h j -> p (h j)"), pb[2][:, :], ALU.add,
                            R=[Rm[i], pb[2]], W=[Rm[i]])
                kv = ktok[:, tl, :].rearrange("p (h d) -> p h d", d=64)
                qv = qtok[:, tl, :].rearrange("p (h d) -> p h d", d=64)
                vv = vtok[:, tl, :].rearrange("p (h d) -> p h d", d=64)
                self.tt("pool", vb[i][:], vv, bd.unsqueeze(2).to_broadcast([128, 4, 64]), ALU.mult, R=[vtok, beta], W=[vb[i]])
                self.tt("pool", kbg[i][:], kv, sm_[:, 8:12].unsqueeze(2).to_broadcast([128, 4, 64]), ALU.mult, R=[ktok, sm_], W=[kbg[i]])
                self.tt("pool", kdec[i][:], kv, sm_[:, 4:8].unsqueeze(2).to_broadcast([128, 4, 64]), ALU.mult, R=[ktok, sm_], W=[kdec[i]])
                self.tt("pool", qdec[i][:], qv, sm_[:, 0:4].unsqueeze(2).to_broadcast([128, 4, 64]), ALU.mult, R=[qtok, sm_], W=[qdec[i]])
                for h in range(4):
                    self.mm(pb[3][:, h * 64:(h + 1) * 64], Rm[i][:, h, :], vb[i][:, h, :], True, True, R=[Rm[i], vb[i]], W=[pb[3]])
                self.cp("act", uu[i][:].rearrange("p h d -> p (h d)"), pb[3][:, 0:256], R=[pb[3]], W=[uu[i]])
                for h in range(4):
                    self.mm(pb[4][0:64, h * 128:(h + 1) * 128], kbg[i][:, h, :], Rm[i][:, h, :], True, True, R=[kbg[i], Rm[i]], W=[pb[4]])
                self.cp("dve", wT[i][:].rearrange("p h j -> p (h j)"), pb[4][0:64, :], R=[pb[4]], W=[wT[i]])
                for h in range(4):
                    P.op("pe", lambda e, h=h, p4=p4, i=i: e.transpose(p4[0:64, h * 128:(h + 1) * 128], qdec[i][:, h, :], self.ident[:]),
                         R=[qdec[i], self.ident], W=[b4])
                self.cp("act", qdT[i][:].rearrange("p h j -> p (h j)"), p4[0:64, 0:512], R=[b4], W=[qdT[i]])
                for c in ([0, 1] if d == 0 else [1, 0]):
                    if TRG < 4:
                        continue
                    cs = slice(c * 64, (c + 1) * 64)
                    vn_ = vn[c]
                    for h in range(4):
                        self.mm(pb[5][:, h * 64:(h + 1) * 64], wT[i][:, h, :], S16[:, h, :], True, True, R=[wT[i], S16], W=[pb[5]])
                    self.tt("dve", vn_[cs].rearrange("p h d -> p (h d)"), uu[i][cs].rearrange("p h d -> p (h d)"), pb[5][cs, 0:256],
                            ALU.subtract, R=[uu[i], pb[5]], W=[vn_])
                    for h in range(4):
                        self.mm(pb[6][:, h * 64:(h + 1) * 64], qdT[i][:, h, :], S16[:, h, :], True, False, R=[qdT[i], S16], W=[pb[6]])
                        self.mm(pb[6][:, h * 64:(h + 1) * 64], qkT[i][cs, h, :], vn_[cs, h, :], False, True, R=[qkT[i], vn_], W=[pb[6]])
                    if d == 0:
                        self.cp("act", oacc[cs, tl, :], pb[6][cs, 0:256], R=[pb[6]], W=[oacc])
                    else:
                        self.tt("dve", oacc[cs, tl, :], oacc[cs, tl, :], pb[6][cs, 0:256], ALU.add, R=[oacc, pb[6]], W=[oacc])
                    for h in range(4):
                        self.mm(pb[7][0:64, 256 + h * 64:256 + (h + 1) * 64], kdec[i][cs, h, :], vn_[cs, h, :], True, True,
                                R=[kdec[i], vn_], W=[pb[7]])
                    self.tt("dve", S32[:], S32[:], GL[i][:, c * 4:(c + 1) * 4].unsqueeze(2).to_broadcast([64, 4, 64]), ALU.mult,
                            R=[S32, GL[i]], W=[S32])
                    self.tt("dve", S32[:].rearrange("p h d -> p (h d)"), S32[:].rearrange("p h d -> p (h d)"), pb[7][0:64, 256:512], ALU.add,
                            R=[S32, pb[7]], W=[S32])
                    self.cp("act", S16[:], S32[:], R=[S32], W=[S16])
        gdn_g = P.sb([128, 64], F32)
        P.dma("sp", gdn_g[:], self.dn_norm_g.t.ap()[l].partition_broadcast(128), R=[self.dn_norm_g], W=[gdn_g])
        gt = [P.sb([128, 256], BF16) for _ in range(2)]
        gs = [P.sb([128, 256], F32) for _ in range(2)]
        sq2 = [P.sb([128, 4, 64], F32) for _ in range(2)]
        s8 = [P.sb([128, 8], F32) for _ in range(2)]
        ob = [P.sb([128, 256], BF16) for _ in range(2)]
        tiles = list(range(0 if ctx_out else 2, NT))
        for nn, t in enumerate(tiles):
            i = nn % 2
            rs = slice(t * 128, (t + 1) * 128)
            P.dma("sp", gt[i][:], self.gated[rs, :], R=[self.gated], W=[gt[i]])
            self.act(gs[i][:], gt[i][:], AF.Silu, R=[gt[i]], W=[gs[i]])
            ov = oacc[:, t, :].rearrange("p (h d) -> p h d", d=64)
            self.tt("pool", sq2[i][:], ov, ov, ALU.mult, R=[oacc], W=[sq2[i]])
            P.op("dve", lambda e, i=i: e.reduce_sum(s8[i][:, 0:4], sq2[i][:], AX.X), R=[sq2[i]], W=[s8[i]])
            self.rsqrt_mean(s8[i][:, 4:8], s8[i][:, 0:4], 64.0, 1e-6, R=[s8[i]], W=[s8[i]])
            self.tt("dve", sq2[i][:], ov, s8[i][:, 4:8].unsqueeze(2).to_broadcast([128, 4, 64]), ALU.mult, R=[oacc, s8[i]], W=[sq2[i]])
            self.tt("dve", sq2[i][:], sq2[i][:], gdn_g[:].unsqueeze(1).to_broadcast([128, 4, 64]), ALU.mult, R=[sq2[i], gdn_g], W=[sq2[i]])
            self.tt("dve", ob[i][:], sq2[i][:].rearrange("p h d -> p (h d)"), gs[i][:], ALU.mult, R=[sq2[i], gs[i]], W=[ob[i]])
            P.dma("pool", self.mix[rs, 256:512], ob[i][:], R=[ob[i]], W=[self.mix])
        P.flush()
        P.release_mid()

def build(nlayers=DEPTH, dbg=False, stages=None):
    B = Builder(nlayers, dbg, stages)
    B.declare()
    B.stage_init()
    for l in range(nlayers):
        if stages is not None and "nomod" in stages:
            continue
        B.stage_mod(l)
        ctx_out = l < DEPTH - 1
        if B.want("inproj"):
            B.stage_inproj(l)
        if B.want("na"):
            B.stage_na(l, ctx_out)
        if B.want("diff"):
            B.stage_diff(l, ctx_out)
        if B.want("fft"):
            B.stage_fft(l, ctx_out)
        if B.want("gdn"):
            B.stage_gdn(l, ctx_out)
        if stages is not None and "inject_dn" in stages:
            inj = B.din("dn_inj", [T, 256])
            B.P.dma("pool", B.mix[:, 256:512], inj[:], R=[inj], W=[B.mix])
            B.P.flush()
        if B.want("outproj"):
            B.stage_outproj(l, ctx_out)
        if B.want("moe"):
            B.stage_route(l, ctx_out)
            B.stage_ffn(l, ctx_out)
            B.stage_combine(l, ctx_out, final=(l == nlayers - 1))
    B.P.close()
    return B


_CONST = None


def prep_shared(inputs):
    global _CONST
    if _CONST is None:
        _CONST = const_tables()
    sh = dict(_CONST)
    f = lambda k: np.ascontiguousarray(np.asarray(inputs[k], dtype=np.float32))
    for k in ("w_mod", "b_mod", "norm1_g", "norm2_g", "ft_w", "w_out", "w_router", "w_gate", "w_up", "w_down",
              "final_norm_g", "df_norm_g", "dn_norm_g", "dn_conv_w"):
        sh[k] = f(k)
    sh["w_in_p"] = np.ascontiguousarray(f("w_in")[:, :, in_proj_perm()])
    rp = f("na_rpb")
    pad = np.zeros((DEPTH, 4, 15, 128), np.float32)
    pad[..., 48:79] = rp[:, :, ::-1, ::-1]
    sh["rpbpad"] = pad
    sh["df_lambda"] = f("df_lambda").reshape(DEPTH, 128)
    sh["dn_a_log"] = f("dn_a_log").reshape(DEPTH, 8)
    sh["dn_dt_bias"] = f("dn_dt_bias").reshape(DEPTH, 8)
    return sh


def prep_sample(inputs, b):
    xc = np.concatenate([np.asarray(inputs["ctx"][b], np.float32), np.asarray(inputs["x"][b], np.float32)], 0)
    cc = np.stack([np.asarray(inputs["c"][b], np.float32), np.asarray(inputs["c_ctx"], np.float32)], 0)
    return {"xc": np.ascontiguousarray(xc), "cc": np.ascontiguousarray(cc)}


_BUILT = {}
NCORES = 4


def kernel(**inputs):
    if "B" not in _BUILT:
        _BUILT["B"] = build()
    B = _BUILT["B"]
    sh = prep_shared(inputs)
    sh = {k: v for k, v in sh.items() if k in B.inputs}
    in_maps = []
    for core in range(NCORES):
        m = dict(sh)
        m.update(prep_sample(inputs, core % 4))
        in_maps.append(m)
    res = run_bass_kernel_spmd(B.nc, in_maps, core_ids=list(range(NCORES)))
    out = np.stack([np.asarray(res.results[b]["out"], dtype=np.float32) for b in range(4)], 0)
    return out
```

```python
import math
import os
TR = int(os.environ.get('K_TR', '9'))
TRG = float(os.environ.get('K_TRG', '9'))
from contextlib import ExitStack

import numpy as np
import ml_dtypes
import concourse.bass as bass
import concourse.mybir as mybir
from concourse.bass_utils import run_bass_kernel_spmd

F32 = mybir.dt.float32
BF16 = mybir.dt.bfloat16
I32 = mybir.dt.int32
ALU = mybir.AluOpType
AF = mybir.ActivationFunctionType
AX = mybir.AxisListType

D = 1024
S = 4096
L = 256
T = S + L
NT = T // 128
DEPTH = 4
NE = 16
CAP_L = 512
CAP_C = 32
ESL = CAP_L + CAP_C
TRASH = NE * ESL
OOB = 1 << 20
GW = 64
VW = 72
EPOCH = 30000
NDMASEM = 8


class Buf:
    __slots__ = ("t", "lw", "rd", "name", "excl", "pe_rg", "pe_last")

    def __init__(self, t, name=""):
        self.t = t
        self.lw = None
        self.rd = {}
        self.name = name
        self.excl = False
        self.pe_rg = None
        self.pe_last = None

    def __getitem__(self, k):
        return self.t[k]


class Op:
    __slots__ = ("eng", "fn", "deps", "kind", "idx", "signal", "sigval")

    def __init__(self, eng, fn, deps, kind, idx):
        self.eng, self.fn, self.deps, self.kind, self.idx = eng, fn, deps, kind, idx
        self.signal = False
        self.sigval = None


class Prog:
    ENGS = ("pe", "act", "dve", "pool", "sp")

    def __init__(self, nc):
        self.nc = nc
        self.es = ExitStack()
        self.ph = ExitStack()
        self.mid = ExitStack()
        self.bufs = []
        self.ops = {e: [] for e in self.ENGS}
        self.nsig = {e: 0 for e in self.ENGS}
        self.ndma = {e: 0 for e in self.ENGS}
        self.cmp_sems = {}
        self.dma_sems = {}
        self.bar_sems = {}
        self.nbar = 0
        self.n = 0
        self.ninst = 0
        self.handles = {"pe": nc.tensor, "act": nc.scalar, "dve": nc.vector, "pool": nc.gpsimd, "sp": nc.sync}

    def _reg(self, t, name):
        b = Buf(t, name)
        self.bufs.append(b)
        return b

    def sb(self, shape, dt, persist=False):
        self.n += 1
        name = f"sb{self.n}"
        st = self.mid if persist == "mid" else (self.es if persist else self.ph)
        return self._reg(st.enter_context(self.nc.sbuf_tensor(name, list(shape), dt)), name)

    def release_mid(self):
        self.mid.close()
        self.mid = ExitStack()

    def ps(self, shape, dt=F32):
        self.n += 1
        name = f"ps{self.n}"
        b = self._reg(self.es.enter_context(self.nc.psum_tensor(name, list(shape), dt)), name)
        b.excl = True
        return b

    def dram(self, name, shape, dt, kind="Internal"):
        return self._reg(self.nc.dram_tensor(name, list(shape), dt, kind=kind), name)

    def view(self, name=""):
        return self._reg(None, name)

    def op(self, eng, fn, R=(), W=(), kind="cmp", rg=None):
        lst = self.ops[eng]
        idx = len(lst)
        deps = set()
        W = list(W) + [b for b in R if b.excl]
        R = [b for b in R if not b.excl]
        extra = set()
        if eng == "pe":
            if rg is None:
                rg = frozenset((0, 1, 2, 3))
            for b in W:
                if b.excl:
                    if b.pe_rg is not None and b.pe_last is not None and not (b.pe_rg & rg):
                        extra.add(b.pe_last)
                    b.pe_rg = rg
                    b.pe_last = ("pe", idx)
        for b in R:
            if b.lw is not None:
                deps.add(b.lw)
        for b in W:
            if b.lw is not None:
                deps.add(b.lw)
            for e2, i2 in b.rd.items():
                deps.add((e2, i2))
        if eng == "pe":
            deps = {d for d in deps if d[0] != "pe"}
        deps |= extra
        o = Op(eng, fn, deps, kind, idx)
        lst.append(o)
        for b in R:
            b.rd[eng] = idx
        for b in W:
            b.lw = (eng, idx)
            b.rd = {}
        return o

    def dma(self, eng, out, in_, R=(), W=(), **kw):
        return self.op(eng, lambda e: e.dma_start(out=out, in_=in_, **kw), R=R, W=W, kind="dma")

    def _csem(self, e, ep):
        if (e, ep) not in self.cmp_sems:
            self.cmp_sems[(e, ep)] = self.es.enter_context(self.nc.semaphore(f"c_{e}_{ep}"))
        return self.cmp_sems[(e, ep)]

    def _dsem(self, e, j):
        if (e, j) not in self.dma_sems:
            self.dma_sems[(e, j)] = self.es.enter_context(self.nc.semaphore(f"d_{e}_{j}"))
        return self.dma_sems[(e, j)]

    def _bsem(self, e):
        if e not in self.bar_sems:
            self.bar_sems[e] = self.es.enter_context(self.nc.semaphore(f"b_{e}"))
        return self.bar_sems[e]

    def flush(self):
        nc = self.nc
        ops = self.ops
        for e in self.ENGS:
            for o in ops[e]:
                for (e2, i2) in o.deps:
                    ops[e2][i2].signal = True
            for o in reversed(ops[e]):
                if o.kind == "cmp":
                    o.signal = True
                    break
        for e in self.ENGS:
            for o in ops[e]:
                if o.kind == "dma":
                    o.sigval = ("dma", self.ndma[e])
                    self.ndma[e] += 1
                elif o.signal:
                    o.sigval = ("cmp", self.nsig[e])
                    self.nsig[e] += 1
        self.nbar += 1
        nbar = self.nbar

        def run(ename, eh):
            waited_cmp = {}
            waited_dma = set()
            last_cmp = None
            my_dmas = []
            for o in ops[ename]:
                best = {}
                for (e2, i2) in o.deps:
                    d = ops[e2][i2]
                    kind, n = d.sigval
                    if kind == "dma":
                        if (e2, n) in waited_dma:
                            continue
                        waited_dma.add((e2, n))
                        eh.wait_ge(self._dsem(e2, n % NDMASEM), 16 * (n // NDMASEM + 1))
                        self.ninst += 1
                    else:
                        if waited_cmp.get(e2, -1) >= n:
                            continue
                        if best.get(e2, -1) < n:
                            best[e2] = n
                for e2, n in best.items():
                    waited_cmp[e2] = n
                    eh.wait_ge(self._csem(e2, n // EPOCH), (n % EPOCH) + 1)
                    self.ninst += 1
                if o.kind == "dma":
                    n = o.sigval[1]
                    if n >= NDMASEM and (ename, n - NDMASEM) not in waited_dma:
                        eh.wait_ge(self._dsem(ename, n % NDMASEM), 16 * (n // NDMASEM))
                        waited_dma.add((ename, n - NDMASEM))
                    ins = o.fn(eh)
                    ins.then_inc(self._dsem(ename, n % NDMASEM), 16)
                    my_dmas.append(n)
                else:
                    ins = o.fn(eh)
                    if o.signal:
                        n = o.sigval[1]
                        ins.then_inc(self._csem(ename, n // EPOCH), 1)
                        last_cmp = n
                self.ninst += 1
            for n in my_dmas[-NDMASEM:]:
                if (ename, n) not in waited_dma:
                    eh.wait_ge(self._dsem(ename, n % NDMASEM), 16 * (n // NDMASEM + 1))
            if last_cmp is not None and waited_cmp.get(ename, -1) < last_cmp:
                eh.wait_ge(self._csem(ename, last_cmp // EPOCH), (last_cmp % EPOCH) + 1)
            eh.sem_inc(self._bsem(ename), 1)
            for e2 in self.ENGS:
                if e2 != ename:
                    eh.wait_ge(self._bsem(e2), nbar)

        with nc.Block() as block:
            @block.sync
            def _(e):
                run("sp", e)

            @block.scalar
            def _(e):
                run("act", e)

            @block.vector
            def _(e):
                run("dve", e)

            @block.tensor
            def _(e):
                run("pe", e)

            @block.gpsimd
            def _(e):
                run("pool", e)
        self.ops = {e: [] for e in self.ENGS}
        for b in self.bufs:
            b.lw = None
            b.rd = {}
            b.pe_rg = None
            b.pe_last = None
        self.ph.close()
        self.ph = ExitStack()

    def close(self):
        self.ph.close()
        self.es.close()


IN_OFF = {"naq": 0, "nak": 256, "nav": 512, "dn": 768, "dna": 1536, "dnb": 1544, "dng": 1552,
          "dfq": 1808, "dfk": 2064, "dfv": 2320, "ftu": 2576}


def _swap_cols(base):
    idx = []
    for hm in range(8):
        for f in range(32):
            idx.append(base + hm * 32 + (f ^ 1))
    return idx


def in_proj_perm():
    p = []
    p += list(range(IN_OFF["naq"], IN_OFF["naq"] + 256))
    p += list(range(IN_OFF["nak"], IN_OFF["nak"] + 256))
    p += list(range(IN_OFF["dn"], IN_OFF["dn"] + 768))
    p += list(range(IN_OFF["dfq"], IN_OFF["dfq"] + 256))
    p += _swap_cols(IN_OFF["dfq"])
    p += list(range(IN_OFF["dfk"], IN_OFF["dfk"] + 256))
    p += _swap_cols(IN_OFF["dfk"])
    p += list(range(IN_OFF["ftu"], IN_OFF["ftu"] + 256))
    assert len(p) == 2560
    p += list(range(IN_OFF["nav"], IN_OFF["nav"] + 256))
    p += list(range(IN_OFF["dfv"], IN_OFF["dfv"] + 256))
    p += list(range(IN_OFF["dng"], IN_OFF["dng"] + 256))
    p += list(range(IN_OFF["dna"], IN_OFF["dna"] + 16))
    assert len(p) == 3344
    return np.array(p)


NWIN = 3344
A_PLAIN = {0: 0, 1: 128, 2: 256, 3: 384, 4: 512, 5: 640, 6: 768, 7: 896, 8: 1024, 9: 1152, 18: 1792, 19: 1920}
A_ROPE = {10: (12, 1280), 11: (13, 1408), 14: (16, 1536), 15: (17, 1664)}


def const_tables():
    c = {}
    c["ident"] = np.eye(128, dtype=np.float32)
    t = np.arange(S)
    pos = np.stack([t // GW, t % GW], -1).astype(np.float32)
    inv = (10000.0 ** (-np.arange(8, dtype=np.float32) / 8)).astype(np.float32)
    ang = (pos[:, :, None] * inv).reshape(S, 16)
    cosf = np.repeat(np.cos(ang), 2, axis=1)
    sinf = np.repeat(np.sin(ang), 2, axis=1)
    sgn = np.tile(np.array([-1.0, 1.0], np.float32), 16)
    sinf = sinf * sgn
    cosT = np.ones((32, T), np.float32)
    sinT = np.zeros((32, T), np.float32)
    cosT[:, L:] = cosf.T
    sinT[:, L:] = sinf.T
    c["cosT"] = np.tile(cosT, (4, 1)).astype(np.float32)
    c["sinT"] = np.tile(sinT, (4, 1)).astype(np.float32)
    cq = np.arange(GW)
    c0 = np.clip(cq - 8, 0, GW - 16)
    inwin = (cq[None, :] >= c0[:, None]) & (cq[None, :] < c0[:, None] + 16)
    c["colwinT"] = np.tile(inwin.T.astype(np.float32), (2, 1))
    def dft(n, scale):
        k = np.arange(n)
        ph = (np.outer(k, k) % n).astype(np.float64) * (2 * np.pi / n)
        return (np.cos(ph) * scale), (np.sin(ph) * scale)
    c64, s64 = dft(64, 1.0 / 8)
    bdc = np.zeros((128, 128)); bds = np.zeros((128, 128))
    for i in range(2):
        bdc[i * 64:(i + 1) * 64, i * 64:(i + 1) * 64] = c64
        bds[i * 64:(i + 1) * 64, i * 64:(i + 1) * 64] = s64
    c["bdc"] = bdc.astype(np.float32)
    c["bds"] = bds.astype(np.float32)
    cS, sS = dft(S, 1.0 / 64)
    def tile_tab(m, n):
        nt = n // 128
        return np.ascontiguousarray(m.reshape(nt, 128, nt, 128).transpose(2, 1, 0, 3)).astype(ml_dtypes.bfloat16)
    c["dftc"] = tile_tab(cS, S)
    c["dfts"] = tile_tab(sS, S)
    cL, sL = dft(L, 1.0 / 16)
    c["dftc_c"] = tile_tab(cL, L)
    c["dfts_c"] = tile_tab(sL, L)
    c["tri"] = np.triu(np.ones((128, 128), np.float32))
    c["ones"] = np.ones((128, 128), np.float32)
    base = np.zeros((128, NT, NE), np.float32)
    for e in range(NE):
        base[:, :2, e] = e * ESL + CAP_L
        base[:, 2:, e] = e * ESL
    c["slotbase"] = base.reshape(128, NT * NE) - OOB
    ii = np.arange(128)
    same = (ii[:, None] // 64) == (ii[None, :] // 64)
    Uf = (same & (ii[:, None] <= ii[None, :])).astype(np.float32)
    Ub = (same & (ii[:, None] >= ii[None, :])).astype(np.float32)
    NEG = -30000.0
    g = {}
    g["U"] = np.stack([Uf, Ub])
    g["negU"] = -g["U"]
    nml_f = np.where(same & (ii[:, None] > ii[None, :]), 0.0, NEG)
    nml_b = np.where(same & (ii[:, None] < ii[None, :]), 0.0, NEG)
    nmq_f = np.where(same & (ii[None, :] >= ii[:, None]), 0.0, NEG)
    nmq_b = np.where(same & (ii[None, :] <= ii[:, None]), 0.0, NEG)
    g["NML"] = np.stack([nml_f, nml_b])
    g["NMQ"] = np.stack([nmq_f, nmq_b])
    ind = np.zeros((2, 128, 64), np.float32)
    ind[0, :64, :] = 1.0
    ind[1, 64:, :] = 1.0
    c["gdn_U"] = g["U"].astype(np.float32)
    c["gdn_negU"] = g["negU"].astype(np.float32)
    c["gdn_NML"] = g["NML"].astype(np.float32)
    c["gdn_NMQ"] = g["NMQ"].astype(np.float32)
    c["gdn_ind"] = ind
    c["gdn_ob"] = same.astype(np.float32)
    return c


class Builder:
    def __init__(self, nlayers=DEPTH, dbg=False, stages=None):
        self.nlayers = nlayers
        self.dbg = dbg
        self.stages = stages
        self.nc = bass.Bass("TRN2", target_bir_lowering=False)
        self.P = Prog(self.nc)
        self.inputs = {}

    def want(self, s):
        return self.stages is None or s in self.stages

    def din(self, name, shape, dt=F32):
        b = self.P.dram(name, shape, dt, kind="ExternalInput")
        self.inputs[name] = b
        return b

    def dscr(self, name, shape, dt, out=False):
        return self.P.dram(name, shape, dt, kind="ExternalOutput" if (out or self.dbg) else "Internal")

    def declare(self):
        P = self.P
        nl = DEPTH
        self.xc = self.din("xc", [T, D])
        self.cc = self.din("cc", [2, D])
        self.w_mod = self.din("w_mod", [nl, D, 6 * D])
        self.b_mod = self.din("b_mod", [nl, 6 * D])
        self.norm1_g = self.din("norm1_g", [nl, D])
        self.norm2_g = self.din("norm2_g", [nl, D])
        self.w_in = self.din("w_in_p", [nl, D, NWIN])
        self.rpbpad = self.din("rpbpad", [nl, 4, 15, 128])
        self.ft_w = self.din("ft_w", [nl, 256, 256])
        self.w_out = self.din("w_out", [nl, D, D])
        self.w_router = self.din("w_router", [nl, D, NE])
        if self.want("moe"):
            self.w_gate = self.din("w_gate", [nl, NE, D, 2 * D])
            self.w_up = self.din("w_up", [nl, NE, D, 2 * D])
            self.w_down = self.din("w_down", [nl, NE, 2 * D, D])
        self.final_g = self.din("final_norm_g", [D])
        self.df_lambda = self.din("df_lambda", [nl, 128])
        self.df_norm_g = self.din("df_norm_g", [nl, 64])
        self.dn_norm_g = self.din("dn_norm_g", [nl, 64])
        self.dn_conv_w = self.din("dn_conv_w", [nl, 5, 768])
        self.dn_a_log = self.din("dn_a_log", [nl, 8])
        self.dn_dt_bias = self.din("dn_dt_bias", [nl, 8])
        self.c_ident = self.din("ident", [128, 128])
        self.c_cosT = self.din("cosT", [128, T])
        self.c_sinT = self.din("sinT", [128, T])
        self.c_colwinT = self.din("colwinT", [128, 64])
        self.c_bdc = self.din("bdc", [128, 128])
        self.c_bds = self.din("bds", [128, 128])
        if self.want("fft"):
            self.c_dftc = self.din("dftc", [32, 128, 32, 128], BF16)
            self.c_dfts = self.din("dfts", [32, 128, 32, 128], BF16)
        self.c_dftc_c = self.din("dftc_c", [2, 128, 2, 128], BF16)
        self.c_dfts_c = self.din("dfts_c", [2, 128, 2, 128], BF16)
        self.c_tri = self.din("tri", [128, 128])
        self.c_ones = self.din("ones", [128, 128])
        self.c_slotbase = self.din("slotbase", [128, NT * NE])
        self.c_gU = self.din("gdn_U", [2, 128, 128])
        self.c_gnegU = self.din("gdn_negU", [2, 128, 128])
        self.c_gNML = self.din("gdn_NML", [2, 128, 128])
        self.c_gNMQ = self.din("gdn_NMQ", [2, 128, 128])
        self.c_gind = self.din("gdn_ind", [2, 128, 64])
        self.c_gob = self.din("gdn_ob", [128, 128])
        self.out = P.dram("out", [S, D], F32, kind="ExternalOutput")
        self.xres = self.dscr("xres", [T, D], F32)
        self.projT = self.dscr("projT", [2048, T], BF16)
        self.navd = self.dscr("navd", [T, 4 * VW], BF16)
        self.dfvd = self.dscr("dfvd", [T, 4 * VW], BF16)
        self.gated = self.dscr("gated", [T, 256], BF16)
        self.abd = self.dscr("abd", [T, 16], F32)
        self.mix = self.dscr("mix", [T, D], BF16)
        self.h2d = self.dscr("h2d", [T, D], BF16)
        self.xg = self.dscr("xg", [TRASH + 128, D], BF16)
        self.ybuf = self.dscr("ybuf", [TRASH + 128, D], F32)
        self.rpbz = self.dscr("rpbz", [60 * 64 * 129 + 256], F32)
        self.ident = P.sb([128, 128], BF16, persist=True)
        self.identf = P.sb([128, 128], F32, persist=True)
        self.onesf = P.sb([128, 128], F32, persist=True)
        self.trif = P.sb([128, 128], F32, persist=True)
        self.mT = P.sb([128, 48, 2], F32, persist=True)
        self.gs1T = P.sb([128, 8, 2], F32, persist=True)
        self.g1T = P.sb([128, 8], F32, persist=True)
        self.g2T = P.sb([128, 8], F32, persist=True)
        self.aff = P.sb([128, NT, NE], F32, persist=True)
        self.desti = P.sb([128, NT, NE], I32, persist=True)
        self.gatev = P.sb([128, NT, NE], F32, persist=True)
        self.pb = [P.ps([128, 512], F32) for _ in range(8)]

    def stage_init(self):
        P = self.P
        P.dma("pool", self.ident[:], self.c_ident[:], R=[self.c_ident], W=[self.ident])
        P.dma("sp", self.identf[:], self.c_ident[:], R=[self.c_ident], W=[self.identf])
        P.dma("sp", self.onesf[:], self.c_ones[:], R=[self.c_ones], W=[self.onesf])
        P.dma("sp", self.trif[:], self.c_tri[:], R=[self.c_tri], W=[self.trif])
        for i in range(2):
            r0 = i * (T // 2)
            P.dma("sp", self.xres[r0:r0 + T // 2, :], self.xc[r0:r0 + T // 2, :], R=[self.xc], W=[self.xres])
        z = P.sb([128, D], F32)
        P.op("dve", lambda e: e.memset(z[:], 0.0), W=[z])
        P.dma("sp", self.ybuf[TRASH:TRASH + 128, :], z[:], R=[z], W=[self.ybuf])
        P.flush()

    def stage_mod(self, l):
        P = self.P
        pb = self.pb
        sT = P.sb([128, 8, 2], F32)
        for r in range(2):
            P.dma("sp", sT[:, :, r], self.cc.t.ap()[r].rearrange("(c p) -> p c", p=128), R=[self.cc], W=[sT],
                  allow_slow_non_contiguous=True)
        P.op("act", lambda e: e.activation(sT[:], sT[:], AF.Silu), R=[sT], W=[sT])
        bT = P.sb([128, 48], F32)
        P.dma("sp", bT[:], self.b_mod.t.ap()[l].rearrange("(c p) -> p c", p=128), R=[self.b_mod], W=[bT],
              allow_slow_non_contiguous=True)
        P.dma("sp", self.g1T[:], self.norm1_g.t.ap()[l].rearrange("(c p) -> p c", p=128), R=[self.norm1_g],
              W=[self.g1T], allow_slow_non_contiguous=True)
        P.dma("sp", self.g2T[:], self.norm2_g.t.ap()[l].rearrange("(c p) -> p c", p=128), R=[self.norm2_g],
              W=[self.g2T], allow_slow_non_contiguous=True)
        wt = [P.sb([128, 8, 512], F32) for _ in range(2)]
        acc = pb[0]
        for n in range(12):
            w = wt[n % 2]
            P.dma("sp", w[:], self.w_mod.t.ap()[l, :, n * 512:(n + 1) * 512].rearrange("(k p) n -> p k n", p=128),
                  R=[self.w_mod], W=[w])
            for j in range(4):
                col = n * 4 + j
                for k in range(8):
                    P.op("pe", lambda e, w=w, j=j, k=k, col=col: e.matmul(
                        acc[:, col * 2:col * 2 + 2], w[:, k, j * 128:(j + 1) * 128], sT[:, k, :],
                        start=(k == 0), stop=(k == 7)), R=[w, sT], W=[acc])
        P.op("dve", lambda e: e.tensor_tensor(
            self.mT[:], acc[:, 0:96].rearrange("p (c r) -> p c r", r=2),
            bT[:].unsqueeze(2).to_broadcast([128, 48, 2]), ALU.add), R=[acc, bT], W=[self.mT])
        P.op("dve", lambda e: e.tensor_scalar(self.gs1T[:], self.mT[:, 8:16, :], 1.0, None, ALU.add),
             R=[self.mT], W=[self.gs1T])
        P.op("dve", lambda e: e.tensor_tensor(self.gs1T[:], self.gs1T[:],
                                              self.g1T[:].unsqueeze(2).to_broadcast([128, 8, 2]), ALU.mult),
             R=[self.gs1T, self.g1T], W=[self.gs1T])
        if self.dbg:
            dm = self.dscr(f"dbg_mT{l}", [128, 96], F32)
            P.dma("sp", dm[:], self.mT[:].rearrange("p c r -> p (c r)"), R=[self.mT], W=[dm])
        P.flush()

    def bcast_vec(self, dst, srcT_fn, R):
        P = self.P
        dg = P.sb([128, 128], F32)
        for c in range(8):
            bank = self.pb[4 + (c % 2)]
            P.op("dve", lambda e, c=c: e.tensor_scalar(dg[:], self.identf[:], srcT_fn(c), None, ALU.mult),
                 R=[self.identf] + R, W=[dg])
            P.op("pe", lambda e, bank=bank: e.matmul(bank[:, 0:128], self.onesf[:], dg[:], start=True, stop=True),
                 R=[dg, self.onesf], W=[bank])
            P.op("act", lambda e, c=c, bank=bank: e.copy(dst[:, c * 128:(c + 1) * 128], bank[:, 0:128]),
                 R=[bank], W=[dst])

    def stage_inproj(self, l):
        P = self.P
        pb = self.pb
        win = P.sb([128, 8, NWIN], BF16)
        for k in range(8):
            P.dma("pool", win[:, k, :], self.w_in.t.ap()[l, k * 128:(k + 1) * 128, :], R=[self.w_in], W=[win])
        xt = [P.sb([128, D], F32) for _ in range(4)]
        xn = [P.sb([128, D], BF16) for _ in range(4)]
        junk = P.sb([128, D], BF16)
        ss = [P.sb([128, 2], F32) for _ in range(4)]
        hT = P.sb([128, 8, 512], BF16)
        cosb = P.sb([128, 512], F32)
        sinb = P.sb([128, 512], F32)
        stg = [P.sb([128, 512], BF16) for _ in range(4)]
        r1 = [P.sb([128, 512], F32) for _ in range(2)]
        r2 = [P.sb([128, 512], F32) for _ in range(2)]
        vst = [P.sb([128, 4, VW], BF16) for _ in range(2)]
        fst = [P.sb([128, 4, VW], BF16) for _ in range(2)]
        gst = [P.sb([128, 256], BF16) for _ in range(2)]
        ast = [P.sb([128, 16], F32) for _ in range(2)]
        for b_ in vst + fst:
            P.op("dve", lambda e, b_=b_: e.memset(b_[:], 1.0), W=[b_])
        blocks = [(0, 2, 1)] + [(2 + 4 * i, 4, 0) for i in range(8)]
        nst = 0
        for (t0, nt, s) in blocks:
            ntok = nt * 128
            c0 = t0 * 128
            P.dma("sp", cosb[:, :ntok], self.c_cosT[:, c0:c0 + ntok], R=[self.c_cosT], W=[cosb])
            P.dma("sp", sinb[:, :ntok], self.c_sinT[:, c0:c0 + ntok], R=[self.c_sinT], W=[sinb])
            for i in range(nt):
                t = t0 + i
                P.dma("sp", xt[i][:], self.xres[t * 128:(t + 1) * 128, :], R=[self.xres], W=[xt[i]])
                P.op("dve", lambda e, i=i: e.memset(ss[i][:], 0.0), W=[ss[i]])
                P.op("act", lambda e, i=i: e.activation(junk[:], xt[i][:], AF.Square, accum_out=ss[i][:, 0:1]),
                     R=[xt[i], ss[i]], W=[junk, ss[i]])
                P.op("act", lambda e, i=i: e.activation(ss[i][:, 1:2], ss[i][:, 0:1], AF.Sqrt, bias=1e-6, scale=1.0 / D),
                     R=[ss[i]], W=[ss[i]])
                P.op("dve", lambda e, i=i: e.reciprocal(ss[i][:, 1:2], ss[i][:, 1:2]), R=[ss[i]], W=[ss[i]])
                P.op("act", lambda e, i=i: e.activation(xn[i][:], xt[i][:], AF.Copy, scale=ss[i][:, 1:2]),
                     R=[xt[i], ss[i]], W=[xn[i]])
            if TR < 2:
                continue
            for c in range(8):
                bank = pb[c % 2]
                pT = bank[:].bitcast(BF16)
                for i in range(nt):
                    P.op("pe", lambda e, i=i, c=c, pT=pT: e.transpose(
                        pT[:, i * 128:(i + 1) * 128], xn[i][:, c * 128:(c + 1) * 128], self.ident[:]),
                        R=[xn[i], self.ident], W=[bank])
                P.op("act", lambda e, c=c, pT=pT, s=s, ntok=ntok: e.activation(
                    hT[:, c, :ntok], pT[:, :ntok], AF.Identity, scale=self.gs1T[:, c, s:s + 1],
                    bias=self.mT[:, c, s:s + 1]), R=[bank, self.gs1T, self.mT], W=[hT])
            if TR < 3:
                continue
            def mm_chunk(j, bank):
                for k in range(8):
                    P.op("pe", lambda e, j=j, k=k, bank=bank: e.matmul(
                        bank[:, :ntok], win[:, k, j * 128:(j + 1) * 128], hT[:, k, :ntok],
                        start=(k == 0), stop=(k == 7)), R=[win, hT], W=[bank])
            for j in range(20):
                if j in A_PLAIN:
                    bank = pb[2 + (j % 2)]
                    mm_chunk(j, bank)
                    st = stg[nst % 4]
                    nst += 1
                    if j % 2 == 0:
                        P.op("act", lambda e, st=st, bank=bank: e.copy(st[:, :ntok], bank[:, :ntok]), R=[bank], W=[st])
                    else:
                        P.op("dve", lambda e, st=st, bank=bank: e.tensor_copy(st[:, :ntok], bank[:, :ntok]),
                             R=[bank], W=[st])
                    r0 = A_PLAIN[j]
                    P.dma("pool", self.projT[r0:r0 + 128, c0:c0 + ntok], st[:, :ntok], R=[st], W=[self.projT])
                elif j in A_ROPE:
                    j2, r0 = A_ROPE[j]
                    b1, b2 = pb[4 + (j % 2) * 2], pb[5 + (j % 2) * 2]
                    mm_chunk(j, b1)
                    mm_chunk(j2, b2)
                    a1, a2 = r1[j % 2], r2[j % 2]
                    P.op("dve", lambda e, a1=a1, b1=b1: e.tensor_tensor(a1[:, :ntok], b1[:, :ntok], cosb[:, :ntok], ALU.mult),
                         R=[b1, cosb], W=[a1])
                    P.op("dve", lambda e, a2=a2, b2=b2: e.tensor_tensor(a2[:, :ntok], b2[:, :ntok], sinb[:, :ntok], ALU.mult),
                         R=[b2, sinb], W=[a2])
                    st = stg[nst % 4]
                    nst += 1
                    P.op("pool", lambda e, st=st, a1=a1, a2=a2: e.tensor_tensor(st[:, :ntok], a1[:, :ntok], a2[:, :ntok], ALU.add),
                         R=[a1, a2], W=[st])
                    P.dma("pool", self.projT[r0:r0 + 128, c0:c0 + ntok], st[:, :ntok], R=[st], W=[self.projT])
            if TR < 4:
                continue
            for i in range(nt):
                t = t0 + i
                b1, b2 = pb[2 + (i % 2)], pb[4 + (i % 2)]
                for k in range(8):
                    P.op("pe", lambda e, i=i, k=k, b1=b1: e.matmul(
                        b1[:, :], hT[:, k, i * 128:(i + 1) * 128], win[:, k, 2560:3072],
                        start=(k == 0), stop=(k == 7)), R=[win, hT], W=[b1])
                for k in range(8):
                    P.op("pe", lambda e, i=i, k=k, b2=b2: e.matmul(
                        b2[:, :272], hT[:, k, i * 128:(i + 1) * 128], win[:, k, 3072:3344],
                        start=(k == 0), stop=(k == 7)), R=[win, hT], W=[b2])
                v, f, g, a = vst[i % 2], fst[i % 2], gst[i % 2], ast[i % 2]
                P.op("act", lambda e, v=v, b1=b1: e.copy(v[:, :, 0:64], b1[:, 0:256].rearrange("p (h d) -> p h d", d=64)),
                     R=[b1], W=[v])
                P.op("dve", lambda e, f=f, b1=b1: e.tensor_copy(f[:, :, 0:64], b1[:, 256:512].rearrange("p (h d) -> p h d", d=64)),
                     R=[b1], W=[f])
                P.op("act", lambda e, g=g, b2=b2: e.copy(g[:], b2[:, 0:256]), R=[b2], W=[g])
                P.op("dve", lambda e, a=a, b2=b2: e.tensor_copy(a[:], b2[:, 256:272]), R=[b2], W=[a])
                rs = slice(t * 128, (t + 1) * 128)
                if TR < 5:
                    continue
                P.dma("pool", self.navd[rs, :], v[:].rearrange("p h d -> p (h d)"), R=[v], W=[self.navd])
                P.dma("pool", self.dfvd[rs, :], f[:].rearrange("p h d -> p (h d)"), R=[f], W=[self.dfvd])
                P.dma("pool", self.gated[rs, :], g[:], R=[g], W=[self.gated])
                P.dma("pool", self.abd[rs, :], a[:], R=[a], W=[self.abd])
        P.flush()


    def oob_reg(self, e):
        if getattr(self, "_oob", None) is None:
            self._oob = e.to_reg(TRASH - 1)
        return self._oob

    def mm(self, out, lhsT, rhs, start, stop, R, W, **kw):
        bp = lhsT.base_partition()
        kk = lhsT.shape[0]
        rg = frozenset(range(bp // 32, (bp + kk - 1) // 32 + 1))
        self.P.op("pe", lambda e: e.matmul(out, lhsT, rhs, start=start, stop=stop, **kw), R=R, W=W, rg=rg)

    def act(self, out, in_, func, R, W, **kw):
        self.P.op("act", lambda e: e.activation(out, in_, func, **kw), R=R, W=W)

    def tt(self, eng, out, in0, in1, op, R, W):
        self.P.op(eng, lambda e: e.tensor_tensor(out, in0, in1, op), R=R, W=W)

    def ts(self, eng, out, in0, s1, s2, op0, op1, R, W):
        if op1 is None:
            self.P.op(eng, lambda e: e.tensor_scalar(out, in0, s1, s2, op0), R=R, W=W)
        else:
            self.P.op(eng, lambda e: e.tensor_scalar(out, in0, s1, s2, op0, op1), R=R, W=W)

    def stt(self, eng, out, in0, scalar, in1, op0, op1, R, W):
        self.P.op(eng, lambda e: e.scalar_tensor_tensor(out, in0, scalar, in1, op0, op1), R=R, W=W)

    def cp(self, eng, out, in_, R, W):
        if eng == "act":
            self.P.op("act", lambda e: e.copy(out, in_), R=R, W=W)
        else:
            self.P.op(eng, lambda e: e.tensor_copy(out, in_), R=R, W=W)

    def rsqrt_mean(self, out, in_, n, eps, R, W):
        self.act(out, in_, AF.Sqrt, R=R, W=W, bias=eps, scale=1.0 / n)
        self.P.op("dve", lambda e: e.reciprocal(out, out), R=W, W=W)

    def stage_na(self, l, ctx_out):
        P = self.P
        pb = self.pb
        zdst = self.rpbz.t.ap()[0:60 * 8256].rearrange("(a b c) -> a b c", b=64, c=129)[:, :, 0:128]
        zsrc = self.rpbpad.t.ap()[l].rearrange("h j i -> (h j) i").unsqueeze(1).to_broadcast([60, 64, 128])
        P.dma("sp", zdst, zsrc, R=[self.rpbpad], W=[self.rpbz])
        zv = self.rpbz.t.ap()[63:63 + 60 * 8256].rearrange("(a r) -> a r", r=8256)[:, 0:8192] \
            .rearrange("a (k q) -> k a q", q=128)[:, :, 0:64]
        bank = P.sb([128, 60, 64], F32)
        colw = P.sb([128, 64], F32)
        P.dma("sp", colw[:], self.c_colwinT[:], R=[self.c_colwinT], W=[colw])
        P.dma("sp", bank[0:64], zv, R=[self.rpbz], W=[bank])
        P.dma("sp", bank[64:128], zv, R=[self.rpbz], W=[bank])
        self.act(bank[:], bank[:], AF.Exp, R=[bank], W=[bank])
        self.tt("dve", bank[:], bank[:], colw[:].unsqueeze(1).to_broadcast([128, 60, 64]), ALU.mult,
                R=[bank, colw], W=[bank])
        qT = [P.sb([128, T], BF16) for _ in range(2)]
        kT = [P.sb([128, T], BF16) for _ in range(2)]
        for hp in range(2):
            P.dma("sp", qT[hp][:], self.projT[hp * 128:(hp + 1) * 128, :], R=[self.projT], W=[qT[hp]])
            P.dma("sp", kT[hp][:], self.projT[256 + hp * 128:256 + (hp + 1) * 128, :], R=[self.projT], W=[kT[hp]])
        vsb = P.sb([128, NT, 4 * VW], BF16)
        P.dma("sp", vsb[:], self.navd.t.ap().rearrange("(n p) c -> p n c", p=128), R=[self.navd], W=[vsb])
        E = [P.sb([128, 8, 128], F32) for _ in range(3)]
        PT = [P.sb([128, 8, 128], BF16) for _ in range(3)]
        mst = [P.sb([128, 256], BF16) for _ in range(3)]
        rd = [P.sb([128, 1], F32) for _ in range(2)]

        def start_row(r):
            return min(max(r - 4, 0), 56)

        n = 0
        pend_tail = []
        qtiles = ([(-2, 0), (-1, 1)] if ctx_out else []) + [(m, 2 + m) for m in range(32)]
        for (m, qt) in qtiles:
            ms = mst[qt % 3]
            for h in range(4):
                hp, hl = h // 2, h % 2
                psl = slice(hl * 64, hl * 64 + 64)
                if m < 0:
                    chunks = [0, 1]
                else:
                    c_lo = start_row(2 * m) // 2
                    c_hi = (start_row(2 * m + 1) + 7) // 2
                    chunks = [0, 1] + [2 + c for c in range(c_lo, c_hi + 1)]
                nch = len(chunks)
                bA, bB = pb[2 * (n % 3)], pb[2 * (n % 3) + 1]
                Eb, PTb = E[n % 3], PT[n % 3]
                for ci, kt in enumerate(chunks):
                    bk = bA if ci < 4 else bB
                    off = (ci % 4) * 128
                    self.mm(bk[:, off:off + 128], kT[hp][psl, kt * 128:(kt + 1) * 128], qT[hp][psl, qt * 128:(qt + 1) * 128],
                            True, True, R=[kT[hp], qT[hp]], W=[bk])
                self.act(PTb[:, 0:2, :], bA[:, 0:256].rearrange("p (c q) -> p c q", q=128), AF.Exp,
                         R=[bA], W=[PTb], scale=0.125)
                if nch > 2:
                    na = min(nch, 4) - 2
                    self.act(Eb[:, 2:2 + na, :], bA[:, 256:256 + na * 128].rearrange("p (c q) -> p c q", q=128), AF.Exp,
                             R=[bA], W=[Eb], scale=0.125)
                if nch > 4:
                    nb = nch - 4
                    self.act(Eb[:, 4:4 + nb, :], bB[:, 0:nb * 128].rearrange("p (c q) -> p c q", q=128), AF.Exp,
                             R=[bB], W=[Eb], scale=0.125)
                for ci in range(2, nch):
                    c = chunks[ci] - 2
                    for kr in range(2):
                        krow = 2 * c + kr
                        ks = slice(kr * 64, kr * 64 + 64)
                        jj = []
                        for rr in range(2):
                            qrow = 2 * m + rr
                            st = start_row(qrow)
                            if st <= krow < st + 8:
                                jj.append(14 - (krow - qrow + 7))
                            else:
                                jj.append(None)
                        eng = "dve" if (ci + kr) % 2 == 0 else "pool"
                        if jj[0] is not None and jj[1] is not None:
                            assert jj[1] == jj[0] + 1
                            j0 = h * 15 + jj[0]
                            self.tt(eng, PTb[ks, ci, :].rearrange("p (r q) -> p r q", q=64),
                                    Eb[ks, ci, :].rearrange("p (r q) -> p r q", q=64),
                                    bank[ks, j0:j0 + 2, :], ALU.mult, R=[Eb, bank], W=[PTb])
                        else:
                            for rr in range(2):
                                if jj[rr] is None:
                                    P.op(eng, lambda e, ks=ks, ci=ci, rr=rr, PTb=PTb: e.memset(
                                        PTb[ks, ci, rr * 64:(rr + 1) * 64], 0.0), W=[PTb])
                                else:
                                    j0 = h * 15 + jj[rr]
                                    self.tt(eng, PTb[ks, ci, rr * 64:(rr + 1) * 64], Eb[ks, ci, rr * 64:(rr + 1) * 64],
                                            bank[ks, j0, :], ALU.mult, R=[Eb, bank], W=[PTb])
                def tail(n=n, chunks=chunks, nch=nch, PTb=PTb, h=h, ms=ms, qt=qt):
                    ob = pb[6 + (n % 2)]
                    for ci, kt in enumerate(chunks):
                        self.mm(ob[:, 0:65], PTb[:, ci, :], vsb[:, kt, h * VW:h * VW + 65], ci == 0, ci == nch - 1,
                                R=[PTb, vsb], W=[ob])
                    r_ = rd[n % 2]
                    P.op("dve", lambda e, r_=r_, ob=ob: e.reciprocal(r_[:], ob[:, 64:65]), R=[ob], W=[r_])
                    self.ts("dve", ms[:, h * 64:(h + 1) * 64], ob[:, 0:64], r_[:, 0:1], None, ALU.mult, None,
                            R=[ob, r_], W=[ms])
                    if h == 3:
                        P.dma("pool", self.mix[qt * 128:(qt + 1) * 128, 0:256], ms[:], R=[ms], W=[self.mix])
                pend_tail.append(tail)
                if len(pend_tail) > 2:
                    pend_tail.pop(0)()
                n += 1
        while pend_tail:
            pend_tail.pop(0)()
        P.flush()

    def stage_diff(self, l, ctx_out):
        P = self.P
        pb = self.pb
        lam_init = 0.8 - 0.6 * math.exp(-0.3 * l)
        lv = P.sb([1, 128], F32)
        P.dma("sp", lv[:], self.df_lambda.t.ap()[l].unsqueeze(0), R=[self.df_lambda], W=[lv])
        pr = P.sb([1, 64], F32)
        lvv = lv[:].rearrange("p (a b) -> p a b", b=32)
        self.tt("dve", pr[:].rearrange("p (a b) -> p a b", b=32), lvv[:, 0:4:2, :], lvv[:, 1:4:2, :], ALU.mult, R=[lv], W=[pr])
        sm = P.sb([1, 4], F32)
        P.op("dve", lambda e: e.reduce_sum(sm[:, 0:2], pr[:].rearrange("p (a b) -> p a b", b=32), AX.X), R=[pr], W=[sm])
        self.act(sm[:, 0:2], sm[:, 0:2], AF.Exp, R=[sm], W=[sm])
        self.tt("dve", sm[:, 2:3], sm[:, 1:2], sm[:, 0:1], ALU.subtract, R=[sm], W=[sm])
        self.ts("dve", sm[:, 3:4], sm[:, 2:3], -lam_init, None, ALU.add, None, R=[sm], W=[sm])
        self.mm(pb[7][:, 0:1], self.onesf[0:1, :], sm[0:1, 3:4], True, True, R=[self.onesf, sm], W=[pb[7]])
        neglam = P.sb([128, 1], F32)
        self.cp("dve", neglam[:], pb[7][:, 0:1], R=[pb[7]], W=[neglam])
        gdf = P.sb([128, 64], F32)
        P.dma("sp", gdf[:], self.df_norm_g.t.ap()[l].partition_broadcast(128), R=[self.df_norm_g], W=[gdf])
        self.ts("dve", gdf[:], gdf[:], 1.0 - lam_init, None, ALU.mult, None, R=[gdf], W=[gdf])
        kT = [P.sb([128, T], BF16) for _ in range(2)]
        qA = [P.sb([128, T], BF16) for _ in range(2)]
        qB = [P.sb([128, T], BF16) for _ in range(2)]
        for hp in range(2):
            P.dma("sp", kT[hp][:], self.projT[1536 + hp * 128:1536 + (hp + 1) * 128, :], R=[self.projT], W=[kT[hp]])
            P.op("pool", lambda e, hp=hp: e.memset(qA[hp][:], 0.0), W=[qA[hp]])
            P.op("pool", lambda e, hp=hp: e.memset(qB[hp][:], 0.0), W=[qB[hp]])
            for hl in range(2):
                r0 = 1280 + hp * 128 + hl * 64
                P.dma("sp", qA[hp][hl * 64:hl * 64 + 32, :], self.projT[r0:r0 + 32, :], R=[self.projT], W=[qA[hp]])
                P.dma("sp", qB[hp][hl * 64 + 32:hl * 64 + 64, :], self.projT[r0 + 32:r0 + 64, :], R=[self.projT], W=[qB[hp]])
        vsb = P.sb([128, NT, 4 * VW], BF16)
        P.dma("sp", vsb[:], self.dfvd.t.ap().rearrange("(n p) c -> p n c", p=128), R=[self.dfvd], W=[vsb])
        P1 = [P.sb([128, 512], BF16) for _ in range(3)]
        P2 = [P.sb([128, 512], BF16) for _ in range(3)]
        mst = [P.sb([128, 4, 256], BF16) for _ in range(2)]
        rc = P.sb([128, 8], F32)
        av = P.sb([128, 4, 64], F32)
        bv = P.sb([128, 4, 64], F32)
        sq = P.sb([128, 4, 64], F32)
        s4 = P.sb([128, 8], F32)
        oT = P.sb([65, 2, 512], F32)
        scale = 1.0 / math.sqrt(32.0)
        qblocks = ([(0, 256, list(range(2)))] if ctx_out else []) + [(L + i * 512, 512, list(range(NT))) for i in range(8)]
        n = 0
        for bi, (q0, nq, kcs) in enumerate(qblocks):
            nqs = nq // 128
            ms = mst[bi % 2]
            for h in range(4):
                hp, hl = h // 2, h % 2
                psl = slice(hl * 64, hl * 64 + 64)
                o1, o2 = pb[6], pb[7]
                def score(kc):
                    nn_ = n
                    s1, s2 = pb[(2 * nn_) % 6], pb[(2 * nn_ + 1) % 6]
                    p1, p2 = P1[nn_ % 3], P2[nn_ % 3]
                    lhs = kT[hp][psl, kc * 128:(kc + 1) * 128]
                    self.mm(s1[:, :nq], lhs, qA[hp][psl, q0:q0 + nq], True, True, R=[kT[hp], qA[hp]], W=[s1])
                    self.mm(s2[:, :nq], lhs, qB[hp][psl, q0:q0 + nq], True, True, R=[kT[hp], qB[hp]], W=[s2])
                    self.act(p1[:, :nq], s1[:, :nq], AF.Exp, R=[s1], W=[p1], scale=scale)
                    self.act(p2[:, :nq], s2[:, :nq], AF.Exp, R=[s2], W=[p2], scale=scale)
                    return p1, p2

                def pv(ki, kc, p1, p2):
                    vst_ = vsb[:, kc, h * VW:h * VW + 65]
                    self.mm(o1[0:65, :nq], vst_, p1[:, :nq], ki == 0, ki == len(kcs) - 1, R=[p1, vsb], W=[o1])
                    self.mm(o2[0:65, :nq], vst_, p2[:, :nq], ki == 0, ki == len(kcs) - 1, R=[p2, vsb], W=[o2])

                pend = []
                for ki, kc in enumerate(kcs):
                    pp = score(kc)
                    n += 1
                    pend.append((ki, kc, pp[0], pp[1]))
                    if len(pend) > 2:
                        pv(*pend.pop(0))
                while pend:
                    pv(*pend.pop(0))
                self.cp("act", oT[:, 0, :nq], o1[0:65, :nq], R=[o1], W=[oT])
                self.cp("dve", oT[:, 1, :nq], o2[0:65, :nq], R=[o2], W=[oT])
                if self.dbg and bi == 0 and h == 0:
                    dd1 = self.dscr(f"dbg_oT{l}", [65, 1024], F32)
                    P.dma("sp", dd1[:], oT[:].rearrange("p a b -> p (a b)"), R=[oT], W=[dd1])
                t1b, t2b = pb[0], pb[1]
                for mi, tb_ in ((0, t1b), (1, t2b)):
                    for qs in range(nqs):
                        P.op("pe", lambda e, mi=mi, tb_=tb_, qs=qs: e.transpose(
                            tb_[:, qs * 65:(qs + 1) * 65], oT[:, mi, qs * 128:(qs + 1) * 128], self.identf[0:65, 0:65]),
                            R=[oT, self.identf], W=[tb_])
                if self.dbg and bi == 0 and h == 0:
                    dd2 = self.dscr(f"dbg_t1b{l}", [128, 512], F32)
                    dtmp = P.sb([128, 512], F32)
                    self.cp("dve", dtmp[:], t1b[:, :], R=[t1b], W=[dtmp])
                    P.dma("sp", dd2[:], dtmp[:], R=[dtmp], W=[dd2])
                o1v = t1b[:, 0:260].rearrange("p (a b) -> p a b", b=65)[:, 0:nqs, :]
                o2v = t2b[:, 0:260].rearrange("p (a b) -> p a b", b=65)[:, 0:nqs, :]
                o1, o2 = t1b, t2b
                P.op("dve", lambda e, o1v=o1v, nqs=nqs: e.reciprocal(rc[:, 0:nqs], o1v[:, :, 64]), R=[o1], W=[rc])
                P.op("dve", lambda e, o2v=o2v, nqs=nqs: e.reciprocal(rc[:, 4:4 + nqs], o2v[:, :, 64]), R=[o2], W=[rc])
                self.tt("dve", av[:, 0:nqs, :], o1v[:, :, 0:64], rc[:, 0:nqs].unsqueeze(2).to_broadcast([128, nqs, 64]),
                        ALU.mult, R=[o1, rc], W=[av])
                self.tt("dve", bv[:, 0:nqs, :], o2v[:, :, 0:64], rc[:, 4:4 + nqs].unsqueeze(2).to_broadcast([128, nqs, 64]),
                        ALU.mult, R=[o2, rc], W=[bv])
                self.stt("dve", av[:, 0:nqs, :], bv[:, 0:nqs, :], neglam[:, 0:1], av[:, 0:nqs, :], ALU.mult, ALU.add,
                         R=[bv, av, neglam], W=[av])
                self.tt("pool", sq[:, 0:nqs, :], av[:, 0:nqs, :], av[:, 0:nqs, :], ALU.mult, R=[av], W=[sq])
                P.op("dve", lambda e, nqs=nqs: e.reduce_sum(s4[:, 0:nqs], sq[:, 0:nqs, :], AX.X), R=[sq], W=[s4])
                self.rsqrt_mean(s4[:, 4:4 + nqs], s4[:, 0:nqs], 64.0, 1e-6, R=[s4], W=[s4])
                self.tt("dve", av[:, 0:nqs, :], av[:, 0:nqs, :], s4[:, 4:4 + nqs].unsqueeze(2).to_broadcast([128, nqs, 64]),
                        ALU.mult, R=[av, s4], W=[av])
                self.tt("dve", ms[:, 0:nqs, h * 64:(h + 1) * 64], av[:, 0:nqs, :],
                        gdf[:].unsqueeze(1).to_broadcast([128, nqs, 64]), ALU.mult, R=[av, gdf], W=[ms])
            P.dma("pool", self.mix[q0:q0 + nq, 512:768].rearrange("(a p) c -> p a c", p=128), ms[:, 0:nqs, :],
                  R=[ms], W=[self.mix])
        P.flush()

    def stage_fft(self, l, ctx_out):
        P = self.P
        pb = self.pb
        ftw = P.sb([128, 2, 256], BF16)
        P.dma("pool", ftw[:], self.ft_w.t.ap()[l].rearrange("(c p) n -> p c n", p=128), R=[self.ft_w], W=[ftw])
        bdc = P.sb([128, 128], BF16)
        bds = P.sb([128, 128], BF16)
        P.dma("pool", bdc[:], self.c_bdc[:], R=[self.c_bdc], W=[bdc])
        P.dma("pool", bds[:], self.c_bds[:], R=[self.c_bds], W=[bds])
        M12 = P.sb([128, 2, 512], BF16)
        for j in range(2):
            self.mm(pb[0][:, 0:256], bdc[:], ftw[:, j, :], True, True, R=[bdc, ftw], W=[pb[0]])
            self.mm(pb[1][:, 0:256], bds[:], ftw[:, j, :], True, True, R=[bds, ftw], W=[pb[1]])
            self.cp("act", M12[:, j, 0:256], pb[0][:, 0:256], R=[pb[0]], W=[M12])
            P.op("act", lambda e, j=j: e.mul(M12[:, j, 256:512], pb[1][:, 0:256], -1.0), R=[pb[1]], W=[M12])
        uT = P.sb([128, 2, T], BF16)
        P.dma("sp", uT[:], self.projT[1792:2048, :].rearrange("(c p) t -> p c t", p=128), R=[self.projT], W=[uT])
        uM = P.sb([128, NT, 512], BF16)
        for t in range(NT):
            bk = pb[t % 2]
            for c in range(2):
                self.mm(bk[:, :], uT[:, c, t * 128:(t + 1) * 128], M12[:, c, :], c == 0, c == 1, R=[uT, M12], W=[bk])
            self.cp("act" if t % 2 == 0 else "dve", uM[:, t, :], bk[:, :], R=[bk], W=[uM])
        ct = [P.sb([128, 32, 128], BF16) for _ in range(2)]
        st = [P.sb([128, 32, 128], BF16) for _ in range(2)]
        og = [P.sb([128, 256], BF16) for _ in range(2)]
        jobs = ([("c", ti) for ti in range(2)] if ctx_out else []) + [("l", ti) for ti in range(32)]
        for n, (kind, ti) in enumerate(jobs):
            c_, s_ = ct[n % 2], st[n % 2]
            if kind == "l":
                ntc, tb = 32, 2
                P.dma("sp", c_[:], self.c_dftc[ti], R=[self.c_dftc], W=[c_])
                P.dma("sp", s_[:], self.c_dfts[ti], R=[self.c_dfts], W=[s_])
            else:
                ntc, tb = 2, 0
                P.dma("sp", c_[:, 0:2, :], self.c_dftc_c[ti], R=[self.c_dftc_c], W=[c_])
                P.dma("sp", s_[:, 0:2, :], self.c_dfts_c[ti], R=[self.c_dfts_c], W=[s_])
            bk = pb[2 + n % 2]
            for tc in range(ntc):
                self.mm(bk[:, 0:256], c_[:, tc, :], uM[:, tb + tc, 0:256], tc == 0, False, R=[c_, uM], W=[bk])
                self.mm(bk[:, 0:256], s_[:, tc, :], uM[:, tb + tc, 256:512], False, tc == ntc - 1, R=[s_, uM], W=[bk])
            o_ = og[n % 2]
            self.cp("act" if n % 2 == 0 else "dve", o_[:], bk[:, 0:256], R=[bk], W=[o_])
            t = tb + ti
            P.dma("pool", self.mix[t * 128:(t + 1) * 128, 768:1024], o_[:], R=[o_], W=[self.mix])
        P.flush()

    def stage_outproj(self, l, ctx_out):
        P = self.P
        pb = self.pb
        wout = P.sb([128, 8, D], BF16)
        for k in range(8):
            P.dma("pool", wout[:, k, :], self.w_out.t.ap()[l, k * 128:(k + 1) * 128, :], R=[self.w_out], W=[wout])
        wr = P.sb([128, 8, NE], BF16)
        P.dma("pool", wr[:], self.w_router.t.ap()[l].rearrange("(k p) n -> p k n", p=128), R=[self.w_router], W=[wr])
        gs2T = P.sb([128, 8, 2], F32)
        self.ts("dve", gs2T[:], self.mT[:, 32:40, :], 1.0, None, ALU.add, None, R=[self.mT], W=[gs2T])
        self.tt("dve", gs2T[:], gs2T[:], self.g2T[:].unsqueeze(2).to_broadcast([128, 8, 2]), ALU.mult,
                R=[gs2T, self.g2T], W=[gs2T])
        streams = [0, 1] if ctx_out else [0]
        m2b, gs2b, sh2b = {}, {}, {}
        for s_ in streams:
            m2b[s_] = P.sb([128, D], F32)
            gs2b[s_] = P.sb([128, D], F32)
            sh2b[s_] = P.sb([128, D], F32)
            self.bcast_vec(m2b[s_], lambda c, s_=s_: self.mT[:, 16 + c, s_:s_ + 1], [self.mT])
            self.bcast_vec(gs2b[s_], lambda c, s_=s_: gs2T[:, c, s_:s_ + 1], [gs2T])
            self.bcast_vec(sh2b[s_], lambda c, s_=s_: self.mT[:, 24 + c, s_:s_ + 1], [self.mT])
        mx = [P.sb([128, D], BF16) for _ in range(2)]
        mxT = [P.sb([128, 8, 128], BF16) for _ in range(2)]
        xt = [P.sb([128, D], F32) for _ in range(2)]
        tmp = [P.sb([128, D], F32) for _ in range(2)]
        xn = [P.sb([128, D], F32) for _ in range(2)]
        junk = P.sb([128, D], BF16)
        ss = [P.sb([128, 2], F32) for _ in range(2)]
        h2 = [P.sb([128, D], BF16) for _ in range(2)]
        h2T = [P.sb([128, 8, 128], BF16) for _ in range(2)]
        sm = [P.sb([128, 4], F32) for _ in range(2)]
        ex = [P.sb([128, NE], F32) for _ in range(2)]
        tiles = list(range(0 if ctx_out else 2, NT))
        pendB = []
        for n, t in enumerate(tiles):
            s_ = 1 if t < 2 else 0
            i = n % 2
            rs = slice(t * 128, (t + 1) * 128)
            P.dma("sp", mx[i][:], self.mix[rs, :], R=[self.mix], W=[mx[i]])
            P.dma("sp", xt[i][:], self.xres[rs, :], R=[self.xres], W=[xt[i]])
            bT = pb[0 + i]
            pT = bT[:].bitcast(BF16)
            for c in range(8):
                P.op("pe", lambda e, c=c, pT=pT, i=i: e.transpose(pT[:, c * 128:(c + 1) * 128], mx[i][:, c * 128:(c + 1) * 128],
                                                                   self.ident[:]), R=[mx[i], self.ident], W=[bT])
            self.cp("act", mxT[i][:].rearrange("p c t -> p (c t)"), pT[:, :], R=[bT], W=[mxT[i]])
            for nn in range(2):
                bk = pb[2 + nn]
                for k in range(8):
                    self.mm(bk[:, :], mxT[i][:, k, :], wout[:, k, nn * 512:(nn + 1) * 512], k == 0, k == 7,
                            R=[mxT[i], wout], W=[bk])
                self.tt("dve", tmp[i][:, nn * 512:(nn + 1) * 512], bk[:, :], m2b[s_][:, nn * 512:(nn + 1) * 512], ALU.mult,
                        R=[bk, m2b[s_]], W=[tmp[i]])
            self.tt("pool", xn[i][:], xt[i][:], tmp[i][:], ALU.add, R=[xt[i], tmp[i]], W=[xn[i]])
            P.dma("pool", self.xres[rs, :], xn[i][:], R=[xn[i]], W=[self.xres])
            def partB(i=i, s_=s_, t=t, rs=rs):
                self._outproj_B(i, s_, t, rs, ss, junk, xn, tmp, gs2b, sh2b, h2, h2T, wr, sm, ex)
            pendB.append(partB)
            if len(pendB) > 1:
                pendB.pop(0)()
        while pendB:
            pendB.pop(0)()
        if self.dbg:
            da = self.dscr(f"dbg_aff{l}", [128, NT * NE], F32)
            P.dma("sp", da[:], self.aff[:].rearrange("p t e -> p (t e)"), R=[self.aff], W=[da])
        P.flush()

    def _outproj_B(self, i, s_, t, rs, ss, junk, xn, tmp, gs2b, sh2b, h2, h2T, wr, sm, ex):
        P = self.P
        pb = self.pb
        if True:
            P.op("dve", lambda e, i=i: e.memset(ss[i][:], 0.0), W=[ss[i]])
            self.act(junk[:], xn[i][:], AF.Square, R=[xn[i], ss[i]], W=[junk, ss[i]], accum_out=ss[i][:, 0:1])
            self.rsqrt_mean(ss[i][:, 1:2], ss[i][:, 0:1], float(D), 1e-6, R=[ss[i]], W=[ss[i]])
            self.stt("dve", tmp[i][:], xn[i][:], ss[i][:, 1:2], gs2b[s_][:], ALU.mult, ALU.mult,
                     R=[xn[i], ss[i], gs2b[s_]], W=[tmp[i]])
            self.tt("pool", h2[i][:], tmp[i][:], sh2b[s_][:], ALU.add, R=[tmp[i], sh2b[s_]], W=[h2[i]])
            P.dma("pool", self.h2d[rs, :], h2[i][:], R=[h2[i]], W=[self.h2d])
            bT2 = pb[4 + i]
            pT2 = bT2[:].bitcast(BF16)
            for c in range(8):
                P.op("pe", lambda e, c=c, pT2=pT2, i=i: e.transpose(pT2[:, c * 128:(c + 1) * 128], h2[i][:, c * 128:(c + 1) * 128],
                                                                     self.ident[:]), R=[h2[i], self.ident], W=[bT2])
            self.cp("act", h2T[i][:].rearrange("p c t -> p (c t)"), pT2[:, :], R=[bT2], W=[h2T[i]])
            bR = pb[6 + i]
            for k in range(8):
                self.mm(bR[:, 0:NE], h2T[i][:, k, :], wr[:, k, :], k == 0, k == 7, R=[h2T[i], wr], W=[bR])
            P.op("dve", lambda e, i=i, bR=bR: e.reduce_max(sm[i][:, 0:1], bR[:, 0:NE], AX.X), R=[bR], W=[sm[i]])
            self.ts("dve", sm[i][:, 1:2], sm[i][:, 0:1], -1.0, None, ALU.mult, None, R=[sm[i]], W=[sm[i]])
            P.op("dve", lambda e, i=i: e.memset(sm[i][:, 2:3], 0.0), W=[sm[i]])
            self.act(ex[i][:], bR[:, 0:NE], AF.Exp, R=[bR, sm[i]], W=[ex[i], sm[i]], bias=sm[i][:, 1:2], accum_out=sm[i][:, 2:3])
            P.op("dve", lambda e, i=i: e.reciprocal(sm[i][:, 3:4], sm[i][:, 2:3]), R=[sm[i]], W=[sm[i]])
            self.ts("dve", self.aff[:, t, :], ex[i][:], sm[i][:, 3:4], None, ALU.mult, None, R=[ex[i], sm[i]], W=[self.aff])

    def stage_route(self, l, ctx_out):
        P = self.P
        pb = self.pb
        sbase = P.sb([128, NT, NE], F32)
        P.dma("sp", sbase[:].rearrange("p t e -> p (t e)"), self.c_slotbase[:], R=[self.c_slotbase], W=[sbase])
        streams = [(2, 32, CAP_L)] + ([(0, 2, CAP_C)] if ctx_out else [])
        cmpb = P.sb([128, 32, NE], F32)
        lo = P.sb([128, NE], F32)
        mid = P.sb([128, NE], F32)
        cnt = P.sb([128, NE], F32)
        ge = P.sb([128, NE], F32)
        tot = P.sb([128, 32, NE], F32)
        off = P.sb([128, 32, NE], F32)
        pos = P.sb([128, 32, NE], F32)
        m2 = P.sb([128, 32, NE], F32)
        dstf = P.sb([128, 32, NE], F32)
        for (t0, nt, cap) in streams:
            affv = self.aff[:, t0:t0 + nt, :]
            cv = cmpb[:, 0:nt, :]
            P.op("dve", lambda e: e.memset(lo[:], 0.0), W=[lo])
            for it in range(32):
                hstep = 2.0 ** (-(it + 1))
                self.ts("dve", mid[:], lo[:], hstep, None, ALU.add, None, R=[lo], W=[mid])
                self.tt("dve", cv, affv, mid[:].unsqueeze(1).to_broadcast([128, nt, NE]), ALU.is_gt,
                        R=[self.aff, mid], W=[cmpb])
                P.op("dve", lambda e, cv=cv: e.reduce_sum(cnt[:], cv.rearrange("p t e -> p e t"), AX.X), R=[cmpb], W=[cnt])
                self.mm(pb[0][:, 0:NE], self.onesf[:], cnt[:], True, True, R=[self.onesf, cnt], W=[pb[0]])
                self.ts("dve", ge[:], pb[0][:, 0:NE], cap - 0.5, None, ALU.is_ge, None, R=[pb[0]], W=[ge])
                self.stt("dve", lo[:], ge[:], hstep, lo[:], ALU.mult, ALU.add, R=[ge, lo], W=[lo])
            self.tt("dve", cv, affv, lo[:].unsqueeze(1).to_broadcast([128, nt, NE]), ALU.is_gt, R=[self.aff, lo], W=[cmpb])
            cvf = cv.rearrange("p t e -> p (t e)")
            self.mm(pb[1][:, 0:nt * NE], self.trif[:], cvf, True, True, R=[self.trif, cmpb], W=[pb[1]])
            self.mm(pb[2][:, 0:nt * NE], self.onesf[:], cvf, True, True, R=[self.onesf, cmpb], W=[pb[2]])
            self.cp("act", tot[:, 0:nt, :].rearrange("p t e -> p (t e)"), pb[2][:, 0:nt * NE], R=[pb[2]], W=[tot])
            P.op("dve", lambda e: e.memset(off[:, 0, :], 0.0), W=[off])
            for j in range(1, nt):
                self.tt("dve", off[:, j, :], off[:, j - 1, :], tot[:, j - 1, :], ALU.add, R=[off, tot], W=[off])
            self.tt("dve", pos[:, 0:nt, :].rearrange("p t e -> p (t e)"), pb[1][:, 0:nt * NE],
                    off[:, 0:nt, :].rearrange("p t e -> p (t e)"), ALU.add, R=[pb[1], off], W=[pos])
            self.ts("dve", m2[:, 0:nt, :], pos[:, 0:nt, :], cap + 0.5, None, ALU.is_lt, None, R=[pos], W=[m2])
            self.tt("dve", m2[:, 0:nt, :], m2[:, 0:nt, :], cv, ALU.mult, R=[m2, cmpb], W=[m2])
            self.stt("dve", dstf[:, 0:nt, :], pos[:, 0:nt, :], -1.0, sbase[:, t0:t0 + nt, :], ALU.add, ALU.add,
                     R=[pos, sbase], W=[dstf])
            self.tt("dve", dstf[:, 0:nt, :], dstf[:, 0:nt, :], m2[:, 0:nt, :], ALU.mult, R=[dstf, m2], W=[dstf])
            self.ts("dve", dstf[:, 0:nt, :], dstf[:, 0:nt, :], float(OOB), None, ALU.add, None, R=[dstf], W=[dstf])
            self.cp("dve", self.desti[:, t0:t0 + nt, :], dstf[:, 0:nt, :], R=[dstf], W=[self.desti])
            self.tt("dve", self.gatev[:, t0:t0 + nt, :], affv, m2[:, 0:nt, :], ALU.mult, R=[self.aff, m2], W=[self.gatev])
        if self.dbg:
            dd = self.dscr(f"dbg_dest{l}", [128, NT * NE], I32)
            P.dma("sp", dd[:], self.desti[:].rearrange("p t e -> p (t e)"), R=[self.desti], W=[dd])
        ht = [P.sb([128, D], BF16) for _ in range(3)]
        tiles = list(range(0 if ctx_out else 2, NT))
        for n, t in enumerate(tiles):
            hb = ht[n % 3]
            P.dma("sp", hb[:], self.h2d[t * 128:(t + 1) * 128, :], R=[self.h2d], W=[hb])
            for e_ in range(NE):
                P.op("pool", lambda e, t=t, e_=e_, hb=hb: e.indirect_dma_start(
                    out=self.xg[:, :], out_offset=bass.IndirectOffsetOnAxis(ap=self.desti[:, t, e_:e_ + 1], axis=0),
                    in_=hb[:, :], in_offset=None, bounds_check=self.oob_reg(e), oob_is_err=False),
                    R=[hb, self.desti], W=[], kind="dma")
        P.flush()

    def stage_ffn(self, l, ctx_out):
        P = self.P
        pb = self.pb
        NW = 6
        wbuf = [P.sb([128, 8 * 1024], BF16) for _ in range(NW)]
        nw = [0]

        def load_w(kind, e_, part):
            w = wbuf[nw[0] % NW]
            nw[0] += 1
            if kind == "g":
                src = self.w_gate.t.ap()[l, e_, :, part * 1024:(part + 1) * 1024].rearrange("(k p) n -> p k n", p=128)
                P.dma("pool", w[:].rearrange("p (k n) -> p k n", n=1024), src, R=[self.w_gate], W=[w])
            elif kind == "u":
                src = self.w_up.t.ap()[l, e_, :, part * 1024:(part + 1) * 1024].rearrange("(k p) n -> p k n", p=128)
                P.dma("pool", w[:].rearrange("p (k n) -> p k n", n=1024), src, R=[self.w_up], W=[w])
            else:
                src = self.w_down.t.ap()[l, e_, :, part * 512:(part + 1) * 512].rearrange("(k p) n -> p k n", p=128)
                P.dma("pool", w[:].rearrange("p (k n) -> p k n", n=512), src, R=[self.w_down], W=[w])
            return w

        nsl = ESL if ctx_out else CAP_L
        stiles = [(0, 128), (128, 128), (256, 128), (384, 128)] + ([(512, 32)] if ctx_out else [])
        xr = [P.sb([128, D], BF16) for _ in range(3)]
        xgT = [P.sb([128, 8, ESL], BF16) for _ in range(2)]
        hidT = P.sb([128, 16, ESL], BF16)
        sg = [P.sb([128, 512], F32) for _ in range(2)]
        sgc = P.sb([128, 32], F32)
        yst = [P.sb([128, 512], F32) for _ in range(3)]
        nx = 0
        ny = 0
        for e_ in range(NE):
            xT = xgT[e_ % 2]
            for si, (s0, rows) in enumerate(stiles):
                xb = xr[nx % 3]
                nx += 1
                r0 = e_ * ESL + s0
                P.dma("sp", xb[:rows, :], self.xg[r0:r0 + rows, :], R=[self.xg], W=[xb])
                bT = pb[6 + (si % 2)]
                pT = bT[:].bitcast(BF16)
                for c in range(8):
                    P.op("pe", lambda e, c=c, pT=pT, xb=xb, rows=rows: e.transpose(
                        pT[:, c * 128:c * 128 + rows], xb[:rows, c * 128:(c + 1) * 128], self.ident[:rows, :rows]),
                        R=[xb, self.ident], W=[bT])
                self.cp("act" if si % 2 == 0 else "dve", xT[:, :, s0:s0 + rows],
                        pT[:, :].rearrange("p (c t) -> p c t", t=128)[:, :, 0:rows], R=[bT], W=[xT])
            for fh in range(2):
                wg = load_w("g", e_, fh)
                wu = load_w("u", e_, fh)
                wgv = wg[:].rearrange("p (k n) -> p k n", n=1024)
                wuv = wu[:].rearrange("p (k n) -> p k n", n=1024)
                for fc in range(8):
                    fcg = fh * 8 + fc
                    gb, ub, cb = pb[0 + (fcg % 2)], pb[2 + (fcg % 2)], pb[4 + (fcg % 2)]
                    for k in range(8):
                        lw = wgv[:, k, fc * 128:(fc + 1) * 128]
                        self.mm(gb[:, :], lw, xT[:, k, 0:512], k == 0, k == 7, R=[wg, xT], W=[gb])
                        if ctx_out:
                            self.mm(cb[:, 0:32], lw, xT[:, k, 512:544], k == 0, k == 7, R=[wg, xT], W=[cb])
                    for k in range(8):
                        lw = wuv[:, k, fc * 128:(fc + 1) * 128]
                        self.mm(ub[:, :], lw, xT[:, k, 0:512], k == 0, k == 7, R=[wu, xT], W=[ub])
                        if ctx_out:
                            self.mm(cb[:, 32:64], lw, xT[:, k, 512:544], k == 0, k == 7, R=[wu, xT], W=[cb])
                    sgb = sg[fcg % 2]
                    self.act(sgb[:], gb[:, :], AF.Silu, R=[gb], W=[sgb])
                    self.tt("dve", hidT[:, fcg, 0:512], sgb[:], ub[:, :], ALU.mult, R=[sgb, ub], W=[hidT])
                    if ctx_out:
                        self.act(sgc[:], cb[:, 0:32], AF.Silu, R=[cb], W=[sgc])
                        self.tt("dve", hidT[:, fcg, 512:544], sgc[:], cb[:, 32:64], ALU.mult, R=[sgc, cb], W=[hidT])
            for nn in range(2):
                wd = load_w("d", e_, nn)
                wdv = wd[:].rearrange("p (k n) -> p k n", n=512)
                for si, (s0, rows) in enumerate(stiles):
                    yb = pb[6 + (si % 2)]
                    for fc in range(16):
                        self.mm(yb[:rows, :], hidT[:, fc, s0:s0 + rows], wdv[:, fc, :], fc == 0, fc == 15, R=[hidT, wd], W=[yb])
                    ys = yst[ny % 3]
                    ny += 1
                    self.cp("act" if si % 2 == 0 else "dve", ys[:rows, :], yb[:rows, :], R=[yb], W=[ys])
                    r0 = e_ * ESL + s0
                    P.dma("sp", self.ybuf[r0:r0 + rows, nn * 512:(nn + 1) * 512], ys[:rows, :], R=[ys], W=[])
        P.flush()

    def stage_combine(self, l, ctx_out, final):
        P = self.P
        streams = [0, 1] if ctx_out else [0]
        m5b = {}
        for s_ in streams:
            m5b[s_] = P.sb([128, D], F32)
            self.bcast_vec(m5b[s_], lambda c, s_=s_: self.mT[:, 40 + c, s_:s_ + 1], [self.mT])
        if final:
            gfT = P.sb([128, 8], F32)
            P.dma("sp", gfT[:], self.final_g.t.ap().rearrange("(c p) -> p c", p=128), R=[self.final_g], W=[gfT],
                  allow_slow_non_contiguous=True)
            gfb = P.sb([128, D], F32)
            self.bcast_vec(gfb, lambda c: gfT[:, c:c + 1], [gfT])
            junk = P.sb([128, D], BF16)
        G = [P.sb([128, D], F32) for _ in range(NE)]
        for e_ in range(NE):
            P.op("dve" if e_ % 2 == 0 else "pool", lambda e, e_=e_: e.memset(G[e_][:], 0.0), W=[G[e_]])
        xt = [P.sb([128, D], F32) for _ in range(2)]
        acc = [P.sb([128, D], F32) for _ in range(2)]
        ss = [P.sb([128, 2], F32) for _ in range(2)]
        tiles = list(range(0 if ctx_out else 2, NT))
        for n, t in enumerate(tiles):
            s_ = 1 if t < 2 else 0
            i = n % 2
            rs = slice(t * 128, (t + 1) * 128)
            P.dma("sp", xt[i][:], self.xres[rs, :], R=[self.xres], W=[xt[i]])
            for e_ in range(NE):
                P.op("pool", lambda e, t=t, e_=e_: e.indirect_dma_start(
                    out=G[e_][:, :], out_offset=None, in_=self.ybuf[:, :],
                    in_offset=bass.IndirectOffsetOnAxis(ap=self.desti[:, t, e_:e_ + 1], axis=0),
                    bounds_check=self.oob_reg(e), oob_is_err=False), R=[self.ybuf, self.desti], W=[G[e_]], kind="dma")
            a = acc[i]
            self.ts("dve", a[:], G[0][:], self.gatev[:, t, 0:1], None, ALU.mult, None, R=[G[0], self.gatev], W=[a])
            for e_ in range(1, NE):
                self.stt("dve", a[:], G[e_][:], self.gatev[:, t, e_:e_ + 1], a[:], ALU.mult, ALU.add,
                         R=[G[e_], self.gatev, a], W=[a])
            self.tt("dve", a[:], a[:], m5b[s_][:], ALU.mult, R=[a, m5b[s_]], W=[a])
            self.tt("dve", a[:], a[:], xt[i][:], ALU.add, R=[a, xt[i]], W=[a])
            if not final or t < 2:
                P.dma("sp", self.xres[rs, :], a[:], R=[a], W=[self.xres])
            else:
                if self.dbg:
                    P.dma("sp", self.xres[rs, :], a[:], R=[a], W=[self.xres])
                P.op("dve", lambda e, i=i: e.memset(ss[i][:], 0.0), W=[ss[i]])
                self.act(junk[:], a[:], AF.Square, R=[a, ss[i]], W=[junk, ss[i]], accum_out=ss[i][:, 0:1])
                self.rsqrt_mean(ss[i][:, 1:2], ss[i][:, 0:1], float(D), 1e-6, R=[ss[i]], W=[ss[i]])
                self.stt("dve", a[:], a[:], ss[i][:, 1:2], gfb[:], ALU.mult, ALU.mult, R=[a, ss[i], gfb], W=[a])
                P.dma("sp", self.out[(t - 2) * 128:(t - 1) * 128, :], a[:], R=[a], W=[self.out])
        P.flush()


    def _gdn_prep(self, l, qT, kT, qtok, ktok, vtok, gg, beta, nbeta, obb):
        P = self.P
        pb = self.pb
        identb_f = self.identf
        ab = P.sb([128, NT, 16], F32)
        P.dma("sp", ab[:], self.abd.t.ap().rearrange("(n p) c -> p n c", p=128), R=[self.abd], W=[ab])
        dtb = P.sb([128, 8], F32)
        nA = P.sb([128, 8], F32)
        P.dma("sp", dtb[:], self.dn_dt_bias.t.ap()[l].partition_broadcast(128), R=[self.dn_dt_bias], W=[dtb])
        P.dma("sp", nA[:], self.dn_a_log.t.ap()[l].partition_broadcast(128), R=[self.dn_a_log], W=[nA])
        self.act(nA[:], nA[:], AF.Exp, R=[nA], W=[nA])
        self.ts("dve", nA[:], nA[:], -1.0, None, ALU.mult, None, R=[nA], W=[nA])
        self.tt("dve", gg[:], ab[:, :, 0:8], dtb[:].unsqueeze(1).to_broadcast([128, NT, 8]), ALU.add, R=[ab, dtb], W=[gg])
        self.act(gg[:], gg[:], AF.Exp, R=[gg], W=[gg])
        self.act(gg[:], gg[:], AF.Ln, R=[gg], W=[gg], bias=1.0)
        self.tt("dve", gg[:], gg[:], nA[:].unsqueeze(1).to_broadcast([128, NT, 8]), ALU.mult, R=[gg, nA], W=[gg])
        self.act(beta[:], ab[:, :, 8:16], AF.Sigmoid, R=[ab], W=[beta])
        self.ts("dve", nbeta[:], beta[:], -1.0, None, ALU.mult, None, R=[beta], W=[nbeta])
        segs = [(0, L), (L, T)]
        xs = [P.sb([128, T], BF16) for _ in range(2)]
        sil = P.sb([128, T], BF16)
        sqv = P.sb([128, 512], BF16)
        rn = P.sb([128, 512], F32)
        cw = [P.sb([128, 5], F32) for _ in range(2)]
        dg = [P.sb([128, 5, 128], BF16) for _ in range(2)]
        blocks = [(0, L, 0, L)] + [(L + 512 * j, 512, L, T) for j in range(8)]
        nblk = 0
        for cc in range(6):
            x_ = xs[cc % 2]
            w_ = cw[cc % 2]
            dg_ = dg[cc % 2]
            P.dma("sp", x_[:], self.projT[512 + cc * 128:512 + (cc + 1) * 128, :], R=[self.projT], W=[x_])
            P.dma("sp", w_[:], self.dn_conv_w.t.ap()[l, :, cc * 128:(cc + 1) * 128].rearrange("k c -> c k"),
                  R=[self.dn_conv_w], W=[w_], allow_slow_non_contiguous=True)
            for k in range(5):
                self.ts("dve", dg_[:, k, :], self.identf[:], w_[:, k:k + 1], None, ALU.mult, None, R=[self.identf, w_], W=[dg_])
            for (b0, nb, s0, s1) in blocks:
                bk = pb[4 + (nblk % 2)]
                nblk += 1
                self.mm(bk[:, :nb], dg_[:, 2, :], x_[:, b0:b0 + nb], True, False, R=[dg_, x_], W=[bk])
                taps = (0, 1, 3, 4)
                for ti_, k in enumerate(taps):
                    sft = k - 2
                    a_ = max(b0, s0 - sft)
                    b_ = min(b0 + nb, s1 - sft)
                    self.mm(bk[:, a_ - b0:b_ - b0], dg_[:, k, :], x_[:, a_ + sft:b_ + sft], False, ti_ == len(taps) - 1,
                            R=[dg_, x_], W=[bk])
                self.act(sil[:, b0:b0 + nb], bk[:, :nb], AF.Silu, R=[bk], W=[sil])
            if cc < 4:
                dst = qT[cc] if cc < 2 else kT[cc - 2]
                scl = 0.125 if cc < 2 else 1.0
                for b0 in range(0, T, 512):
                    nb = min(512, T - b0)
                    bk = pb[(b0 // 512) % 2]
                    self.tt("pool", sqv[:, :nb], sil[:, b0:b0 + nb], sil[:, b0:b0 + nb], ALU.mult, R=[sil], W=[sqv])
                    self.mm(bk[:, :nb], obb[:], sqv[:, :nb], True, True, R=[obb, sqv], W=[bk])
                    self.act(rn[:, :nb], bk[:, :nb], AF.Sqrt, R=[bk], W=[rn], bias=1e-6, scale=1.0)
                    P.op("dve", lambda e, nb=nb: e.reciprocal(rn[:, :nb], rn[:, :nb]), R=[rn], W=[rn])
                    self.stt("dve", dst[:, b0:b0 + nb], sil[:, b0:b0 + nb], scl, rn[:, :nb], ALU.mult, ALU.mult,
                             R=[sil, rn], W=[dst])
                srcT = dst
            else:
                srcT = sil
            tok = qtok if cc < 2 else (ktok if cc < 4 else vtok)
            hp = cc % 2
            for t in range(NT):
                bT = pb[2 + (t % 2)]
                pT = bT[:].bitcast(BF16)
                P.op("pe", lambda e, t=t, pT=pT, srcT=srcT: e.transpose(pT[:, 0:128], srcT[:, t * 128:(t + 1) * 128], self.ident[:]),
                     R=[srcT, self.ident], W=[bT])
                self.cp("act" if t % 2 == 0 else "dve", tok[:, t, hp * 128:(hp + 1) * 128], pT[:, 0:128], R=[bT], W=[tok])

    def stage_gdn(self, l, ctx_out):
        P = self.P
        pb = self.pb
        qT = [P.sb([128, T], BF16, persist="mid") for _ in range(2)]
        kT = [P.sb([128, T], BF16, persist="mid") for _ in range(2)]
        qtok = P.sb([128, NT, 256], BF16, persist="mid")
        ktok = P.sb([128, NT, 256], BF16, persist="mid")
        vtok = P.sb([128, NT, 256], BF16, persist="mid")
        gg = P.sb([128, NT, 8], F32, persist="mid")
        beta = P.sb([128, NT, 8], F32, persist="mid")
        nbeta = P.sb([128, NT, 8], F32, persist="mid")
        obb = P.sb([128, 128], BF16)
        P.dma("pool", obb[:], self.c_gob[:], R=[self.c_gob], W=[obb])
        self._gdn_prep(l, qT, kT, qtok, ktok, vtok, gg, beta, nbeta, obb)
        P.flush()
        oacc = P.sb([128, NT, 256], F32)
        cst = {}
        for nm, src, shp in (("U", self.c_gU, [2, 128]), ("negU", self.c_gnegU, [2, 128]), ("NML", self.c_gNML, [2, 128]),
                             ("NMQ", self.c_gNMQ, [2, 128])):
            tl = P.sb([128, 2, 128], F32)
            P.dma("sp", tl[:], src.t.ap().rearrange("d p i -> p d i"), R=[src], W=[tl])
            cst[nm] = tl
        ind = P.sb([128, 2, 64], F32)
        P.dma("sp", ind[:], self.c_gind.t.ap().rearrange("c p m -> p c m"), R=[self.c_gind], W=[ind])
        obf = P.sb([128, 128], F32)
        P.dma("sp", obf[:], self.c_gob[:], R=[self.c_gob], W=[obf])
        negones = P.sb([128, 128], F32)
        self.ts("dve", negones[:], self.onesf[:], -1.0, None, ALU.mult, None, R=[self.onesf], W=[negones])
        S32 = P.sb([64, 4, 64], F32)
        S16 = P.sb([64, 4, 64], BF16)
        NB = 2
        rhs1 = [P.sb([128, 4, 128], F32) for _ in range(NB)]
        gbm = [P.sb([128, 4, 128], F32) for _ in range(NB)]
        t1 = [P.sb([128, 4, 128], F32) for _ in range(NB)]
        t2 = [P.sb([128, 4, 128], F32) for _ in range(NB)]
        XT = [P.sb([128, 4, 128], BF16) for _ in range(NB)]
        X = [P.sb([128, 4, 128], BF16) for _ in range(NB)]
        Rm = [P.sb([128, 4, 128], BF16) for _ in range(NB)]
        qkT = [P.sb([128, 4, 128], BF16) for _ in range(NB)]
        sml = [P.sb([128, 16], F32) for _ in range(NB)]
        GL = [P.sb([64, 8], F32) for _ in range(NB)]
        vb = [P.sb([128, 4, 64], BF16) for _ in range(NB)]
        kbg = [P.sb([128, 4, 64], BF16) for _ in range(NB)]
        kdec = [P.sb([128, 4, 64], BF16) for _ in range(NB)]
        qdec = [P.sb([128, 4, 64], BF16) for _ in range(NB)]
        uu = [P.sb([128, 4, 64], F32) for _ in range(NB)]
        wT = [P.sb([64, 4, 128], BF16) for _ in range(NB)]
        qdT = [P.sb([64, 4, 128], BF16) for _ in range(NB)]
        vn = [P.sb([128, 4, 64], BF16) for _ in range(2)]
        n = 0
        for d in range(2):
            order = list(range(NT)) if d == 0 else [1, 0] + list(range(NT - 1, 1, -1))
            P.op("dve", lambda e: e.memset(S32[:], 0.0), W=[S32])
            P.op("dve", lambda e: e.memset(S16[:], 0.0), W=[S16])
            U_ = cst["U"][:, d, :]
            nU_ = cst["negU"][:, d, :]
            RU = [cst["U"], cst["negU"]]
            for tl in order:
                if TRG < 2:
                    continue
                i = n % NB
                n += 1
                ts_ = slice(tl * 128, (tl + 1) * 128)
                gd = gg[:, tl, d * 4:(d + 1) * 4]
                bd = beta[:, tl, d * 4:(d + 1) * 4]
                nbd = nbeta[:, tl, d * 4:(d + 1) * 4]
                self.tt("dve", rhs1[i][:], U_.unsqueeze(1).to_broadcast([128, 4, 128]),
                        gd.unsqueeze(2).to_broadcast([128, 4, 128]), ALU.mult, R=[cst["U"], gg], W=[rhs1[i]])
                r1f = rhs1[i][:].rearrange("p h i -> p (h i)")
                self.mm(pb[0][:, :], self.onesf[:], r1f, True, True, R=[self.onesf, rhs1[i]], W=[pb[0]])
                if TRG < 2.2:
                    continue
                sb_ = pb[7]
                self.mm(sb_[:, 0:4], U_, gd, True, True, R=RU + [gg], W=[sb_])
                self.mm(sb_[:, 4:8], obf[:], gd, True, True, R=[obf, gg], W=[sb_])
                self.mm(sb_[0:64, 8:12], ind[:, 0, :], gd, True, True, R=[ind, gg], W=[sb_])
                self.mm(sb_[0:64, 12:16], ind[:, 1, :], gd, True, True, R=[ind, gg], W=[sb_])
                sm_ = sml[i]
                self.tt("dve", sm_[:, 4:8], sb_[:, 4:8], sb_[:, 0:4], ALU.subtract, R=[sb_], W=[sm_]) if False else None
                self.cp("dve", sm_[:, 0:8], sb_[:, 0:8], R=[sb_], W=[sm_])
                self.cp("dve", sm_[:, 12:16], sb_[:, 0:4], R=[sb_], W=[sm_])
                self.stt("dve", gbm[i][:], pb[0][:, :].rearrange("p (h j) -> p h j", j=128), -1.0,
                         sm_[:, 12:16].unsqueeze(2).to_broadcast([128, 4, 128]), ALU.mult, ALU.add,
                         R=[pb[0], sm_], W=[gbm[i]])
                self.tt("dve", sm_[:, 4:8], sm_[:, 4:8], sm_[:, 0:4], ALU.subtract, R=[sm_], W=[sm_])
                self.act(sm_[:, 0:8], sm_[:, 0:8], AF.Exp, R=[sm_], W=[sm_])
                self.act(GL[i][:], sb_[0:64, 8:16], AF.Exp, R=[sb_], W=[GL[i]])
                self.tt("dve", sm_[:, 8:12], sm_[:, 0:4], bd, ALU.mult, R=[sm_, beta], W=[sm_])
                if TRG < 2.3:
                    continue
                for h in range(4):
                    hp, hl = h // 2, h % 2
                    hs = slice(hl * 64, hl * 64 + 64)
                    self.mm(pb[2][:, h * 128:(h + 1) * 128], kT[hp][hs, ts_], kT[hp][hs, ts_], True, True, R=[kT[hp]], W=[pb[2]])
                    self.mm(pb[3][:, h * 128:(h + 1) * 128], kT[hp][hs, ts_], qT[hp][hs, ts_], True, True, R=[kT[hp], qT[hp]], W=[pb[3]])
                if TRG < 2.4:
                    continue
                self.stt("dve", t1[i][:], gbm[i][:], 0.0,
                         cst["NML"][:, d, :].unsqueeze(1).to_broadcast([128, 4, 128]), ALU.min, ALU.add,
                         R=[gbm[i], cst["NML"]], W=[t1[i]])
                self.act(t1[i][:], t1[i][:], AF.Exp, R=[t1[i]], W=[t1[i]])
                self.tt("dve", t1[i][:], t1[i][:], pb[2][:, :].rearrange("p (h j) -> p h j", j=128), ALU.mult, R=[t1[i], pb[2]], W=[t1[i]])
                self.tt("pool", XT[i][:], t1[i][:], nbd.unsqueeze(2).to_broadcast([128, 4, 128]), ALU.mult, R=[t1[i], nbeta], W=[XT[i]])
                if TRG < 2.5:
                    continue
                self.stt("dve", t2[i][:], gbm[i][:], 0.0,
                         cst["NMQ"][:, d, :].unsqueeze(1).to_broadcast([128, 4, 128]), ALU.max, ALU.subtract,
                         R=[gbm[i], cst["NMQ"]], W=[t2[i]])
                self.act(t2[i][:], t2[i][:], AF.Exp, R=[t2[i]], W=[t2[i]], scale=-1.0)
                self.tt("dve", qkT[i][:], t2[i][:], pb[3][:, :].rearrange("p (h j) -> p h j", j=128), ALU.mult, R=[t2[i], pb[3]], W=[qkT[i]])
                if TRG < 3:
                    continue
                b4 = pb[4]
                p4 = b4[:].bitcast(BF16)
                for h in range(4):
                    P.op("pe", lambda e, h=h, p4=p4, i=i: e.transpose(p4[:, h * 128:(h + 1) * 128], XT[i][:, h, :], self.ident[:]),
                         R=[XT[i], self.ident], W=[b4])
                self.cp("act", X[i][:].rearrange("p h j -> p (h j)"), p4[:, 0:512], R=[b4], W=[X[i]])
                self.tt("pool", Rm[i][:], X[i][:], self.ident[:].unsqueeze(1).to_broadcast([128, 4, 128]), ALU.add,
                        R=[X[i], self.ident], W=[Rm[i]])
                Y, YT = X[i], XT[i]
                for kk in range(1, 6):
                    if kk < 5:
                        for h in range(4):
                            self.mm(pb[0][:, h * 128:(h + 1) * 128], YT[:, h, :], Y[:, h, :], True, True, R=[Y, YT], W=[pb[0]])
                    for h in range(4):
                        self.mm(pb[1][:, h * 128:(h + 1) * 128], Y[:, h, :], YT[:, h, :], True, True, R=[Y, YT], W=[pb[1]])
                    if kk < 5:
                        self.cp("act", Y[:].rearrange("p h j -> p (h j)"), pb[0][:, :], R=[pb[0]], W=[Y])
                    self.cp("dve", YT[:].rearrange("p h j -> p (h j)"), pb[1][:, :], R=[pb[1]], W=[YT])
                    for h in range(4):
                        self.mm(pb[2][:, h * 128:(h + 1) * 128], YT[:, h, :], Rm[i][:, h, :], True, True, R=[YT, Rm[i]], W=[pb[2]])
                    self.tt("dve", Rm[i][:].rearrange("p h j -> p (h j)"), Rm[i][:].rearrange("p h j -> p (h j)"), pb[2][:, :], ALU.add,
                            R=[Rm[i], pb[2]], W=[Rm[i]])
                kv = ktok[:, tl, :].rearrange("p (h d) -> p h d", d=64)
                qv = qtok[:, tl, :].rearrange("p (h d) -> p h d", d=64)
                vv = vtok[:, tl, :].rearrange("p (h d) -> p h d", d=64)
                self.tt("pool", vb[i][:], vv, bd.unsqueeze(2).to_broadcast([128, 4, 64]), ALU.mult, R=[vtok, beta], W=[vb[i]])
                self.tt("pool", kbg[i][:], kv, sm_[:, 8:12].unsqueeze(2).to_broadcast([128, 4, 64]), ALU.mult, R=[ktok, sm_], W=[kbg[i]])
                self.tt("pool", kdec[i][:], kv, sm_[:, 4:8].unsqueeze(2).to_broadcast([128, 4, 64]), ALU.mult, R=[ktok, sm_], W=[kdec[i]])
                self.tt("pool", qdec[i][:], qv, sm_[:, 0:4].unsqueeze(2).to_broadcast([128, 4, 64]), ALU.mult, R=[qtok, sm_], W=[qdec[i]])
                for h in range(4):
                    self.mm(pb[3][:, h * 64:(h + 1) * 64], Rm[i][:, h, :], vb[i][:, h, :], True, True, R=[Rm[i], vb[i]], W=[pb[3]])
                self.cp("act", uu[i][:].rearrange("p h d -> p (h d)"), pb[3][:, 0:256], R=[pb[3]], W=[uu[i]])
                for h in range(4):
                    self.mm(pb[4][0:64, h * 128:(h + 1) * 128], kbg[i][:, h, :], Rm[i][:, h, :], True, True, R=[kbg[i], Rm[i]], W=[pb[4]])
                self.cp("dve", wT[i][:].rearrange("p h j -> p (h j)"), pb[4][0:64, :], R=[pb[4]], W=[wT[i]])
                for h in range(4):
                    P.op("pe", lambda e, h=h, p4=p4, i=i: e.transpose(p4[0:64, h * 128:(h + 1) * 128], qdec[i][:, h, :], self.ident[:]),
                         R=[qdec[i], self.ident], W=[b4])
                self.cp("act", qdT[i][:].rearrange("p h j -> p (h j)"), p4[0:64, 0:512], R=[b4], W=[qdT[i]])
                for c in ([0, 1] if d == 0 else [1, 0]):
                    if TRG < 4:
                        continue
                    cs = slice(c * 64, (c + 1) * 64)
                    vn_ = vn[c]
                    for h in range(4):
                        self.mm(pb[5][:, h * 64:(h + 1) * 64], wT[i][:, h, :], S16[:, h, :], True, True, R=[wT[i], S16], W=[pb[5]])
                    self.tt("dve", vn_[cs].rearrange("p h d -> p (h d)"), uu[i][cs].rearrange("p h d -> p (h d)"), pb[5][cs, 0:256],
                            ALU.subtract, R=[uu[i], pb[5]], W=[vn_])
                    for h in range(4):
                        self.mm(pb[6][:, h * 64:(h + 1) * 64], qdT[i][:, h, :], S16[:, h, :], True, False, R=[qdT[i], S16], W=[pb[6]])
                        self.mm(pb[6][:, h * 64:(h + 1) * 64], qkT[i][cs, h, :], vn_[cs, h, :], False, True, R=[qkT[i], vn_], W=[pb[6]])
                    if d == 0:
                        self.cp("act", oacc[cs, tl, :], pb[6][cs, 0:256], R=[pb[6]], W=[oacc])
                    else:
                        self.tt("dve", oacc[cs, tl, :], oacc[cs, tl, :], pb[6][cs, 0:256], ALU.add, R=[oacc, pb[6]], W=[oacc])
                    for h in range(4):
                        self.mm(pb[7][0:64, 256 + h * 64:256 + (h + 1) * 64], kdec[i][cs, h, :], vn_[cs, h, :], True, True,
                                R=[kdec[i], vn_], W=[pb[7]])
                    self.tt("dve", S32[:], S32[:], GL[i][:, c * 4:(c + 1) * 4].unsqueeze(2).to_broadcast([64, 4, 64]), ALU.mult,
                            R=[S32, GL[i]], W=[S32])
                    self.tt("dve", S32[:].rearrange("p h d -> p (h d)"), S32[:].rearrange("p h d -> p (h d)"), pb[7][0:64, 256:512], ALU.add,
                            R=[S32, pb[7]], W=[S32])
                    self.cp("act", S16[:], S32[:], R=[S32], W=[S16])
        gdn_g = P.sb([128, 64], F32)
        P.dma("sp", gdn_g[:], self.dn_norm_g.t.ap()[l].partition_broadcast(128), R=[self.dn_norm_g], W=[gdn_g])
        gt = [P.sb([128, 256], BF16) for _ in range(2)]
        gs = [P.sb([128, 256], F32) for _ in range(2)]
        sq2 = [P.sb([128, 4, 64], F32) for _ in range(2)]
        s8 = [P.sb([128, 8], F32) for _ in range(2)]
        ob = [P.sb([128, 256], BF16) for _ in range(2)]
        tiles = list(range(0 if ctx_out else 2, NT))
        for nn, t in enumerate(tiles):
            i = nn % 2
            rs = slice(t * 128, (t + 1) * 128)
            P.dma("sp", gt[i][:], self.gated[rs, :], R=[self.gated], W=[gt[i]])
            self.act(gs[i][:], gt[i][:], AF.Silu, R=[gt[i]], W=[gs[i]])
            ov = oacc[:, t, :].rearrange("p (h d) -> p h d", d=64)
            self.tt("pool", sq2[i][:], ov, ov, ALU.mult, R=[oacc], W=[sq2[i]])
            P.op("dve", lambda e, i=i: e.reduce_sum(s8[i][:, 0:4], sq2[i][:], AX.X), R=[sq2[i]], W=[s8[i]])
            self.rsqrt_mean(s8[i][:, 4:8], s8[i][:, 0:4], 64.0, 1e-6, R=[s8[i]], W=[s8[i]])
            self.tt("dve", sq2[i][:], ov, s8[i][:, 4:8].unsqueeze(2).to_broadcast([128, 4, 64]), ALU.mult, R=[oacc, s8[i]], W=[sq2[i]])
            self.tt("dve", sq2[i][:], sq2[i][:], gdn_g[:].unsqueeze(1).to_broadcast([128, 4, 64]), ALU.mult, R=[sq2[i], gdn_g], W=[sq2[i]])
            self.tt("dve", ob[i][:], sq2[i][:].rearrange("p h d -> p (h d)"), gs[i][:], ALU.mult, R=[sq2[i], gs[i]], W=[ob[i]])
            P.dma("pool", self.mix[rs, 256:512], ob[i][:], R=[ob[i]], W=[self.mix])
        P.flush()
        P.release_mid()

def build(nlayers=DEPTH, dbg=False, stages=None):
    B = Builder(nlayers, dbg, stages)
    B.declare()
    B.stage_init()
    for l in range(nlayers):
        if stages is not None and "nomod" in stages:
            continue
        B.stage_mod(l)
        ctx_out = l < DEPTH - 1
        if B.want("inproj"):
            B.stage_inproj(l)
        if B.want("na"):
            B.stage_na(l, ctx_out)
        if B.want("diff"):
            B.stage_diff(l, ctx_out)
        if B.want("fft"):
            B.stage_fft(l, ctx_out)
        if B.want("gdn"):
            B.stage_gdn(l, ctx_out)
        if stages is not None and "inject_dn" in stages:
            inj = B.din("dn_inj", [T, 256])
            B.P.dma("pool", B.mix[:, 256:512], inj[:], R=[inj], W=[B.mix])
            B.P.flush()
        if B.want("outproj"):
            B.stage_outproj(l, ctx_out)
        if B.want("moe"):
            B.stage_route(l, ctx_out)
            B.stage_ffn(l, ctx_out)
            B.stage_combine(l, ctx_out, final=(l == nlayers - 1))
    B.P.close()
    return B


_CONST = None


def prep_shared(inputs):
    global _CONST
    if _CONST is None:
        _CONST = const_tables()
    sh = dict(_CONST)
    f = lambda k: np.ascontiguousarray(np.asarray(inputs[k], dtype=np.float32))
    for k in ("w_mod", "b_mod", "norm1_g", "norm2_g", "ft_w", "w_out", "w_router", "w_gate", "w_up", "w_down",
              "final_norm_g", "df_norm_g", "dn_norm_g", "dn_conv_w"):
        sh[k] = f(k)
    sh["w_in_p"] = np.ascontiguousarray(f("w_in")[:, :, in_proj_perm()])
    rp = f("na_rpb")
    pad = np.zeros((DEPTH, 4, 15, 128), np.float32)
    pad[..., 48:79] = rp[:, :, ::-1, ::-1]
    sh["rpbpad"] = pad
    sh["df_lambda"] = f("df_lambda").reshape(DEPTH, 128)
    sh["dn_a_log"] = f("dn_a_log").reshape(DEPTH, 8)
    sh["dn_dt_bias"] = f("dn_dt_bias").reshape(DEPTH, 8)
    return sh


def prep_sample(inputs, b):
    xc = np.concatenate([np.asarray(inputs["ctx"][b], np.float32), np.asarray(inputs["x"][b], np.float32)], 0)
    cc = np.stack([np.asarray(inputs["c"][b], np.float32), np.asarray(inputs["c_ctx"], np.float32)], 0)
    return {"xc": np.ascontiguousarray(xc), "cc": np.ascontiguousarray(cc)}


_BUILT = {}
NCORES = 4


def kernel(**inputs):
    if "B" not in _BUILT:
        _BUILT["B"] = build()
    B = _BUILT["B"]
    sh = prep_shared(inputs)
    sh = {k: v for k, v in sh.items() if k in B.inputs}
    in_maps = []
    for core in range(NCORES):
        m = dict(sh)
        m.update(prep_sample(inputs, core % 4))
        in_maps.append(m)
    res = run_bass_kernel_spmd(B.nc, in_maps, core_ids=list(range(NCORES)))
    out = np.stack([np.asarray(res.results[b]["out"], dtype=np.float32) for b in range(4)], 0)
    return out
```
